# Optimizing a Trainium2 kernel written in Bass

```python
import math
import jax
import jax.numpy as jnp
from jax import lax
import numpy as np


D_MODEL = 2048
BATCH = 1
SEQ = 16384
DEPTH = 4

CHUNK = 64
Q_BLOCK = 128
ROPE_THETA = 10000.0
EPS = 1e-6
NEG = -1e30

A_HEAD_DIM = 128
A_WIDTH = D_MODEL // 4
A_HEADS = A_WIDTH // A_HEAD_DIM
IDX_HEADS = 8
IDX_DIM = 64
TOPK_MAX = 256

S5_GROUP = 16
S5_WIDTH = D_MODEL // 4
S5_GROUPS = S5_WIDTH // S5_GROUP
S5_STATE = 64
S5_DT_MIN = 0.001
S5_DT_MAX = 0.1

C_QK_DIM = 64
C_V_DIM = 2 * C_QK_DIM
C_WIDTH = D_MODEL // 2
C_HEADS = C_WIDTH // C_V_DIM

MIX_WIDTH = A_WIDTH + S5_WIDTH + C_WIDTH
IN_WIDTHS = (A_WIDTH, A_WIDTH, A_WIDTH, IDX_HEADS * IDX_DIM, IDX_DIM, IDX_HEADS, S5_WIDTH, 2 * C_HEADS * C_QK_DIM, 2 * C_HEADS * C_QK_DIM, C_HEADS * C_V_DIM)
IN_WIDTH = 3 * A_WIDTH + IDX_HEADS * IDX_DIM + IDX_DIM + IDX_HEADS + S5_WIDTH + 4 * C_HEADS * C_QK_DIM + C_HEADS * C_V_DIM

MEM_TOKENS = 256
X_HEADS = 4
X_HEAD_DIM = D_MODEL // X_HEADS
D_FF = 4 * D_MODEL

kernel_name = 'hybrid_dsa_s5_diffattn_encoder'


def _rmsnorm(x, g):
    xf = x.astype(jnp.float32)
    xf = xf * lax.rsqrt(jnp.mean(xf * xf, axis=-1, keepdims=True) + EPS)
    return (xf * g.astype(jnp.float32)).astype(x.dtype)


def _rope_tables(L, dim):
    inv = ROPE_THETA ** (-jnp.arange(0, dim, 2, dtype=jnp.float32) / dim)
    ang = jnp.arange(L, dtype=jnp.float32)[:, None] * inv[None, :]
    return jnp.cos(ang), jnp.sin(ang)


def _rope(t, cos, sin):
    shape = (1, cos.shape[0]) + (1,) * (t.ndim - 3) + (cos.shape[1],)
    c = cos.reshape(shape).astype(t.dtype)
    s = sin.reshape(shape).astype(t.dtype)
    t1, t2 = jnp.split(t, 2, axis=-1)
    return jnp.concatenate([t1 * c - t2 * s, t1 * s + t2 * c], axis=-1)


def _split_columns(proj):
    outs, start = [], 0
    for w in IN_WIDTHS:
        outs.append(proj[..., start:start + w])
        start += w
    return outs


def _to_blocks(t):
    B, L = t.shape[:2]
    t = t.reshape((B, L // Q_BLOCK, Q_BLOCK) + t.shape[2:])
    return jnp.moveaxis(t, 1, 0)


def _from_blocks(t):
    t = jnp.moveaxis(t, 0, 1)
    return t.reshape((t.shape[0], t.shape[1] * t.shape[2]) + t.shape[3:])


def _dsa_mixer(q, k, v, q_idx, k_idx, w_idx):
    B, L = q.shape[:2]
    top_k = min(TOPK_MAX, L // 4)
    key_chunk = jnp.arange(L) // CHUNK
    bidx = jnp.arange(B)[:, None, None]
    k_idx_f = k_idx.astype(jnp.float32)

    def block(args):
        qb, qib, wb, qpos = args
        q_chunk = qpos // CHUNK
        allowed = key_chunk[None, :] <= q_chunk[:, None]
        dots = jnp.einsum('bqhd,bkd->bqhk', qib.astype(jnp.float32), k_idx_f) * (IDX_DIM ** -0.5)
        score = jnp.einsum('bqh,bqhk->bqk', wb.astype(jnp.float32) * (IDX_HEADS ** -0.5), jax.nn.relu(dots))
        score = jnp.where(allowed[None], score, NEG)
        _, sel = lax.top_k(score, top_k)
        valid = (sel // CHUNK) <= q_chunk[None, :, None]
        k_sel = k[bidx, sel]
        v_sel = v[bidx, sel]
        s = jnp.einsum('bqhd,bqkhd->bhqk', qb, k_sel).astype(jnp.float32) * (A_HEAD_DIM ** -0.5)
        s = jnp.where(valid[:, None], s, NEG)
        p = jax.nn.softmax(s, axis=-1).astype(v.dtype)
        return jnp.einsum('bhqk,bqkhd->bqhd', p, v_sel)

    qpos = jnp.arange(L).reshape(L // Q_BLOCK, Q_BLOCK)
    out = lax.map(block, (_to_blocks(q), _to_blocks(q_idx), _to_blocks(w_idx), qpos))
    return _from_blocks(out)


def _s5_mixer(u, a_re, a_im, log_dt, b_re, b_im, c_re, c_im, d_skip, w_glu, b_glu):
    f32 = jnp.float32
    B, L = u.shape[:2]
    uf = u.astype(f32).reshape(B, L, S5_GROUPS, S5_GROUP)
    a_re = a_re.astype(f32)
    a_im = a_im.astype(f32)
    dt = jnp.exp(log_dt.astype(f32))[:, None]
    mag = jnp.exp(a_re * dt)
    lb_re = mag * jnp.cos(a_im * dt)
    lb_im = mag * jnp.sin(a_im * dt)
    den = a_re * a_re + a_im * a_im
    f_re = ((lb_re - 1.0) * a_re + lb_im * a_im) / den
    f_im = (lb_im * a_re - (lb_re - 1.0) * a_im) / den
    b_re = b_re.astype(f32)
    b_im = b_im.astype(f32)
    bb_re = f_re[..., None] * b_re - f_im[..., None] * b_im
    bb_im = f_re[..., None] * b_im + f_im[..., None] * b_re
    bu_re = jnp.einsum('gnp,blgp->blgn', bb_re, uf)
    bu_im = jnp.einsum('gnp,blgp->blgn', bb_im, uf)
    al_re = jnp.broadcast_to(lb_re, bu_re.shape)
    al_im = jnp.broadcast_to(lb_im, bu_im.shape)

    def combine(e1, e2):
        a1r, a1i, b1r, b1i = e1
        a2r, a2i, b2r, b2i = e2
        return (a2r * a1r - a2i * a1i,
                a2r * a1i + a2i * a1r,
                a2r * b1r - a2i * b1i + b2r,
                a2r * b1i + a2i * b1r + b2i)

    _, _, x_re, x_im = lax.associative_scan(combine, (al_re, al_im, bu_re, bu_im), axis=1)
    y = (jnp.einsum('gpn,blgn->blgp', c_re.astype(f32), x_re)
         - jnp.einsum('gpn,blgn->blgp', c_im.astype(f32), x_im)
         + d_skip.astype(f32) * uf)
    y = jax.nn.gelu(y.reshape(B, L, S5_WIDTH))
    y = y * jax.nn.sigmoid(y @ w_glu.astype(f32) + b_glu.astype(f32))
    return y.astype(u.dtype)


def _diff_mixer(q, k, v, lam, sub_gain, lam_init):
    B, L = q.shape[:2]
    q = q.reshape(B, L, C_HEADS, 2, C_QK_DIM)
    k = k.reshape(B, L, C_HEADS, 2, C_QK_DIM)
    key_chunk = jnp.arange(L) // CHUNK

    def block(args):
        qb, qpos = args
        allowed = key_chunk[None, :] <= (qpos // CHUNK)[:, None]
        s = jnp.einsum('bqhcd,bkhcd->bchqk', qb, k).astype(jnp.float32) * (C_QK_DIM ** -0.5)
        s = jnp.where(allowed, s, NEG)
        p = jax.nn.softmax(s, axis=-1)
        diff = (p[:, 0] - lam * p[:, 1]).astype(v.dtype)
        return jnp.einsum('bhqk,bkhd->bqhd', diff, v)

    qpos = jnp.arange(L).reshape(L // Q_BLOCK, Q_BLOCK)
    o = _from_blocks(lax.map(block, (_to_blocks(q), qpos)))
    o = _rmsnorm(o, sub_gain) * (1.0 - lam_init)
    return o.reshape(B, L, C_WIDTH)


def _cross_attn(h, mem_n, wq, wk, wv, wo):
    B, L = h.shape[:2]
    M = mem_n.shape[1]
    q = (h @ wq).reshape(B, L, X_HEADS, X_HEAD_DIM)
    k = (mem_n @ wk).reshape(B, M, X_HEADS, X_HEAD_DIM)
    v = (mem_n @ wv).reshape(B, M, X_HEADS, X_HEAD_DIM)
    s = jnp.einsum('bqhd,bkhd->bhqk', q, k).astype(jnp.float32) * (X_HEAD_DIM ** -0.5)
    p = jax.nn.softmax(s, axis=-1).astype(h.dtype)
    o = jnp.einsum('bhqk,bkhd->bqhd', p, v).reshape(B, L, D_MODEL)
    return o @ wo


def setup_inputs(seed: int = 0) -> dict:
    key = jax.random.key(seed)
    ks = jax.random.split(key, 32)
    f32 = jnp.float32
    G, N, P = S5_GROUPS, S5_STATE, S5_GROUP

    def nrm(k, shape, scale):
        return scale * jax.random.normal(k, shape, f32)

    def gain(k, shape):
        return 1.0 + 0.01 * jax.random.normal(k, shape, f32)

    return {
        'x': nrm(ks[0], (BATCH, SEQ, D_MODEL), 1.0),
        'mem': nrm(ks[1], (BATCH, MEM_TOKENS, D_MODEL), 1.0),
        'norm_mix': gain(ks[2], (DEPTH, D_MODEL)),
        'w_in': nrm(ks[3], (DEPTH, D_MODEL, IN_WIDTH), D_MODEL ** -0.5),
        's5_a_re': -0.5 + nrm(ks[4], (DEPTH, G, N), 0.01),
        's5_a_im': math.pi * jnp.arange(N, dtype=f32) + nrm(ks[5], (DEPTH, G, N), 0.01),
        's5_log_dt': jax.random.uniform(ks[6], (DEPTH, G), f32, math.log(S5_DT_MIN), math.log(S5_DT_MAX)),
        's5_b_re': nrm(ks[7], (DEPTH, G, N, P), (2 * P) ** -0.5),
        's5_b_im': nrm(ks[8], (DEPTH, G, N, P), (2 * P) ** -0.5),
        's5_c_re': nrm(ks[9], (DEPTH, G, P, N), N ** -0.5),
        's5_c_im': nrm(ks[10], (DEPTH, G, P, N), N ** -0.5),
        's5_d': nrm(ks[11], (DEPTH, G, P), 1.0),
        's5_w_glu': nrm(ks[12], (DEPTH, S5_WIDTH, S5_WIDTH), S5_WIDTH ** -0.5),
        's5_b_glu': nrm(ks[13], (DEPTH, S5_WIDTH), 0.01),
        'diff_lam_q1': nrm(ks[14], (DEPTH, C_QK_DIM), 0.1),
        'diff_lam_k1': nrm(ks[15], (DEPTH, C_QK_DIM), 0.1),
        'diff_lam_q2': nrm(ks[16], (DEPTH, C_QK_DIM), 0.1),
        'diff_lam_k2': nrm(ks[17], (DEPTH, C_QK_DIM), 0.1),
        'diff_subln': gain(ks[18], (DEPTH, C_V_DIM)),
        'w_out': nrm(ks[19], (DEPTH, MIX_WIDTH, D_MODEL), MIX_WIDTH ** -0.5),
        'norm_xattn': gain(ks[20], (DEPTH, D_MODEL)),
        'norm_mem': gain(ks[21], (DEPTH, D_MODEL)),
        'xattn_q': nrm(ks[22], (DEPTH, D_MODEL, D_MODEL), D_MODEL ** -0.5),
        'xattn_k': nrm(ks[23], (DEPTH, D_MODEL, D_MODEL), D_MODEL ** -0.5),
        'xattn_v': nrm(ks[24], (DEPTH, D_MODEL, D_MODEL), D_MODEL ** -0.5),
        'xattn_o': nrm(ks[25], (DEPTH, D_MODEL, D_MODEL), D_MODEL ** -0.5),
        'norm_mlp': gain(ks[26], (DEPTH, D_MODEL)),
        'w_ff1': nrm(ks[27], (DEPTH, D_MODEL, D_FF), D_MODEL ** -0.5),
        'w_ff2': nrm(ks[28], (DEPTH, D_FF, D_MODEL), D_FF ** -0.5),
        'norm_final': gain(ks[29], (D_MODEL,)),
    }


def reference(x, mem, norm_mix, w_in, s5_a_re, s5_a_im, s5_log_dt, s5_b_re, s5_b_im, s5_c_re, s5_c_im, s5_d, s5_w_glu, s5_b_glu, diff_lam_q1, diff_lam_k1, diff_lam_q2, diff_lam_k2, diff_subln, w_out, norm_xattn, norm_mem, xattn_q, xattn_k, xattn_v, xattn_o, norm_mlp, w_ff1, w_ff2, norm_final):
    f32 = jnp.float32
    B, L, _ = x.shape
    cos_a, sin_a = _rope_tables(L, A_HEAD_DIM)
    cos_i, sin_i = _rope_tables(L, IDX_DIM)
    cos_c, sin_c = _rope_tables(L, C_QK_DIM)
    for l in range(DEPTH):
        h = _rmsnorm(x, norm_mix[l])
        q_a, k_a, v_a, q_i, k_i, w_i, u_s, q_c, k_c, v_c = _split_columns(h @ w_in[l])

        q_a = _rope(q_a.reshape(B, L, A_HEADS, A_HEAD_DIM), cos_a, sin_a)
        k_a = _rope(k_a.reshape(B, L, A_HEADS, A_HEAD_DIM), cos_a, sin_a)
        v_a = v_a.reshape(B, L, A_HEADS, A_HEAD_DIM)
        q_i = _rope(q_i.reshape(B, L, IDX_HEADS, IDX_DIM), cos_i, sin_i)
        k_i = _rope(k_i, cos_i, sin_i)
        o_a = _dsa_mixer(q_a, k_a, v_a, q_i, k_i, w_i).reshape(B, L, A_WIDTH)

        o_s = _s5_mixer(u_s, s5_a_re[l], s5_a_im[l], s5_log_dt[l], s5_b_re[l], s5_b_im[l], s5_c_re[l], s5_c_im[l], s5_d[l], s5_w_glu[l], s5_b_glu[l])

        q_c = _rope(q_c.reshape(B, L, 2 * C_HEADS, C_QK_DIM), cos_c, sin_c)
        k_c = _rope(k_c.reshape(B, L, 2 * C_HEADS, C_QK_DIM), cos_c, sin_c)
        v_c = v_c.reshape(B, L, C_HEADS, C_V_DIM)
        lam_init = 0.8 - 0.6 * math.exp(-0.3 * l)
        lam = (jnp.exp(jnp.sum(diff_lam_q1[l].astype(f32) * diff_lam_k1[l].astype(f32)))
               - jnp.exp(jnp.sum(diff_lam_q2[l].astype(f32) * diff_lam_k2[l].astype(f32)))
               + lam_init)
        o_c = _diff_mixer(q_c, k_c, v_c, lam, diff_subln[l], lam_init)

        x = x + jnp.concatenate([o_a, o_s, o_c], axis=-1) @ w_out[l]

        x = x + _cross_attn(_rmsnorm(x, norm_xattn[l]), _rmsnorm(mem, norm_mem[l]), xattn_q[l], xattn_k[l], xattn_v[l], xattn_o[l])

        h = _rmsnorm(x, norm_mlp[l])
        x = x + jnp.square(jax.nn.relu(h @ w_ff1[l])) @ w_ff2[l]
    return _rmsnorm(x, norm_final)
```

```python
import math
import contextlib
import numpy as np
import concourse.bass as bass
import concourse.mybir as mybir
from concourse.bass_utils import run_bass_kernel_spmd

F32 = mybir.dt.float32
BF16 = mybir.dt.bfloat16
AF = mybir.ActivationFunctionType
ALU = mybir.AluOpType
AX = mybir.AxisListType

NCORES = 8
D = 2048
L = 16384
DEPTH = 4
TPC = L // NCORES
NSLOT = TPC // 128
EPS = 1e-6
IN_WIDTH = 5704
OFF = dict(q_a=0, k_a=512, v_a=1024, q_i=1536, k_i=2048, w_i=2112, u_s=2120, q_c=2632, k_c=3656, v_c=4680)


def blk_of(c, j):
    return 16 * (j // 2) + (c if j % 2 == 0 else 15 - c)


class Buf:
    __slots__ = ("name", "w", "r")

    def __init__(self, name):
        self.name = name
        self.w = None
        self.r = {}


class Prog:
    SAME_ENGINE_SYNC = True

    def __init__(self, nc, n_dma_sems=24):
        self.nc = nc
        self.es = contextlib.ExitStack()
        self.eng = {"pe": nc.tensor, "act": nc.scalar, "dve": nc.vector, "pool": nc.gpsimd, "sp": nc.sync}
        self.sems = []
        self.esem = {}
        self.cnt = {}
        for k in self.eng:
            self.esem[k] = self._newsem("e_" + k)
            self.cnt[k] = 0
        self.waited = {k: {} for k in self.eng}
        self.pe_sems = set()
        self.dma_pool = {}
        for q in ("sp", "pool", "act"):
            self.dma_pool[q] = [[self._newsem(f"d_{q}{i}"), 0] for i in range(n_dma_sems if q != "act" else 4)]
        self.dma_rr = {q: 0 for q in self.dma_pool}
        self.n_inst = 0

    def _newsem(self, name):
        s = self.es.enter_context(self.nc.semaphore(name))
        self.sems.append(s)
        return len(self.sems) - 1

    def begin_phase(self, tag):
        self.pes = contextlib.ExitStack()
        self.ptag = tag
        self.pidx = getattr(self, "pidx", 0) + 1

    def end_phase(self):
        self.barrier()
        self.pes.close()
        self.pes = None

    @contextlib.contextmanager
    def sub_scope(self):
        outer = self.pes
        self.pes = contextlib.ExitStack()
        try:
            yield
        finally:
            self.barrier()
            self.pes.close()
            self.pes = outer

    def sb(self, name, shape, dtype):
        self.uid = getattr(self, "uid", 0) + 1
        return self.pes.enter_context(self.nc.sbuf_tensor(f"sb{self.uid}_{name}", list(shape), dtype))

    def ps(self, name, shape, dtype):
        self.uid = getattr(self, "uid", 0) + 1
        return self.pes.enter_context(self.nc.psum_tensor(f"ps{self.uid}_{name}", list(shape), dtype))

    def _wait(self, e, semidx, val):
        if val <= 0:
            return
        if self.waited[e].get(semidx, 0) >= val:
            return
        self.eng[e].wait_ge(self.sems[semidx], val)
        self.waited[e][semidx] = val

    def _deps(self, e, reads, writes):
        deps = {}
        for b in reads:
            if b.w is not None:
                s, v = b.w
                deps[s] = max(deps.get(s, 0), v)
        for b in writes:
            if b.w is not None:
                s, v = b.w
                deps[s] = max(deps.get(s, 0), v)
            for s, v in b.r.items():
                deps[s] = max(deps.get(s, 0), v)
        for s, v in deps.items():
            if (not self.SAME_ENGINE_SYNC) and s == self.esem[e]:
                continue
            if e == "pe" and (s == self.esem["pe"] or s in self.pe_sems):
                continue
            self._wait(e, s, v)

    def _mark(self, ev, reads, writes):
        s, v = ev
        for b in reads:
            if b.r.get(s, 0) < v:
                b.r[s] = v
        for b in writes:
            b.w = ev
            b.r = {}

    SEM_ROTATE = 16000

    def op(self, e, fn, reads=(), writes=()):
        if self.cnt[e] >= self.SEM_ROTATE:
            self.old_esem = getattr(self, "old_esem", [])
            self.old_esem.append((self.esem[e], self.cnt[e]))
            if e == "pe":
                self.pe_sems.add(self.esem[e])
            self.esem[e] = self._newsem(f"e_{e}_{len(self.sems)}")
            self.cnt[e] = 0
        self._deps(e, reads, writes)
        inst = fn(self.eng[e])
        self.cnt[e] += 1
        inst.then_inc(self.sems[self.esem[e]], 1)
        self._mark((self.esem[e], self.cnt[e]), reads, writes)
        self.n_inst += 1

    def dma(self, q, out, in_, reads=(), writes=(), **kw):
        pool = self.dma_pool[q]
        i = self.dma_rr[q]
        self.dma_rr[q] = (i + 1) % len(pool)
        semidx, tot = pool[i]
        self._wait(q, semidx, tot)
        self._deps(q, reads, writes)
        inst = self.eng[q].dma_start(out=out, in_=in_, **kw)
        inst.then_inc(self.sems[semidx], 16)
        pool[i][1] = tot + 16
        self._mark((semidx, tot + 16), reads, writes)
        self.n_inst += 1

    def wait_all(self, e):
        for k in self.eng:
            if k != e:
                self._wait(e, self.esem[k], self.cnt[k])
        for semidx, tot in getattr(self, "old_esem", []):
            self._wait(e, semidx, tot)
        for q, pool in self.dma_pool.items():
            for semidx, tot in pool:
                self._wait(e, semidx, tot)

    def barrier(self):
        for e in self.eng:
            self.wait_all(e)

    def finish(self):
        self.barrier()
        self.nc.all_engine_barrier()
        self.es.close()


def core_positions(c):
    pos = np.empty(TPC, np.int64)
    for j in range(NSLOT):
        pos[128 * j:128 * (j + 1)] = 128 * blk_of(c, j) + np.arange(128)
    return pos


def rope_tables_fm(c):
    pos = core_positions(c).astype(np.float32)
    out = {}
    for nm, dim in (("A", 128), ("C", 64)):
        inv = (np.float32(10000.0) ** (-(np.arange(0, dim, 2, dtype=np.float32)) / np.float32(dim))).astype(np.float32)
        ang = (pos[:, None] * inv[None, :]).astype(np.float32)
        cos = np.cos(ang).astype(np.float32).T
        sin = np.sin(ang).astype(np.float32).T
        rep = 128 // (dim // 2)
        out["cos" + nm] = np.ascontiguousarray(np.tile(cos, (rep, 1)))
        out["sin" + nm] = np.ascontiguousarray(np.tile(sin, (rep, 1)))
    return out


def win_units():
    perm = []
    fm = []

    def add_rope(name, base, nheads, hd, dst, table):
        half = hd // 2
        hpp = 128 // half
        for p0 in range(0, nheads, hpp):
            heads = list(range(p0, min(nheads, p0 + hpp)))
            A = [base + hd * h + i for h in heads for i in range(half)]
            Bc = [base + hd * h + half + i for h in heads for i in range(half)]
            col0 = len(perm)
            perm.extend(A)
            perm.extend(Bc)
            rowsA = [(hd * h, half, half * k) for k, h in enumerate(heads)]
            rowsB = [(hd * h + half, half, half * k) for k, h in enumerate(heads)]
            fm.append(dict(name=name, col0=col0, M=len(A), rope=table, dst=dst, rowsA=rowsA, rowsB=rowsB))

    add_rope("q_a", OFF["q_a"], 4, 128, "qaT", "A")
    add_rope("k_a", OFF["k_a"], 4, 128, "kaT", "A")
    add_rope("q_i", OFF["q_i"], 8, 64, "qiT", "C")
    add_rope("k_i", OFF["k_i"], 1, 64, "kiT", "C")
    add_rope("q_c", OFF["q_c"], 16, 64, "qcT", "C")
    add_rope("k_c", OFF["k_c"], 16, 64, "kcT", "C")
    for p in range(2):
        col0 = len(perm)
        perm.extend(range(OFF["u_s"] + 256 * p, OFF["u_s"] + 256 * (p + 1)))
        fm.append(dict(name="u_s", col0=col0, M=128, rope=None, dst="uT",
                       rowsA=[(256 * p, 128, 0)], rowsB=[(256 * p + 128, 128, 0)]))
    tm = []
    for name, base, width, dst in (("v_a", OFF["v_a"], 512, "va"), ("v_c0", OFF["v_c"], 512, "vc"),
                                   ("v_c1", OFF["v_c"] + 512, 512, "vc"), ("w_i", OFF["w_i"], 8, "wi")):
        col0 = len(perm)
        perm.extend(range(base, base + width))
        tm.append(dict(name=name, col0=col0, width=width, dst=dst, dcol=(512 if name == "v_c1" else 0)))
    assert len(perm) == IN_WIDTH and len(set(perm)) == IN_WIDTH
    return np.array(perm), fm, tm


class NormT:
    def __init__(self, P, ntok, tag):
        self.P = P
        self.ntok = ntok
        self.tag = tag
        self.hT = P.sb(f"{tag}_hT", [128, 16, ntok], BF16)
        self.hT_b = [Buf(f"hT{j}") for j in range(ntok // 128)]

    def alloc_scratch(self):
        P, tag = self.P, self.tag
        self.xs = [P.sb(f"{tag}_xs{i}", [128, D], F32) for i in range(2)]
        self.xs_b = [Buf(f"{tag}_xs{i}") for i in range(2)]
        self.gb = P.sb(f"{tag}_gb", [128, D], F32)
        self.gb_b = Buf("gb")
        self.hs = [P.sb(f"{tag}_hs{i}", [128, D], BF16) for i in range(2)]
        self.hs_b = [Buf(f"hs{i}") for i in range(2)]
        self.st = [P.sb(f"{tag}_st{i}", [128, 4], F32) for i in range(2)]
        self.st_b = [Buf(f"st{i}") for i in range(2)]
        self.tp = [P.ps(f"{tag}_tp{i}", [128, 512], BF16) for i in range(2)]
        self.tp_b = [Buf(f"tp{i}") for i in range(2)]

    def load_gain(self, g_ap_dram):
        P = self.P
        t = g_ap_dram.tensor
        src = bass.AP(tensor=t, offset=g_ap_dram.offset, ap=[[0, 128], [1, D]])
        P.dma("sp", self.gb[:], src, writes=[self.gb_b])

    def run(self, x_dram, g_dram, C, hT=None, hT_b=None, ntok=None):
        with self.P.sub_scope():
            self.alloc_scratch()
            self.epsb = C["epsb"]
            self.load_gain(g_dram)
            self._run(x_dram, C["ident"], C["ident_b"], hT if hT is not None else self.hT,
                      hT_b if hT_b is not None else self.hT_b, ntok if ntok is not None else self.ntok)

    def _run(self, x_dram, ident, ident_b, hT, hT_b, ntok):
        P = self.P
        nslot = ntok // 128
        k = 0
        for j in range(nslot):
            i = j % 2
            P.dma("sp", self.xs[i][:], x_dram[128 * j:128 * (j + 1), :], writes=[self.xs_b[i]])
            P.op("act", lambda e: e.activation(out=self.hs[i][:], in_=self.xs[i][:], func=AF.Square,
                                               accum_out=self.st[i][:, 0:1]),
                 reads=[self.xs_b[i]], writes=[self.hs_b[i], self.st_b[i]])
            P.op("act", lambda e: e.activation(out=self.st[i][:, 1:2], in_=self.st[i][:, 0:1], func=AF.Sqrt,
                                               scale=1.0 / D, bias=self.epsb[:, 0:1]),
                 reads=[self.st_b[i]], writes=[self.st_b[i]])
            P.op("dve", lambda e: e.reciprocal(out=self.st[i][:, 2:3], in_=self.st[i][:, 1:2]),
                 reads=[self.st_b[i]], writes=[self.st_b[i]])
            P.op("dve", lambda e: e.scalar_tensor_tensor(out=self.hs[i][:], in0=self.xs[i][:],
                                                         scalar=self.st[i][:, 2:3], in1=self.gb[:],
                                                         op0=ALU.mult, op1=ALU.mult),
                 reads=[self.xs_b[i], self.st_b[i], self.gb_b], writes=[self.hs_b[i]])
            for q in range(4):
                tb = k % 2
                k += 1
                for r in range(4):
                    kt = 4 * q + r
                    P.op("pe", lambda e: e.transpose(out=self.tp[tb][:, 128 * r:128 * (r + 1)],
                                                     in_=self.hs[i][:, 128 * kt:128 * (kt + 1)],
                                                     identity=ident[:]),
                         reads=[self.hs_b[i], ident_b], writes=[self.tp_b[tb]])
                dst = hT[:, 4 * q:4 * q + 4, 128 * j:128 * (j + 1)]
                src = self.tp[tb][:].rearrange("p (r t) -> p r t", r=4)
                if q % 2 == 0:
                    P.op("act", lambda e: e.copy(out=dst, in_=src), reads=[self.tp_b[tb]], writes=[hT_b[j]])
                else:
                    P.op("dve", lambda e: e.tensor_copy(out=dst, in_=src), reads=[self.tp_b[tb]],
                         writes=[hT_b[j]])


def make_consts(P, ident_dram):
    c = {}
    c["ident_f"] = P.sb("ident_f", [128, 128], F32)
    c["ident_f_b"] = Buf("ident_f")
    c["ident"] = P.sb("ident", [128, 128], BF16)
    c["ident_b"] = Buf("ident")
    c["epsb"] = P.sb("epsb", [128, 1], F32)
    c["epsb_b"] = Buf("epsb")
    P.dma("sp", c["ident_f"][:], ident_dram, writes=[c["ident_f_b"]])
    P.op("dve", lambda e: e.tensor_copy(out=c["ident"][:], in_=c["ident_f"][:]), reads=[c["ident_f_b"]],
         writes=[c["ident_b"]])
    P.op("dve", lambda e: e.memset(c["epsb"][:], EPS), writes=[c["epsb_b"]])
    return c


def emit_phase_A(P, T, FM, TM):
    nc = P.nc
    C = make_consts(P, T["ident"])
    nt = NormT(P, TPC, "A")
    nt.run(T["x"], T["g"], C)

    tabs = {}
    tab_b = Buf("ropetabs")
    for nm in ("cosA", "sinA", "cosC", "sinC"):
        tabs[nm] = P.sb("tab_" + nm, [128, TPC], F32)
        P.dma("sp", tabs[nm][:], T[nm], writes=[tab_b])
    wbuf = [P.sb(f"A_w{i}", [128, 16, 512], BF16) for i in range(2)]
    wbuf_b = [Buf(f"A_w{i}") for i in range(2)]
    pacc = [P.ps(f"A_acc{i}", [128, 512], F32) for i in range(4)]
    pacc_b = [Buf(f"A_acc{i}") for i in range(4)]
    tmp = [P.sb(f"A_t{i}", [128, 512], F32) for i in range(4)]
    tmp_b = [Buf(f"A_t{i}") for i in range(4)]
    ob = [P.sb(f"A_o{i}", [128, 512], BF16) for i in range(4)]
    ob_b = [Buf(f"A_o{i}") for i in range(4)]
    obf = [P.sb(f"A_of{i}", [128, 8], F32) for i in range(2)]
    obf_b = [Buf(f"A_of{i}") for i in range(2)]
    win = T["win"]
    wt = win.tensor

    wi_ = 0
    pa = 0
    oi = 0
    for u in FM:
        wb = wi_ % 2
        wi_ += 1
        M = u["M"]
        src = bass.AP(tensor=wt, offset=win.offset + u["col0"], ap=[[IN_WIDTH, 128], [128 * IN_WIDTH, 16], [1, 2 * M]])
        P.dma("pool", wbuf[wb][:, :, 0:2 * M], src, writes=[wbuf_b[wb]])
        for tg in range(4):
            ts = slice(512 * tg, 512 * (tg + 1))
            a_i = pa % 4
            b_i = (pa + 1) % 4
            pa += 2
            for half, pi in ((0, a_i), (1, b_i)):
                for kt in range(16):
                    P.op("pe", lambda e: e.matmul(out=pacc[pi][0:M, :], lhsT=wbuf[wb][:, kt, half * M:(half + 1) * M],
                                                  rhs=nt.hT[:, kt, ts], start=(kt == 0), stop=(kt == 15)),
                         reads=[wbuf_b[wb]] + nt.hT_b[4 * tg:4 * tg + 4], writes=[pacc_b[pi]])
            o1 = oi % 4
            o2 = (oi + 1) % 4
            oi += 2
            a = pacc[a_i][0:M, :]
            b = pacc[b_i][0:M, :]
            if u["rope"] is not None:
                cs = tabs["cos" + u["rope"]][0:M, ts]
                sn = tabs["sin" + u["rope"]][0:M, ts]
                t0, t1, t2, t3 = (tmp[i][0:M, :] for i in range(4))
                P.op("dve", lambda e: e.tensor_tensor(out=t0, in0=a, in1=cs, op=ALU.mult),
                     reads=[pacc_b[a_i], tab_b], writes=[tmp_b[0]])
                P.op("dve", lambda e: e.tensor_tensor(out=t1, in0=b, in1=sn, op=ALU.mult),
                     reads=[pacc_b[b_i], tab_b], writes=[tmp_b[1]])
                P.op("dve", lambda e: e.tensor_tensor(out=t2, in0=a, in1=sn, op=ALU.mult),
                     reads=[pacc_b[a_i], tab_b], writes=[tmp_b[2]])
                P.op("dve", lambda e: e.tensor_tensor(out=t3, in0=b, in1=cs, op=ALU.mult),
                     reads=[pacc_b[b_i], tab_b], writes=[tmp_b[3]])
                P.op("pool", lambda e: e.tensor_tensor(out=ob[o1][0:M, :], in0=t0, in1=t1, op=ALU.subtract),
                     reads=[tmp_b[0], tmp_b[1]], writes=[ob_b[o1]])
                P.op("pool", lambda e: e.tensor_tensor(out=ob[o2][0:M, :], in0=t2, in1=t3, op=ALU.add),
                     reads=[tmp_b[2], tmp_b[3]], writes=[ob_b[o2]])
            else:
                P.op("act", lambda e: e.copy(out=ob[o1][0:M, :], in_=a), reads=[pacc_b[a_i]], writes=[ob_b[o1]])
                P.op("dve", lambda e: e.tensor_copy(out=ob[o2][0:M, :], in_=b), reads=[pacc_b[b_i]],
                     writes=[ob_b[o2]])
            dst = T[u["dst"]]
            for rows, oidx in ((u["rowsA"], o1), (u["rowsB"], o2)):
                for (r0, nr, p0) in rows:
                    P.dma("sp", dst[r0:r0 + nr, ts], ob[oidx][p0:p0 + nr, :], reads=[ob_b[oidx]])
    for u in TM:
        wb = wi_ % 2
        wi_ += 1
        W = u["width"]
        src = bass.AP(tensor=wt, offset=win.offset + u["col0"], ap=[[IN_WIDTH, 128], [128 * IN_WIDTH, 16], [1, W]])
        P.dma("pool", wbuf[wb][:, :, 0:W], src, writes=[wbuf_b[wb]])
        dst = T[u["dst"]]
        for j in range(NSLOT):
            pi = pa % 4
            pa += 1
            for kt in range(16):
                P.op("pe", lambda e: e.matmul(out=pacc[pi][:, 0:W], lhsT=nt.hT[:, kt, 128 * j:128 * (j + 1)],
                                              rhs=wbuf[wb][:, kt, 0:W], start=(kt == 0), stop=(kt == 15)),
                     reads=[wbuf_b[wb], nt.hT_b[j]], writes=[pacc_b[pi]])
            if u["name"] == "w_i":
                o1 = j % 2
                P.op("dve", lambda e: e.tensor_copy(out=obf[o1][:, 0:W], in_=pacc[pi][:, 0:W]), reads=[pacc_b[pi]],
                     writes=[obf_b[o1]])
                P.dma("sp", dst[128 * j:128 * (j + 1), :], obf[o1][:, 0:W], reads=[obf_b[o1]])
            else:
                o1 = oi % 4
                oi += 1
                if j % 2 == 0:
                    P.op("act", lambda e: e.copy(out=ob[o1][:, 0:W], in_=pacc[pi][:, 0:W]), reads=[pacc_b[pi]],
                         writes=[ob_b[o1]])
                else:
                    P.op("dve", lambda e: e.tensor_copy(out=ob[o1][:, 0:W], in_=pacc[pi][:, 0:W]),
                         reads=[pacc_b[pi]], writes=[ob_b[o1]])
                P.dma("sp", dst[128 * j:128 * (j + 1), u["dcol"]:u["dcol"] + W], ob[o1][:, 0:W], reads=[ob_b[o1]])


def build_A():
    nc = bass.Bass("TRN2", target_bir_lowering=False)
    perm, FM, TM = win_units()
    T = {}

    def din(name, shape, dt=F32):
        T[name] = nc.dram_tensor(name, list(shape), dt, kind="ExternalInput").ap()

    def dout(name, shape, dt=BF16):
        T[name] = nc.dram_tensor(name, list(shape), dt, kind="ExternalOutput").ap()

    din("x", [TPC, D])
    din("g", [1, D])
    din("win", [D, IN_WIDTH])
    din("ident", [128, 128])
    for nm in ("cosA", "sinA", "cosC", "sinC"):
        din(nm, [128, TPC])
    dout("qaT", [512, TPC]); dout("kaT", [512, TPC]); dout("qiT", [512, TPC]); dout("kiT", [64, TPC])
    dout("qcT", [1024, TPC]); dout("kcT", [1024, TPC]); dout("uT", [512, TPC])
    dout("va", [TPC, 512]); dout("vc", [TPC, 1024]); dout("wi", [TPC, 8], F32)
    P = Prog(nc)
    P.begin_phase("A")
    emit_phase_A(P, T, FM, TM)
    P.end_phase()
    P.finish()
    return nc, perm


def w_src(w_ap, ld, row0, KT, col0, width):
    return bass.AP(tensor=w_ap.tensor, offset=w_ap.offset + row0 * ld + col0, ap=[[ld, 128], [128 * ld, KT], [1, width]])


class WStream:
    def __init__(self, P, tag, nbuf=2):
        self.P = P
        self.buf = [P.sb(f"{tag}_w{i}", [128, 16, 512], BF16) for i in range(nbuf)]
        self.b = [Buf(f"{tag}_w{i}") for i in range(nbuf)]
        self.i = 0
        self.n = nbuf

    def load(self, w_ap, ld, row0, KT, col0, width):
        i = self.i % self.n
        self.i += 1
        self.P.dma("pool", self.buf[i][:, 0:KT, 0:width], w_src(w_ap, ld, row0, KT, col0, width), writes=[self.b[i]])
        return self.buf[i], self.b[i]


def alloc_xr(P, tag):
    return ([P.sb(f"{tag}_xr{i}", [128, 512], F32) for i in range(3)], [Buf(f"{tag}_xr{i}") for i in range(3)])


def emit_resid_proj(P, lhsT, lhsT_b, KT_total, w_ap, x_in, x_out, ws, pacc, pacc_b, xrs, ntok=TPC):
    nslot = ntok // 128
    xr, xr_b = xrs
    xi = 0
    pa = 0
    nchunk = KT_total // 16
    for nb in range(4):
        if nchunk == 1:
            wt, wb = ws.load(w_ap, D, 0, 16, 512 * nb, 512)
            for j in range(nslot):
                pi = pa % len(pacc)
                pa += 1
                lb = lhsT_b[j] if isinstance(lhsT_b, list) else lhsT_b
                for kt in range(16):
                    P.op("pe", lambda e: e.matmul(out=pacc[pi][:], lhsT=lhsT[:, kt, 128 * j:128 * (j + 1)],
                                                  rhs=wt[:, kt, :], start=(kt == 0), stop=(kt == 15)),
                         reads=[wb, lb], writes=[pacc_b[pi]])
                k = xi % 3
                xi += 1
                P.dma("sp", xr[k][:], x_in[128 * j:128 * (j + 1), 512 * nb:512 * (nb + 1)], writes=[xr_b[k]])
                P.op("dve", lambda e: e.tensor_tensor(out=xr[k][:], in0=pacc[pi][:], in1=xr[k][:], op=ALU.add),
                     reads=[pacc_b[pi], xr_b[k]], writes=[xr_b[k]])
                P.dma("sp", x_out[128 * j:128 * (j + 1), 512 * nb:512 * (nb + 1)], xr[k][:], reads=[xr_b[k]])
        else:
            assert nslot <= len(pacc)
            for ch in range(nchunk):
                wt, wb = ws.load(w_ap, D, 2048 * ch, 16, 512 * nb, 512)
                for j in range(nslot):
                    lb = lhsT_b[j] if isinstance(lhsT_b, list) else lhsT_b
                    for kt in range(16):
                        P.op("pe", lambda e: e.matmul(out=pacc[j][:], lhsT=lhsT[:, 16 * ch + kt, 128 * j:128 * (j + 1)],
                                                      rhs=wt[:, kt, :], start=(ch == 0 and kt == 0),
                                                      stop=(ch == nchunk - 1 and kt == 15)),
                             reads=[wb, lb], writes=[pacc_b[j]])
            for j in range(nslot):
                k = xi % 3
                xi += 1
                P.dma("sp", xr[k][:], x_in[128 * j:128 * (j + 1), 512 * nb:512 * (nb + 1)], writes=[xr_b[k]])
                P.op("dve", lambda e: e.tensor_tensor(out=xr[k][:], in0=pacc[j][:], in1=xr[k][:], op=ALU.add),
                     reads=[pacc_b[j], xr_b[k]], writes=[xr_b[k]])
                P.dma("sp", x_out[128 * j:128 * (j + 1), 512 * nb:512 * (nb + 1)], xr[k][:], reads=[xr_b[k]])


def emit_phase_O(P, T):
    cat = P.sb("O_cat", [128, 16, TPC], BF16)
    cat_b = Buf("O_cat")
    for kt in range(16):
        P.dma("sp", cat[:, kt, :], T["catT"][128 * kt:128 * (kt + 1), :], writes=[cat_b])
    ws = WStream(P, "O")
    pacc = [P.ps(f"O_acc{i}", [128, 512], F32) for i in range(4)]
    pacc_b = [Buf(f"O_acc{i}") for i in range(4)]
    emit_resid_proj(P, cat, cat_b, 16, T["wout"], T["x"], T["x1"], ws, pacc, pacc_b, alloc_xr(P, "O"))


def emit_phase_X(P, T):
    C = make_consts(P, T["ident"])
    ones = P.sb("X_ones", [128, 128], BF16)
    ones_b = Buf("X_ones")
    P.op("dve", lambda e: e.memset(ones[:], 1.0), writes=[ones_b])
    nt = NormT(P, TPC, "X")
    memT = P.sb("X_memT", [128, 16, 256], BF16)
    memT_b = [Buf("X_memT0"), Buf("X_memT1")]
    nt.run(T["x1"], T["gx"], C)
    nt.run(T["mem"], T["gm"], C, hT=memT, hT_b=memT_b, ntok=256)
    hT, hT_b = nt.hT, nt.hT_b

    ws = WStream(P, "X")
    pacc = [P.ps(f"X_acc{i}", [128, 512], F32) for i in range(6)]
    pacc_b = [Buf(f"X_acc{i}") for i in range(6)]
    pa = 0
    kmT = P.sb("X_kmT", [128, 16, 256], BF16)
    kmT_b = Buf("X_kmT")
    vm = P.sb("X_vm", [128, 2, D], BF16)
    vm_b = Buf("X_vm")
    for nb4 in range(4):
        wt, wb = ws.load(T["wk"], D, 0, 16, 512 * nb4, 512)
        for r in range(4):
            pi = pa % 6
            pa += 1
            for kt in range(16):
                P.op("pe", lambda e: e.matmul(out=pacc[pi][:, 0:256], lhsT=wt[:, kt, 128 * r:128 * (r + 1)],
                                              rhs=memT[:, kt, :], start=(kt == 0), stop=(kt == 15)),
                     reads=[wb] + memT_b, writes=[pacc_b[pi]])
            P.op("act", lambda e: e.copy(out=kmT[:, 4 * nb4 + r, :], in_=pacc[pi][:, 0:256]), reads=[pacc_b[pi]],
                 writes=[kmT_b])
    for nb4 in range(4):
        wt, wb = ws.load(T["wv"], D, 0, 16, 512 * nb4, 512)
        for mt in range(2):
            pi = pa % 6
            pa += 1
            for kt in range(16):
                P.op("pe", lambda e: e.matmul(out=pacc[pi][:], lhsT=memT[:, kt, 128 * mt:128 * (mt + 1)],
                                              rhs=wt[:, kt, :], start=(kt == 0), stop=(kt == 15)),
                     reads=[wb, memT_b[mt]], writes=[pacc_b[pi]])
            P.op("dve", lambda e: e.tensor_copy(out=vm[:, mt, 512 * nb4:512 * (nb4 + 1)], in_=pacc[pi][:]),
                 reads=[pacc_b[pi]], writes=[vm_b])
    oT = hT
    qT = [P.sb(f"X_qT{i}", [128, 16, 512], BF16) for i in range(1)]
    qT_b = [Buf(f"X_qT{i}") for i in range(1)]
    pT = [P.sb(f"X_pT{i}", [128, 2, 512], BF16) for i in range(2)]
    pT_b = [Buf(f"X_pT{i}") for i in range(2)]
    rs = [P.sb(f"X_rs{i}", [128, 512], F32) for i in range(2)]
    rs_b = [Buf(f"X_rs{i}") for i in range(2)]
    sc = 1.0 / math.sqrt(512.0)
    for tg in range(4):
        ts = slice(512 * tg, 512 * (tg + 1))
        for nb4 in range(4):
            wt, wb = ws.load(T["wq"], D, 0, 16, 512 * nb4, 512)
            for r in range(4):
                pi = pa % 6
                pa += 1
                for kt in range(16):
                    P.op("pe", lambda e: e.matmul(out=pacc[pi][:], lhsT=wt[:, kt, 128 * r:128 * (r + 1)],
                                                  rhs=hT[:, kt, ts], start=(kt == 0), stop=(kt == 15)),
                         reads=[wb] + hT_b[4 * tg:4 * tg + 4], writes=[pacc_b[pi]])
                if r % 2 == 0:
                    P.op("act", lambda e: e.copy(out=qT[0][:, 4 * nb4 + r, :], in_=pacc[pi][:]), reads=[pacc_b[pi]],
                         writes=[qT_b[0]])
                else:
                    P.op("dve", lambda e: e.tensor_copy(out=qT[0][:, 4 * nb4 + r, :], in_=pacc[pi][:]),
                         reads=[pacc_b[pi]], writes=[qT_b[0]])
        for hx in range(4):
            pb = hx % 2
            for mt in range(2):
                pi = pa % 6
                pa += 1
                for dt_ in range(4):
                    P.op("pe", lambda e: e.matmul(out=pacc[pi][:], lhsT=kmT[:, 4 * hx + dt_, 128 * mt:128 * (mt + 1)],
                                                  rhs=qT[0][:, 4 * hx + dt_, :], start=(dt_ == 0), stop=(dt_ == 3)),
                         reads=[kmT_b, qT_b[0]], writes=[pacc_b[pi]])
                P.op("act", lambda e: e.activation(out=pT[pb][:, mt, :], in_=pacc[pi][:], func=AF.Exp, scale=sc),
                     reads=[pacc_b[pi]], writes=[pT_b[pb]])
            pi = pa % 6
            pa += 1
            for mt in range(2):
                P.op("pe", lambda e: e.matmul(out=pacc[pi][:], lhsT=ones[:], rhs=pT[pb][:, mt, :], start=(mt == 0),
                                              stop=(mt == 1)), reads=[ones_b, pT_b[pb]], writes=[pacc_b[pi]])
            P.op("dve", lambda e: e.reciprocal(out=rs[pb][:], in_=pacc[pi][:]), reads=[pacc_b[pi]], writes=[rs_b[pb]])
            for dvt in range(4):
                pi = pa % 6
                pa += 1
                for mt in range(2):
                    c0 = 512 * hx + 128 * dvt
                    P.op("pe", lambda e: e.matmul(out=pacc[pi][:], lhsT=vm[:, mt, c0:c0 + 128], rhs=pT[pb][:, mt, :],
                                                  start=(mt == 0), stop=(mt == 1)),
                         reads=[vm_b, pT_b[pb]], writes=[pacc_b[pi]])
                P.op("dve", lambda e: e.tensor_tensor(out=oT[:, 4 * hx + dvt, ts], in0=pacc[pi][:], in1=rs[pb][:],
                                                      op=ALU.mult), reads=[pacc_b[pi], rs_b[pb]],
                     writes=hT_b[4 * tg:4 * tg + 4])
    emit_resid_proj(P, oT, hT_b, 16, T["wo"], T["x1"], T["x2"], ws, pacc[0:4], pacc_b[0:4], alloc_xr(P, "X"))


def emit_phase_M(P, T):
    C = make_consts(P, T["ident"])
    nt = NormT(P, TPC, "M")
    nt.run(T["x2"], T["gmlp"], C)
    hT, hT_b = nt.hT, nt.hT_b
    ws = WStream(P, "M")
    pacc = [P.ps(f"M_acc{i}", [128, 512], F32) for i in range(6)]
    pacc_b = [Buf(f"M_acc{i}") for i in range(6)]
    hid = P.sb("M_hid", [128, 64, 512], BF16)
    hid_b = Buf("M_hid")
    rl = [P.sb(f"M_rl{i}", [128, 512], F32) for i in range(3)]
    rl_b = [Buf(f"M_rl{i}") for i in range(3)]
    pa = 0
    ri = 0
    xrs = alloc_xr(P, "M")
    for tg in range(4):
        ts = slice(512 * tg, 512 * (tg + 1))
        for fb4 in range(16):
            wt, wb = ws.load(T["w1"], 8192, 0, 16, 512 * fb4, 512)
            for r in range(4):
                pi = 4 + (pa % 2)
                pa += 1
                for kt in range(16):
                    P.op("pe", lambda e: e.matmul(out=pacc[pi][:], lhsT=wt[:, kt, 128 * r:128 * (r + 1)],
                                                  rhs=hT[:, kt, ts], start=(kt == 0), stop=(kt == 15)),
                         reads=[wb] + hT_b[4 * tg:4 * tg + 4], writes=[pacc_b[pi]])
                k = ri % 3
                ri += 1
                P.op("act", lambda e: e.activation(out=rl[k][:], in_=pacc[pi][:], func=AF.Relu), reads=[pacc_b[pi]],
                     writes=[rl_b[k]])
                P.op("pool", lambda e: e.tensor_tensor(out=hid[:, 4 * fb4 + r, :], in0=rl[k][:], in1=rl[k][:],
                                                       op=ALU.mult), reads=[rl_b[k]], writes=[hid_b])
        emit_resid_proj(P, hid, hid_b, 64, T["w2"], T["x2"][512 * tg:512 * (tg + 1), :],
                        T["x3"][512 * tg:512 * (tg + 1), :], ws, pacc[0:4], pacc_b[0:4], xrs, ntok=512)


def emit_phase_F(P, T):
    xs = [P.sb(f"F_xs{i}", [128, D], F32) for i in range(2)]
    xs_b = [Buf(f"F_xs{i}") for i in range(2)]
    ys = [P.sb(f"F_ys{i}", [128, D], F32) for i in range(2)]
    ys_b = [Buf(f"F_ys{i}") for i in range(2)]
    junk = P.sb("F_junk", [128, D], BF16)
    junk_b = Buf("F_junk")
    gb = P.sb("F_gb", [128, D], F32)
    gb_b = Buf("F_gb")
    st = [P.sb(f"F_st{i}", [128, 4], F32) for i in range(2)]
    st_b = [Buf(f"F_st{i}") for i in range(2)]
    epsb = P.sb("F_eps", [128, 1], F32)
    epsb_b = Buf("F_eps")
    P.op("dve", lambda e: e.memset(epsb[:], EPS), writes=[epsb_b])
    g = T["gf"]
    P.dma("sp", gb[:], bass.AP(tensor=g.tensor, offset=g.offset, ap=[[0, 128], [1, D]]), writes=[gb_b])
    for j in range(NSLOT):
        i = j % 2
        P.dma("sp", xs[i][:], T["x3"][128 * j:128 * (j + 1), :], writes=[xs_b[i]])
        P.op("act", lambda e: e.activation(out=junk[:], in_=xs[i][:], func=AF.Square, accum_out=st[i][:, 0:1]),
             reads=[xs_b[i]], writes=[junk_b, st_b[i]])
        P.op("act", lambda e: e.activation(out=st[i][:, 1:2], in_=st[i][:, 0:1], func=AF.Sqrt, scale=1.0 / D,
                                           bias=epsb[:, 0:1]), reads=[st_b[i], epsb_b], writes=[st_b[i]])
        P.op("dve", lambda e: e.reciprocal(out=st[i][:, 2:3], in_=st[i][:, 1:2]), reads=[st_b[i]], writes=[st_b[i]])
        P.op("dve", lambda e: e.scalar_tensor_tensor(out=ys[i][:], in0=xs[i][:], scalar=st[i][:, 2:3], in1=gb[:],
                                                     op0=ALU.mult, op1=ALU.mult),
             reads=[xs_b[i], st_b[i], gb_b], writes=[ys_b[i]])
        P.dma("sp", T["y"][128 * j:128 * (j + 1), :], ys[i][:], reads=[ys_b[i]])


def build_OXM(final=False):
    nc = bass.Bass("TRN2", target_bir_lowering=False)
    T = {}

    def din(name, shape, dt=F32):
        T[name] = nc.dram_tensor(name, list(shape), dt, kind="ExternalInput").ap()

    def dout(name, shape, dt=F32):
        T[name] = nc.dram_tensor(name, list(shape), dt, kind="ExternalOutput").ap()

    def dint(name, shape, dt=F32):
        T[name] = nc.dram_tensor(name, list(shape), dt, kind="Internal").ap()

    din("x", [TPC, D]); din("catT", [D, TPC], BF16); din("wout", [D, D]); din("ident", [128, 128])
    din("mem", [256, D]); din("gx", [1, D]); din("gm", [1, D])
    for w in ("wq", "wk", "wv", "wo"):
        din(w, [D, D])
    din("gmlp", [1, D]); din("w1", [D, 8192]); din("w2", [8192, D])
    dint("x1", [TPC, D]); dint("x2", [TPC, D])
    if final:
        dint("x3", [TPC, D]); din("gf", [1, D]); dout("y", [TPC, D])
    else:
        dout("x3", [TPC, D])
    P = Prog(nc)
    for nm, fn in (("O", emit_phase_O), ("X", emit_phase_X), ("M", emit_phase_M)) + ((("F", emit_phase_F),) if final else ()):
        P.begin_phase(nm)
        fn(P, T)
        P.end_phase()
    P.finish()
    return nc


MASK_NEG = -30000.0


def mask_rows_q(c):
    qm = np.zeros((64, TPC), np.float32)
    for j in range(NSLOT):
        g = j // 4
        for half in range(2):
            qc = 2 * blk_of(c, j) + half - 64 * g
            cols = slice(128 * j + 64 * half, 128 * j + 64 * half + 64)
            qm[qc + 1:, cols] = MASK_NEG
    return qm


def mask_rows_k():
    km = np.zeros((NCORES, 64, TPC), np.float32)
    for r in range(NCORES):
        for j in range(NSLOT):
            g = j // 4
            for half in range(2):
                kc = 2 * blk_of(r, j) + half - 64 * g
                km[r, kc, 128 * j + 64 * half:128 * j + 64 * half + 64] = 1.0
    return km.reshape(NCORES * 64, TPC)


def emit_phase_C(P, T, groups=range(4), heads=range(8)):
    sc = 1.0 / 8.0
    ones = P.sb("C_ones", [128, 128], BF16)
    ones_b = Buf("C_ones")
    P.op("dve", lambda e: e.memset(ones[:], 1.0), writes=[ones_b])
    epsb = P.sb("C_eps", [128, 1], F32)
    epsb_b = Buf("C_eps")
    P.op("dve", lambda e: e.memset(epsb[:], EPS), writes=[epsb_b])
    lv = P.sb("C_lv", [128, 4, 64], F32)
    lv_b = Buf("C_lv")
    lamv = T["lamv"]
    P.dma("sp", lv[:], bass.AP(tensor=lamv.tensor, offset=lamv.offset, ap=[[0, 128], [64, 4], [1, 64]]), writes=[lv_b])
    lc = P.sb("C_lc", [128, 2], F32)
    lc_b = Buf("C_lc")
    lamc = T["lamc"]
    P.dma("sp", lc[:], bass.AP(tensor=lamc.tensor, offset=lamc.offset, ap=[[0, 128], [1, 2]]), writes=[lc_b])
    sg = P.sb("C_sg", [128, 1], F32)
    sg_b = Buf("C_sg")
    P.dma("sp", sg[:], T["subln"], writes=[sg_b])
    lw = P.sb("C_lw", [128, 8], F32)
    lw_b = Buf("C_lw")
    lj = P.sb("C_lj", [128, 64], F32)
    lj_b = Buf("C_lj")
    for i in range(2):
        P.op("dve", lambda e: e.tensor_tensor(out=lj[:], in0=lv[:, 2 * i, :], in1=lv[:, 2 * i + 1, :], op=ALU.mult),
             reads=[lv_b], writes=[lj_b])
        P.op("dve", lambda e: e.tensor_reduce(out=lw[:, i:i + 1], in_=lj[:], axis=AX.X, op=ALU.add),
             reads=[lj_b], writes=[lw_b])
    P.op("act", lambda e: e.activation(out=lw[:, 2:4], in_=lw[:, 0:2], func=AF.Exp), reads=[lw_b], writes=[lw_b])
    P.op("dve", lambda e: e.tensor_tensor(out=lw[:, 4:5], in0=lw[:, 2:3], in1=lw[:, 3:4], op=ALU.subtract),
         reads=[lw_b], writes=[lw_b])
    P.op("dve", lambda e: e.tensor_tensor(out=lw[:, 5:6], in0=lw[:, 4:5], in1=lc[:, 0:1], op=ALU.add),
         reads=[lw_b, lc_b], writes=[lw_b])
    P.op("dve", lambda e: e.tensor_scalar(out=lw[:, 6:7], in0=lw[:, 5:6], scalar1=-1.0, scalar2=None, op0=ALU.mult),
         reads=[lw_b], writes=[lw_b])
    P.op("dve", lambda e: e.tensor_tensor(out=lw[:, 7:8], in0=sg[:], in1=lc[:, 1:2], op=ALU.mult),
         reads=[sg_b, lc_b], writes=[lw_b])
    neglam = lw[:, 6:7]
    gsc = lw[:, 7:8]

    QT = [P.sb(f"C_QT{i}", [128, 512], BF16) for i in range(2)]
    QT_b = [Buf(f"C_QT{i}") for i in range(2)]
    QB = [[P.sb(f"C_QB{c}{i}", [128, 512], BF16) for i in range(2)] for c in range(2)]
    QB_b = [[Buf(f"C_QB{c}{i}") for i in range(2)] for c in range(2)]
    KT = [P.sb(f"C_KT{i}", [128, 8, 1536], BF16) for i in range(2)]
    KT_b = [Buf(f"C_KT{i}") for i in range(2)]
    KB = [[P.sb(f"C_KB{c}{i}", [128, 8, 512], BF16) for i in range(2)] for c in range(2)]
    KB_b = [[Buf(f"C_KB{c}{i}") for i in range(2)] for c in range(2)]
    VT = [P.sb(f"C_VT{i}", [128, 8, 16, 128], BF16) for i in range(2)]
    VT_b = [Buf(f"C_VT{i}") for i in range(2)]
    NPT = 4
    PT = [P.sb(f"C_PT{i}", [128, 512], BF16) for i in range(NPT)]
    PT_b = [Buf(f"C_PT{i}") for i in range(NPT)]
    sps = [P.ps(f"C_S{i}", [128, 512], F32) for i in range(3)]
    sps_b = [Buf(f"C_S{i}") for i in range(3)]
    oacc = [P.ps(f"C_O{i}", [128, 512], F32) for i in range(2)]
    oacc_b = [Buf(f"C_O{i}") for i in range(2)]
    sacc = [P.ps(f"C_Z{i}", [128, 512], F32) for i in range(2)]
    sacc_b = [Buf(f"C_Z{i}") for i in range(2)]
    lnp = P.ps("C_ln", [128, 512], F32)
    lnp_b = Buf("C_ln")
    rs = P.sb("C_rs", [128, 512], F32)
    rs_b = Buf("C_rs")
    on0 = P.sb("C_on0", [128, 512], F32)
    on0_b = Buf("C_on0")
    od = P.sb("C_od", [128, 512], F32)
    od_b = Buf("C_od")
    sq = P.sb("C_sq", [128, 512], BF16)
    sq_b = Buf("C_sq")
    fin = [P.sb(f"C_fin{i}", [128, 512], BF16) for i in range(2)]
    fin_b = [Buf(f"C_fin{i}") for i in range(2)]

    qcT, kcT, vc, qm, km = T["qcT"], T["kcT_g"], T["vc_g"], T["qm"], T["km_g"]
    items = [(g, h) for g in groups for h in heads]

    def load(idx):
        g, h = items[idx]
        b = idx % 2
        qs = slice(512 * g, 512 * (g + 1))
        first_of_g = (idx == 0) or (items[idx - 1][0] != g)
        second_of_g = (idx >= 1 and items[idx - 1][0] == g) and (idx == 1 or items[idx - 2][0] != g)
        P.dma("sp", QT[b][:], qcT[128 * h:128 * (h + 1), qs], writes=[QT_b[b]])
        for c in range(2):
            P.dma("sp", QB[c][b][0:64, :], qcT[128 * h + 64 * c:128 * h + 64 * c + 64, qs], writes=[QB_b[c][b]])
            src = bass.AP(tensor=kcT.tensor, offset=kcT.offset + (128 * h + 64 * c) * TPC + 512 * g,
                          ap=[[TPC, 64], [1024 * TPC, 8], [1, 512]])
            P.dma("sp", KB[c][b][0:64, :, :], src, writes=[KB_b[c][b]])
            if first_of_g or second_of_g:
                P.dma("sp", QB[c][b][64:128, :], qm[:, qs], writes=[QB_b[c][b]])
                srcm = bass.AP(tensor=km.tensor, offset=km.offset + 512 * g, ap=[[TPC, 64], [64 * TPC, 8], [1, 512]])
                P.dma("sp", KB[c][b][64:128, :, :], srcm, writes=[KB_b[c][b]])
        if g > 0:
            for r in range(8):
                P.dma("sp", KT[b][:, r, 0:512 * g], kcT[r * 1024 + 128 * h:r * 1024 + 128 * (h + 1), 0:512 * g],
                      writes=[KT_b[b]])
        nj = 4 * g + 4
        for r in range(8):
            src = bass.AP(tensor=vc.tensor, offset=vc.offset + r * TPC * 1024 + 128 * h,
                          ap=[[1024, 128], [128 * 1024, nj], [1, 128]])
            P.dma("sp", VT[b][:, r, 0:nj, :], src, writes=[VT_b[b]])

    state = dict(si=0, pi=0, fi=0)

    def compute(idx):
        g, h = items[idx]
        b = idx % 2
        tiles = []
        for c in range(2):
            for r in range(8):
                for j in range(4 * g):
                    tiles.append((c, r, j, False))
                for jb in range(4):
                    tiles.append((c, r, 4 * g + jb, True))
        nt_c = len(tiles) // 2

        def s_mm(t):
            c, r, j, bnd = tiles[t]
            s_i = (state["si"] + t) % 3
            if bnd:
                jb = j - 4 * g
                P.op("pe", lambda e: e.matmul(out=sps[s_i][:], lhsT=KB[c][b][:, r, 128 * jb:128 * (jb + 1)],
                                              rhs=QB[c][b][:, :], start=True, stop=True),
                     reads=[KB_b[c][b], QB_b[c][b]], writes=[sps_b[s_i]])
            else:
                P.op("pe", lambda e: e.matmul(out=sps[s_i][:], lhsT=KT[b][64 * c:64 * c + 64, r, 128 * j:128 * (j + 1)],
                                              rhs=QT[b][64 * c:64 * c + 64, :], start=True, stop=True),
                     reads=[KT_b[b], QT_b[b]], writes=[sps_b[s_i]])

        def epilogue(c):
            P.op("dve", lambda e: e.reciprocal(out=rs[:], in_=sacc[c][:]), reads=[sacc_b[c]], writes=[rs_b])
            if c == 0:
                P.op("dve", lambda e: e.tensor_tensor(out=on0[:], in0=oacc[0][:], in1=rs[:], op=ALU.mult),
                     reads=[oacc_b[0], rs_b], writes=[on0_b])
                return
            P.op("dve", lambda e: e.tensor_tensor(out=od[:], in0=oacc[1][:], in1=rs[:], op=ALU.mult),
                 reads=[oacc_b[1], rs_b], writes=[od_b])
            P.op("dve", lambda e: e.scalar_tensor_tensor(out=od[:], in0=od[:], scalar=neglam, in1=on0[:],
                                                         op0=ALU.mult, op1=ALU.add),
                 reads=[od_b, on0_b, lw_b], writes=[od_b])
            P.op("act", lambda e: e.activation(out=sq[:], in_=od[:], func=AF.Square), reads=[od_b], writes=[sq_b])
            P.op("pe", lambda e: e.matmul(out=lnp[:], lhsT=ones[:], rhs=sq[:], start=True, stop=True),
                 reads=[ones_b, sq_b], writes=[lnp_b])
            P.op("act", lambda e: e.activation(out=rs[:], in_=lnp[:], func=AF.Sqrt, scale=1.0 / 128.0,
                                               bias=epsb[:, 0:1]), reads=[lnp_b, epsb_b], writes=[rs_b])
            P.op("dve", lambda e: e.reciprocal(out=rs[:], in_=rs[:]), reads=[rs_b], writes=[rs_b])
            f = state["fi"] % 2
            state["fi"] += 1
            P.op("dve", lambda e: e.scalar_tensor_tensor(out=fin[f][:], in0=od[:], scalar=gsc, in1=rs[:],
                                                         op0=ALU.mult, op1=ALU.mult),
                 reads=[od_b, rs_b, lw_b], writes=[fin_b[f]])
            P.dma("sp", T["catT"][1024 + 128 * h:1024 + 128 * (h + 1), 512 * g:512 * (g + 1)], fin[f][:],
                  reads=[fin_b[f]])

        s_mm(0)
        for t in range(len(tiles)):
            c, r, j, bnd = tiles[t]
            if t + 1 < len(tiles):
                s_mm(t + 1)
            s_i = (state["si"] + t) % 3
            p_i = (state["pi"] + t) % NPT
            P.op("act", lambda e: e.activation(out=PT[p_i][:], in_=sps[s_i][:], func=AF.Exp, scale=sc),
                 reads=[sps_b[s_i]], writes=[PT_b[p_i]])
            tc = t % nt_c
            P.op("pe", lambda e: e.matmul(out=oacc[c][:], lhsT=VT[b][:, r, j, :], rhs=PT[p_i][:], start=(tc == 0),
                                          stop=(tc == nt_c - 1)),
                 reads=[VT_b[b], PT_b[p_i]], writes=[oacc_b[c]])
            P.op("pe", lambda e: e.matmul(out=sacc[c][:], lhsT=ones[:], rhs=PT[p_i][:], start=(tc == 0),
                                          stop=(tc == nt_c - 1)),
                 reads=[ones_b, PT_b[p_i]], writes=[sacc_b[c]])
            if tc == nt_c - 1:
                epilogue(c)
        state["si"] += len(tiles)
        state["pi"] += len(tiles)

    load(0)
    for idx in range(len(items)):
        if idx + 1 < len(items):
            load(idx + 1)
        compute(idx)


def build_C(groups=range(4), heads=range(8)):
    nc = bass.Bass("TRN2", target_bir_lowering=False)
    T = {}

    def din(name, shape, dt=F32):
        T[name] = nc.dram_tensor(name, list(shape), dt, kind="ExternalInput").ap()

    def dout(name, shape, dt=F32):
        T[name] = nc.dram_tensor(name, list(shape), dt, kind="ExternalOutput").ap()

    din("qcT", [1024, TPC], BF16); din("kcT_g", [8 * 1024, TPC], BF16); din("vc_g", [8 * TPC, 1024], BF16)
    din("qm", [64, TPC], BF16); din("km_g", [8 * 64, TPC], BF16)
    din("lamv", [4, 64]); din("lamc", [1, 2]); din("subln", [128, 1])
    dout("catT", [D, TPC], BF16)
    P = Prog(nc)
    P.begin_phase("C")
    emit_phase_C(P, T, groups, heads)
    P.end_phase()
    P.finish()
    return nc


def s5_host_layout(a_re, a_im, log_dt, b_re, b_im, c_re, c_im, dsk, w_glu, b_glu):
    def st(a):
        return np.ascontiguousarray(a.reshape(16, 2, 64).transpose(1, 2, 0).reshape(128, 16)).astype(np.float32)
    out = dict(s5_are=st(a_re), s5_aim=st(a_im), s5_ldt=st(np.repeat(log_dt[:, None], 64, axis=1)))
    def bl(b):
        return np.ascontiguousarray(b.reshape(16, 2, 64, 16).transpose(1, 2, 0, 3).reshape(128, 16, 16)).astype(np.float32)
    out["s5_bre"] = bl(b_re)
    out["s5_bim"] = bl(b_im)
    out["s5_cre"] = bl(c_re.transpose(0, 2, 1))
    out["s5_cim"] = bl(c_im.transpose(0, 2, 1))
    out["s5_d"] = np.ascontiguousarray(dsk.reshape(4, 128).T).astype(np.float32)
    out["s5_wglu"] = np.ascontiguousarray(w_glu).astype(np.float32)
    out["s5_bglu"] = np.ascontiguousarray(b_glu.reshape(4, 128).T).astype(np.float32)
    return out


S5_IN = dict(s5_are=[128, 16], s5_aim=[128, 16], s5_ldt=[128, 16], s5_bre=[128, 16, 16], s5_bim=[128, 16, 16],
             s5_cre=[128, 16, 16], s5_cim=[128, 16, 16], s5_d=[128, 4], s5_wglu=[512, 512], s5_bglu=[128, 4])


def cplx_mul(P, eng, out_r, out_i, a_r, a_i, b_r, b_i, tmp, reads, writes, tmp_b):
    t0, t1 = tmp
    P.op(eng, lambda e: e.tensor_tensor(out=t0, in0=a_r, in1=b_r, op=ALU.mult), reads=reads, writes=[tmp_b])
    P.op(eng, lambda e: e.tensor_tensor(out=t1, in0=a_i, in1=b_i, op=ALU.mult), reads=reads, writes=[tmp_b])
    P.op(eng, lambda e: e.tensor_tensor(out=out_r, in0=t0, in1=t1, op=ALU.subtract), reads=[tmp_b], writes=writes)
    P.op(eng, lambda e: e.tensor_tensor(out=t0, in0=a_r, in1=b_i, op=ALU.mult), reads=reads + writes, writes=[tmp_b])
    P.op(eng, lambda e: e.tensor_tensor(out=t1, in0=a_i, in1=b_r, op=ALU.mult), reads=reads + writes, writes=[tmp_b])
    P.op(eng, lambda e: e.tensor_tensor(out=out_i, in0=t0, in1=t1, op=ALU.add), reads=[tmp_b], writes=writes)


def s5_prep(P, T, C, need_rev, need_fwd):
    R = {}
    prm = Buf("s5prm")
    R["prm"] = prm

    def ld(name, shape):
        t = P.sb("S_" + name, shape, F32)
        P.dma("sp", t[:], T[name], writes=[prm])
        return t
    are, aim, ldt = ld("s5_are", [128, 16]), ld("s5_aim", [128, 16]), ld("s5_ldt", [128, 16])
    bre, bim = ld("s5_bre", [128, 16, 16]), ld("s5_bim", [128, 16, 16])
    w = P.sb("S_w", [128, 24, 16], F32)

    def W(i):
        return w[:, i, :]

    def tt(o, a, b, op):
        P.op("dve", lambda e: e.tensor_tensor(out=o, in0=a, in1=b, op=op), reads=[prm], writes=[prm])

    def ts(o, a, s1, s2, op0, op1=None):
        if op1 is None:
            P.op("dve", lambda e: e.tensor_scalar(out=o, in0=a, scalar1=s1, scalar2=None, op0=op0), reads=[prm], writes=[prm])
        else:
            P.op("dve", lambda e: e.tensor_scalar(out=o, in0=a, scalar1=s1, scalar2=s2, op0=op0, op1=op1), reads=[prm],
                 writes=[prm])

    def act(o, a, f, scale=1.0, bias=None):
        if bias is None:
            P.op("act", lambda e: e.activation(out=o, in_=a, func=f, scale=scale), reads=[prm], writes=[prm])
        else:
            P.op("act", lambda e: e.activation(out=o, in_=a, func=f, scale=scale, bias=bias), reads=[prm], writes=[prm])

    halfpi = P.sb("S_hpi", [128, 1], F32)
    P.op("dve", lambda e: e.memset(halfpi[:], math.pi / 2), writes=[prm])
    dt, ar, ai, mag, cs, sn = W(0), W(1), W(2), W(3), W(4), W(5)
    act(dt, ldt[:], AF.Exp)
    tt(ar, are[:], dt, ALU.mult)
    tt(ai, aim[:], dt, ALU.mult)
    act(mag, ar, AF.Exp)
    act(sn, ai, AF.Sin, scale=1.0 / 16.0)
    act(cs, ai, AF.Sin, scale=1.0 / 16.0, bias=halfpi[:, 0:1])
    for _ in range(4):
        tt(W(6), cs, cs, ALU.mult)
        tt(W(7), sn, sn, ALU.mult)
        tt(W(8), cs, sn, ALU.mult)
        tt(cs, W(6), W(7), ALU.subtract)
        ts(sn, W(8), 2.0, None, ALU.mult)
    lbr, lbi = W(9), W(10)
    tt(lbr, mag, cs, ALU.mult)
    tt(lbi, mag, sn, ALU.mult)
    den, fr, fi, lm1 = W(11), W(12), W(13), W(14)
    tt(W(6), are[:], are[:], ALU.mult)
    tt(W(7), aim[:], aim[:], ALU.mult)
    tt(den, W(6), W(7), ALU.add)
    P.op("dve", lambda e: e.reciprocal(out=den, in_=den), reads=[prm], writes=[prm])
    ts(lm1, lbr, -1.0, None, ALU.add)
    tt(W(6), lm1, are[:], ALU.mult)
    tt(W(7), lbi, aim[:], ALU.mult)
    tt(W(6), W(6), W(7), ALU.add)
    tt(fr, W(6), den, ALU.mult)
    tt(W(6), lbi, are[:], ALU.mult)
    tt(W(7), lm1, aim[:], ALU.mult)
    tt(W(6), W(6), W(7), ALU.subtract)
    tt(fi, W(6), den, ALU.mult)
    bbr = P.sb("S_bbr", [128, 16, 16], F32)
    bbi = P.sb("S_bbi", [128, 16, 16], F32)
    t2 = P.sb("S_t2", [128, 2, 16, 16], F32)

    def bc(a):
        return bass.AP(tensor=a.tensor, offset=a.offset, ap=[list(a.ap[0]), list(a.ap[1]), [0, 16]])
    tt(t2[:, 0], bre[:], bc(fr), ALU.mult)
    tt(t2[:, 1], bim[:], bc(fi), ALU.mult)
    tt(bbr[:], t2[:, 0], t2[:, 1], ALU.subtract)
    tt(t2[:, 0], bim[:], bc(fr), ALU.mult)
    tt(t2[:, 1], bre[:], bc(fi), ALU.mult)
    tt(bbi[:], t2[:, 0], t2[:, 1], ALU.add)

    def zpad(name, src, dtype, neg=False):
        z = P.sb("S_" + name, [128, 16, 128], dtype)
        P.op("dve", lambda e: e.memset(z[:], 0.0), reads=[prm], writes=[prm])
        for two in range(2):
            zt = z[64 * two:64 * two + 64, :, :]
            dst = bass.AP(tensor=zt.tensor, offset=zt.offset + 16 * two, ap=[list(zt.ap[0]), [512, 4], [160, 4], [1, 16]])
            st_ = src[64 * two:64 * two + 64, :, :]
            s4 = bass.AP(tensor=st_.tensor, offset=st_.offset, ap=[list(st_.ap[0]), [64, 4], [16, 4], [1, 16]])
            if neg:
                P.op("dve", lambda e: e.tensor_scalar(out=dst, in0=s4, scalar1=-1.0, scalar2=None, op0=ALU.mult),
                     reads=[prm], writes=[prm])
            else:
                P.op("dve", lambda e: e.tensor_copy(out=dst, in_=s4), reads=[prm], writes=[prm])
        return z
    zbr = zpad("zbr", bbr, F32)
    zbi = zpad("zbi", bbi, F32)
    BTr = P.sb("S_BTr", [128, 16, 128], BF16)
    BTi = P.sb("S_BTi", [128, 16, 128], BF16)
    with P.sub_scope():
        tps = [P.ps(f"S_tp{i}", [128, 512], F32) for i in range(2)]
        tps_b = [Buf(f"S_tp{i}") for i in range(2)]
        k = 0
        for (z, BT) in ((zbr, BTr), (zbi, BTi)):
            for q in range(4):
                tb = k % 2
                k += 1
                for r in range(4):
                    gp = 4 * q + r
                    P.op("pe", lambda e: e.transpose(out=tps[tb][:, 128 * r:128 * (r + 1)], in_=z[:, gp, :],
                                                     identity=C["ident_f"][:]),
                         reads=[prm, C["ident_f_b"]], writes=[tps_b[tb]])
                P.op("act", lambda e: e.copy(out=BT[:, 4 * q:4 * q + 4, :],
                                             in_=tps[tb][:].rearrange("p (r t) -> p r t", r=4)),
                     reads=[tps_b[tb]], writes=[prm])
    R["BTr"], R["BTi"] = BTr, BTi

    ct = P.sb("S_ct", [128, 2, 16, 64], F32)
    lm = P.sb("S_lm", [128, 4, 16], F32)

    def bcj(a, m):
        return bass.AP(tensor=a.tensor, offset=a.offset, ap=[list(a.ap[0]), list(a.ap[1]), [0, m]])

    def table(name, l_r, l_i, reverse):
        tr = P.sb(f"S_{name}r", [128, 16, 128], F32)
        ti = P.sb(f"S_{name}i", [128, 16, 128], F32)
        one = 127 if reverse else 0
        P.op("dve", lambda e: e.memset(tr[:, :, one:one + 1], 1.0), reads=[prm], writes=[prm])
        P.op("dve", lambda e: e.memset(ti[:, :, one:one + 1], 0.0), reads=[prm], writes=[prm])
        P.op("dve", lambda e: e.tensor_copy(out=lm[:, 0, :], in_=l_r), reads=[prm], writes=[prm])
        P.op("dve", lambda e: e.tensor_copy(out=lm[:, 1, :], in_=l_i), reads=[prm], writes=[prm])
        m = 1
        while m < 128:
            if reverse:
                src = slice(128 - m, 128)
                dst = slice(128 - 2 * m, 128 - m)
            else:
                src = slice(0, m)
                dst = slice(m, 2 * m)
            cplx_mul(P, "dve", tr[:, :, dst], ti[:, :, dst], tr[:, :, src], ti[:, :, src], bcj(lm[:, 0, :], m),
                     bcj(lm[:, 1, :], m), (ct[:, 0, :, 0:m], ct[:, 1, :, 0:m]), [prm], [prm], prm)
            tt(lm[:, 2, :], lm[:, 0, :], lm[:, 0, :], ALU.mult)
            tt(lm[:, 3, :], lm[:, 1, :], lm[:, 1, :], ALU.mult)
            tt(lm[:, 1, :], lm[:, 0, :], lm[:, 1, :], ALU.mult)
            ts(lm[:, 1, :], lm[:, 1, :], 2.0, None, ALU.mult)
            tt(lm[:, 0, :], lm[:, 2, :], lm[:, 3, :], ALU.subtract)
            m *= 2
        return tr, ti

    if need_rev:
        R["Qr"], R["Qi"] = table("Q", lbr, lbi, True)
    if need_fwd:
        R["Pr"], R["Pi"] = table("P", lbr, lbi, False)
        l128 = P.sb("S_l128", [128, 2, 16], F32)
        P.op("dve", lambda e: e.tensor_copy(out=l128[:], in_=lm[:, 0:2, :]), reads=[prm], writes=[prm])
        R["l128"] = l128
        lam1 = P.sb("S_lam1", [128, 2, 16], F32)
        P.op("dve", lambda e: e.tensor_copy(out=lam1[:, 0, :], in_=lbr), reads=[prm], writes=[prm])
        P.op("dve", lambda e: e.tensor_copy(out=lam1[:, 1, :], in_=lbi), reads=[prm], writes=[prm])
        R["lam1"] = lam1
        rm2 = W(15)
        act(rm2, ar, AF.Exp, scale=-2.0)
        tt(W(16), lbr, rm2, ALU.mult)
        tt(W(17), lbi, rm2, ALU.mult)
        ts(W(17), W(17), -1.0, None, ALU.mult)
        R["Ir"], R["Ii"] = table("I", W(16), W(17), False)
        cre, cim = ld("s5_cre", [128, 16, 16]), ld("s5_cim", [128, 16, 16])
        R["ZCr"] = zpad("zcr", cre, BF16)
        R["ZCi"] = zpad("zci", cim, BF16, neg=True)
        dl = ld("s5_d", [128, 4])
        Dd = P.sb("S_Dd", [128, 4, 128], BF16)
        for c4 in range(4):
            P.op("dve", lambda e: e.tensor_scalar(out=Dd[:, c4, :], in0=C["ident_f"][:], scalar1=dl[:, c4:c4 + 1],
                                                  scalar2=None, op0=ALU.mult), reads=[prm, C["ident_f_b"]], writes=[prm])
        R["Dd"] = Dd
        R["bglu"] = ld("s5_bglu", [128, 4])
        wg = P.sb("S_wglu", [128, 4, 512], BF16)
        P.dma("pool", wg[:], w_src(T["s5_wglu"], 512, 0, 4, 0, 512), writes=[prm])
        R["wglu"] = wg
    return R


def load_uT(P, T, ub):
    uT = P.sb("S_uT", [128, 4, TPC], BF16)
    uT_b = Buf("S_uT")
    for c4 in range(4):
        P.dma("sp", uT[:, c4, :], T["uT"][128 * c4:128 * (c4 + 1), :], reads=[ub] if ub is not None else [],
              writes=[uT_b])
    return uT, uT_b


def emit_s5_pass1(P, T, C, uT_dram_b=None):
    R = s5_prep(P, T, C, need_rev=True, need_fwd=False)
    prm = R["prm"]
    uT, uT_b = load_uT(P, T, uT_dram_b)
    bu = [P.ps(f"S1_bu{i}", [128, 512], F32) for i in range(4)]
    bu_b = [Buf(f"S1_bu{i}") for i in range(4)]
    junk = P.sb("S1_junk", [128, 128], F32)
    junk_b = Buf("S1_junk")
    Ea = P.sb("S1_Ea", [128, 4, 16, 16], F32)
    Ea_b = Buf("S1_Ea")
    P.op("dve", lambda e: e.memset(Ea[:], 0.0), writes=[Ea_b])
    k = 0
    for tg in range(4):
        ts_ = slice(512 * tg, 512 * (tg + 1))
        for gp in range(16):
            br_i, bi_i = (2 * k) % 4, (2 * k + 1) % 4
            k += 1
            P.op("pe", lambda e: e.matmul(out=bu[br_i][:], lhsT=R["BTr"][:, gp, :], rhs=uT[:, gp // 4, ts_], start=True,
                                          stop=True), reads=[prm, uT_b], writes=[bu_b[br_i]])
            P.op("pe", lambda e: e.matmul(out=bu[bi_i][:], lhsT=R["BTi"][:, gp, :], rhs=uT[:, gp // 4, ts_], start=True,
                                          stop=True), reads=[prm, uT_b], writes=[bu_b[bi_i]])
            for s in range(4):
                j = 4 * tg + s
                cs_ = slice(128 * s, 128 * (s + 1))
                for kind, (src_i, tab) in enumerate(((br_i, R["Qr"]), (bi_i, R["Qi"]), (br_i, R["Qi"]), (bi_i, R["Qr"]))):
                    P.op("dve", lambda e: e.scalar_tensor_tensor(out=junk[:], in0=bu[src_i][:, cs_], scalar=1.0,
                                                                 in1=tab[:, gp, :], op0=ALU.mult, op1=ALU.mult,
                                                                 accum_out=Ea[:, kind, gp, j:j + 1]),
                         reads=[bu_b[src_i], prm], writes=[junk_b, Ea_b])
    Eo = P.sb("S1_Eo", [128, 2, 256], F32)
    Eo_b = Buf("S1_Eo")
    P.op("dve", lambda e: e.tensor_tensor(out=Eo[:, 0, :], in0=Ea[:, 0].rearrange("p a b -> p (a b)"),
                                          in1=Ea[:, 1].rearrange("p a b -> p (a b)"), op=ALU.subtract),
         reads=[Ea_b], writes=[Eo_b])
    P.op("dve", lambda e: e.tensor_tensor(out=Eo[:, 1, :], in0=Ea[:, 2].rearrange("p a b -> p (a b)"),
                                          in1=Ea[:, 3].rearrange("p a b -> p (a b)"), op=ALU.add),
         reads=[Ea_b], writes=[Eo_b])
    for ri in range(2):
        P.dma("sp", T["E"][ri], Eo[:, ri, :], reads=[Eo_b])


def emit_s5_pass2(P, T, C):
    R = s5_prep(P, T, C, need_rev=False, need_fwd=True)
    prm = R["prm"]
    uT, uT_b = load_uT(P, T, None)
    Eall = P.sb("S2_E", [128, 8, 2, 256], F32)
    Eall_b = Buf("S2_E")
    for r in range(8):
        for ri in range(2):
            P.dma("sp", Eall[:, r, ri, :], T["E_g"][r, ri], writes=[Eall_b])
    selr = P.sb("S2_sel", [128, 8], F32)
    P.dma("sp", selr[:], T["selr"], writes=[Eall_b])
    Sall = P.sb("S2_S", [128, 8, 2, 16, 16], F32)
    Sall_b = Buf("S2_S")
    cur = P.sb("S2_cur", [128, 2, 16], F32)
    tq = P.sb("S2_tq", [128, 4, 16], F32)
    P.op("dve", lambda e: e.memset(cur[:], 0.0), writes=[Sall_b])
    order = {}
    for r in range(8):
        for j in range(NSLOT):
            order[blk_of(r, j)] = (r, j)
    Ev = Eall[:].rearrange("p r i (g j) -> p r i g j", j=16)
    L = R["l128"]
    for b in range(128):
        r, j = order[b]
        for ri in range(2):
            P.op("dve", lambda e: e.tensor_copy(out=Sall[:, r, ri, :, j], in_=cur[:, ri, :]), reads=[Sall_b],
                 writes=[Sall_b])
        if b == 127:
            break
        dd = [Sall_b, prm, Eall_b]
        P.op("dve", lambda e: e.tensor_tensor(out=tq[:, 0, :], in0=cur[:, 0, :], in1=L[:, 0, :], op=ALU.mult), reads=dd, writes=[Sall_b])
        P.op("dve", lambda e: e.tensor_tensor(out=tq[:, 1, :], in0=cur[:, 1, :], in1=L[:, 1, :], op=ALU.mult), reads=dd, writes=[Sall_b])
        P.op("dve", lambda e: e.tensor_tensor(out=tq[:, 2, :], in0=cur[:, 0, :], in1=L[:, 1, :], op=ALU.mult), reads=dd, writes=[Sall_b])
        P.op("dve", lambda e: e.tensor_tensor(out=tq[:, 3, :], in0=cur[:, 1, :], in1=L[:, 0, :], op=ALU.mult), reads=dd, writes=[Sall_b])
        P.op("dve", lambda e: e.tensor_tensor(out=tq[:, 0, :], in0=tq[:, 0, :], in1=tq[:, 1, :], op=ALU.subtract), reads=dd, writes=[Sall_b])
        P.op("dve", lambda e: e.tensor_tensor(out=tq[:, 2, :], in0=tq[:, 2, :], in1=tq[:, 3, :], op=ALU.add), reads=dd, writes=[Sall_b])
        P.op("dve", lambda e: e.tensor_tensor(out=cur[:, 0, :], in0=tq[:, 0, :], in1=Ev[:, r, 0, :, j], op=ALU.add), reads=dd, writes=[Sall_b])
        P.op("dve", lambda e: e.tensor_tensor(out=cur[:, 1, :], in0=tq[:, 2, :], in1=Ev[:, r, 1, :, j], op=ALU.add), reads=dd, writes=[Sall_b])
    Sown = P.sb("S2_own", [128, 2, 16, 16], F32)
    So2 = Sown[:].rearrange("p i g j -> p (i g j)")
    for r in range(8):
        src = Sall[:, r].rearrange("p i g j -> p (i g j)")
        if r == 0:
            P.op("dve", lambda e: e.tensor_scalar(out=So2, in0=src, scalar1=selr[:, 0:1], scalar2=None, op0=ALU.mult),
                 reads=[Sall_b, Eall_b], writes=[Sall_b])
        else:
            P.op("dve", lambda e: e.scalar_tensor_tensor(out=So2, in0=src, scalar=selr[:, r:r + 1], in1=So2,
                                                         op0=ALU.mult, op1=ALU.add), reads=[Sall_b, Eall_b],
                 writes=[Sall_b])
    LS = P.sb("S2_LS", [128, 2, 16, 16], F32)
    tl = P.sb("S2_tl", [128, 2, 16, 16], F32)
    lam1 = R["lam1"]

    def bcj(a, m):
        return bass.AP(tensor=a.tensor, offset=a.offset, ap=[list(a.ap[0]), list(a.ap[1]), [0, m]])
    cplx_mul(P, "dve", LS[:, 0], LS[:, 1], Sown[:, 0], Sown[:, 1], bcj(lam1[:, 0, :], 16), bcj(lam1[:, 1, :], 16),
             (tl[:, 0], tl[:, 1]), [Sall_b, prm], [Sall_b], Sall_b)

    bu = [P.ps(f"S2_bu{i}", [128, 512], F32) for i in range(4)]
    bu_b = [Buf(f"S2_bu{i}") for i in range(4)]
    yps = [P.ps(f"S2_y{i}", [128, 512], F32) for i in range(2)]
    yps_b = [Buf(f"S2_y{i}") for i in range(2)]
    gps = P.ps("S2_g", [128, 512], F32)
    gps_b = Buf("S2_g")
    tmp = [P.sb(f"S2_t{i}", [128, 512], F32) for i in range(4)]
    tmp_b = [Buf(f"S2_t{i}") for i in range(4)]
    z = [P.sb(f"S2_z{i}", [128, 512], F32) for i in range(2)]
    z_b = [Buf(f"S2_z{i}") for i in range(2)]
    cc = [P.sb(f"S2_c{i}", [128, 512], F32) for i in range(2)]
    cc_b = [Buf(f"S2_c{i}") for i in range(2)]
    xb = [[P.sb(f"S2_x{i}{k}", [128, 512], BF16) for k in range(2)] for i in range(2)]
    xb_b = [[Buf(f"S2_x{i}{k}") for k in range(2)] for i in range(2)]
    rmask = P.sb("S2_rm", [128, 512], F32)
    rmask_b = Buf("S2_rm")
    P.op("pool", lambda e: e.memset(rmask[:], 1.0), writes=[rmask_b])
    P.op("pool", lambda e: e.memset(rmask[:].rearrange("p (s i) -> p s i", i=128)[:, :, 0:1], 0.0), reads=[rmask_b],
         writes=[rmask_b])
    yf = P.sb("S2_yf", [128, 4, 512], F32)
    yf_b = Buf("S2_yf")
    yg = P.sb("S2_yg", [128, 4, 512], BF16)
    yg_b = Buf("S2_yg")
    gt = [P.sb(f"S2_gt{i}", [128, 512], F32) for i in range(2)]
    gt_b = [Buf(f"S2_gt{i}") for i in range(2)]
    ob = [P.sb(f"S2_ob{i}", [128, 512], BF16) for i in range(2)]
    ob_b = [Buf(f"S2_ob{i}") for i in range(2)]

    def b4(t, gp):
        a = t[:, gp, :]
        return bass.AP(tensor=a.tensor, offset=a.offset, ap=[list(a.ap[0]), [0, 4], [1, 128]])

    def v4(a):
        return a.rearrange("p (s i) -> p s i", i=128)
    k = 0
    oi = 0
    for tg in range(4):
        ts_ = slice(512 * tg, 512 * (tg + 1))
        for c4 in range(4):
            yi = (4 * tg + c4) % 2
            for q in range(4):
                gp = 4 * c4 + q
                br_i, bi_i = (2 * k) % 4, (2 * k + 1) % 4
                xi = k % 2
                k += 1
                P.op("pe", lambda e: e.matmul(out=bu[br_i][:], lhsT=R["BTr"][:, gp, :], rhs=uT[:, c4, ts_], start=True,
                                              stop=True), reads=[prm, uT_b], writes=[bu_b[br_i]])
                P.op("pe", lambda e: e.matmul(out=bu[bi_i][:], lhsT=R["BTi"][:, gp, :], rhs=uT[:, c4, ts_], start=True,
                                              stop=True), reads=[prm, uT_b], writes=[bu_b[bi_i]])
                bur, bui = v4(bu[br_i][:]), v4(bu[bi_i][:])
                Ir, Ii, Pr, Pi = b4(R["Ir"], gp), b4(R["Ii"], gp), b4(R["Pr"], gp), b4(R["Pi"], gp)
                P.op("dve", lambda e: e.tensor_tensor(out=v4(tmp[0][:]), in0=bur, in1=Ir, op=ALU.mult), reads=[bu_b[br_i], prm], writes=[tmp_b[0]])
                P.op("dve", lambda e: e.tensor_tensor(out=v4(tmp[1][:]), in0=bui, in1=Ii, op=ALU.mult), reads=[bu_b[bi_i], prm], writes=[tmp_b[1]])
                P.op("dve", lambda e: e.tensor_tensor(out=v4(tmp[2][:]), in0=bur, in1=Ii, op=ALU.mult), reads=[bu_b[br_i], prm], writes=[tmp_b[2]])
                P.op("dve", lambda e: e.tensor_tensor(out=v4(tmp[3][:]), in0=bui, in1=Ir, op=ALU.mult), reads=[bu_b[bi_i], prm], writes=[tmp_b[3]])
                P.op("pool", lambda e: e.tensor_tensor(out=z[0][:], in0=tmp[0][:], in1=tmp[1][:], op=ALU.subtract), reads=[tmp_b[0], tmp_b[1]], writes=[z_b[0]])
                P.op("pool", lambda e: e.tensor_tensor(out=z[1][:], in0=tmp[2][:], in1=tmp[3][:], op=ALU.add), reads=[tmp_b[2], tmp_b[3]], writes=[z_b[1]])
                for ri in range(2):
                    zc = v4(z[ri][:])[:, :, 0:1]
                    cs_ = LS[:, ri, gp, 4 * tg:4 * tg + 4]
                    cs3 = bass.AP(tensor=cs_.tensor, offset=cs_.offset, ap=[list(cs_.ap[0]), list(cs_.ap[1]), [1, 1]])
                    P.op("pool", lambda e: e.tensor_tensor(out=zc, in0=zc, in1=cs3, op=ALU.add), reads=[z_b[ri], Sall_b], writes=[z_b[ri]])
                    P.op("dve", lambda e: e.tensor_tensor_scan(out=cc[ri][:], data0=rmask[:], data1=z[ri][:], initial=0.0,
                                                               op0=ALU.mult, op1=ALU.add), reads=[z_b[ri], rmask_b], writes=[cc_b[ri]])
                c0, c1 = v4(cc[0][:]), v4(cc[1][:])
                P.op("dve", lambda e: e.tensor_tensor(out=v4(tmp[0][:]), in0=c0, in1=Pr, op=ALU.mult), reads=[cc_b[0], prm], writes=[tmp_b[0]])
                P.op("dve", lambda e: e.tensor_tensor(out=v4(tmp[1][:]), in0=c1, in1=Pi, op=ALU.mult), reads=[cc_b[1], prm], writes=[tmp_b[1]])
                P.op("dve", lambda e: e.tensor_tensor(out=v4(tmp[2][:]), in0=c0, in1=Pi, op=ALU.mult), reads=[cc_b[0], prm], writes=[tmp_b[2]])
                P.op("dve", lambda e: e.tensor_tensor(out=v4(tmp[3][:]), in0=c1, in1=Pr, op=ALU.mult), reads=[cc_b[1], prm], writes=[tmp_b[3]])
                P.op("pool", lambda e: e.tensor_tensor(out=xb[xi][0][:], in0=tmp[0][:], in1=tmp[1][:], op=ALU.subtract), reads=[tmp_b[0], tmp_b[1]], writes=[xb_b[xi][0]])
                P.op("pool", lambda e: e.tensor_tensor(out=xb[xi][1][:], in0=tmp[2][:], in1=tmp[3][:], op=ALU.add), reads=[tmp_b[2], tmp_b[3]], writes=[xb_b[xi][1]])
                P.op("pe", lambda e: e.matmul(out=yps[yi][:], lhsT=R["ZCr"][:, gp, :], rhs=xb[xi][0][:], start=(q == 0), stop=False),
                     reads=[prm, xb_b[xi][0]], writes=[yps_b[yi]])
                P.op("pe", lambda e: e.matmul(out=yps[yi][:], lhsT=R["ZCi"][:, gp, :], rhs=xb[xi][1][:], start=False, stop=False),
                     reads=[prm, xb_b[xi][1]], writes=[yps_b[yi]])
            P.op("pe", lambda e: e.matmul(out=yps[yi][:], lhsT=R["Dd"][:, c4, :], rhs=uT[:, c4, ts_], start=False, stop=True),
                 reads=[prm, uT_b], writes=[yps_b[yi]])
            g0, g1 = gt[0], gt[1]
            P.op("act", lambda e: e.activation(out=g0[:], in_=yps[yi][:], func=AF.Square), reads=[yps_b[yi]], writes=[gt_b[0]])
            P.op("dve", lambda e: e.tensor_scalar(out=g0[:], in0=g0[:], scalar1=0.044715, scalar2=1.0, op0=ALU.mult, op1=ALU.add),
                 reads=[gt_b[0]], writes=[gt_b[0]])
            P.op("dve", lambda e: e.tensor_tensor(out=g0[:], in0=yps[yi][:], in1=g0[:], op=ALU.mult), reads=[yps_b[yi], gt_b[0]], writes=[gt_b[0]])
            P.op("act", lambda e: e.activation(out=g1[:], in_=g0[:], func=AF.Sigmoid, scale=1.5957691216057308), reads=[gt_b[0]], writes=[gt_b[1]])
            P.op("dve", lambda e: e.tensor_tensor(out=yf[:, c4, :], in0=yps[yi][:], in1=g1[:], op=ALU.mult), reads=[yps_b[yi], gt_b[1]], writes=[yf_b])
            P.op("pool", lambda e: e.tensor_copy(out=yg[:, c4, :], in_=yf[:, c4, :]), reads=[yf_b], writes=[yg_b])
        for oc in range(4):
            for c4 in range(4):
                P.op("pe", lambda e: e.matmul(out=gps[:], lhsT=R["wglu"][:, c4, 128 * oc:128 * (oc + 1)], rhs=yg[:, c4, :],
                                              start=(c4 == 0), stop=(c4 == 3)), reads=[prm, yg_b], writes=[gps_b])
            P.op("act", lambda e: e.activation(out=gt[0][:], in_=gps[:], func=AF.Sigmoid, bias=R["bglu"][:, oc:oc + 1]),
                 reads=[gps_b, prm], writes=[gt_b[0]])
            o_i = oi % 2
            oi += 1
            P.op("dve", lambda e: e.tensor_tensor(out=ob[o_i][:], in0=yf[:, oc, :], in1=gt[0][:], op=ALU.mult),
                 reads=[yf_b, gt_b[0]], writes=[ob_b[o_i]])
            P.dma("sp", T["catT"][512 + 128 * oc:512 + 128 * (oc + 1), ts_], ob[o_i][:], reads=[ob_b[o_i]])


def build_S(which):
    nc = bass.Bass("TRN2", target_bir_lowering=False)
    T = {}

    def din(name, shape, dt=F32):
        T[name] = nc.dram_tensor(name, list(shape), dt, kind="ExternalInput").ap()

    def dout(name, shape, dt=F32):
        T[name] = nc.dram_tensor(name, list(shape), dt, kind="ExternalOutput").ap()
    for k, shp in S5_IN.items():
        din(k, shp)
    din("ident", [128, 128]); din("uT", [512, TPC], BF16)
    P = Prog(nc)
    P.begin_phase("S")
    C = make_consts(P, T["ident"])
    if which == 1:
        dout("E", [2, 128, 256])
        emit_s5_pass1(P, T, C)
    else:
        din("E_g", [8, 2, 128, 256]); din("selr", [128, 8]); dout("catT", [D, TPC], BF16)
        emit_s5_pass2(P, T, C)
    P.end_phase()
    P.finish()
    return nc


DSA_NIT = 27
DSA_LIM = 64.0


def emit_phase_DA(P, T, C, slots=range(NSLOT)):
    U8 = mybir.dt.uint8
    isc = (64.0 ** -0.5) * (8.0 ** -0.5)
    asc = 128.0 ** -0.5
    ones = P.sb("D_ones", [128, 128], BF16)
    zeros = P.sb("D_zeros", [128, 128], BF16)
    cst_b = Buf("D_cst")
    P.op("dve", lambda e: e.memset(ones[:], 1.0), writes=[cst_b])
    P.op("dve", lambda e: e.memset(zeros[:], 0.0), writes=[cst_b])
    score = P.sb("D_score", [128, 8, 2048], F32)
    score_b = Buf("D_score")
    junk = P.sb("D_junk", [128, 8, 2048], U8)
    junk_b = Buf("D_junk")
    qi_t = P.sb("D_qi", [64, 8, 128], BF16)
    qi_b = Buf("D_qi")
    qmq = P.sb("D_qmq", [64, 128], BF16)
    wcol = P.sb("D_wcol", [128, 8], F32)
    dg = P.sb("D_dg", [128, 8, 128], BF16)
    dg_b = Buf("D_dg")
    kir = [P.sb(f"D_kir{i}", [64, 2048], BF16) for i in range(2)]
    kmr = [P.sb(f"D_kmr{i}", [64, 512], BF16) for i in range(2)]
    kir_b = [Buf(f"D_kir{i}") for i in range(2)]
    Rh = [P.sb(f"D_Rh{i}", [128, 8, 512], BF16) for i in range(2)]
    Rh_b = [Buf(f"D_Rh{i}") for i in range(2)]
    qa_t = P.sb("D_qa", [128, 4, 128], BF16)
    qa_b = Buf("D_qa")
    KA = [P.sb(f"D_KA{i}", [128, 4, 2048], BF16) for i in range(2)]
    KA_b = [Buf(f"D_KA{i}") for i in range(2)]
    VA = [P.sb(f"D_VA{i}", [128, 16, 512], BF16) for i in range(2)]
    VA_b = [Buf(f"D_VA{i}") for i in range(2)]
    m01 = [P.sb(f"D_m01{i}", [128, 512], BF16) for i in range(2)]
    m01_b = [Buf(f"D_m01{i}") for i in range(2)]
    mT4 = [P.sb(f"D_mT{i}", [128, 4, 128], BF16) for i in range(2)]
    mT4_b = [Buf(f"D_mT{i}") for i in range(2)]
    PTr = [P.sb(f"D_PTr{i}", [128, 512], BF16) for i in range(2)]
    PTr_b = [Buf(f"D_PTr{i}") for i in range(2)]
    PTm = [P.sb(f"D_PTm{i}", [128, 4, 128], BF16) for i in range(3)]
    PTm_b = [Buf(f"D_PTm{i}") for i in range(3)]
    bs = P.sb("D_bs", [128, 8], F32)
    bs_b = Buf("D_bs")
    rz = P.sb("D_rz", [128, 512], F32)
    rz_b = Buf("D_rz")
    oo = [P.sb(f"D_oo{i}", [128, 4, 128], BF16) for i in range(2)]
    oo_b = [Buf(f"D_oo{i}") for i in range(2)]
    dps = [P.ps(f"D_dps{i}", [128, 512], F32) for i in range(2)]
    dps_b = [Buf(f"D_dps{i}") for i in range(2)]
    scp = P.ps("D_scp", [128, 512], F32)
    scp_b = Buf("D_scp")
    sT = [P.ps(f"D_sT{i}", [128, 512], F32) for i in range(2)]
    sT_b = [Buf(f"D_sT{i}") for i in range(2)]
    oacc = P.ps("D_oacc", [128, 512], F32)
    oacc_b = Buf("D_oacc")
    zacc = P.ps("D_zacc", [128, 512], F32)
    zacc_b = Buf("D_zacc")
    mtp = P.ps("D_mtp", [128, 512], BF16)
    mtp_b = Buf("D_mtp")
    qiT, kiT, wi, qaT, kaT, va, qm, km = (T[k] for k in ("qiT", "kiT_g", "wi", "qaT", "kaT_g", "va_g", "qm", "km_g"))
    ident_f, ident, idb = C["ident_f"], C["ident"], [C["ident_f_b"], C["ident_b"]]

    cnt = dict(d=0, r=0, k=0, s=0, p=0, pm=0, m=0, o=0)
    for j in slots:
        g = j // 4
        nk = 4 * g + 4
        nkeys = 128 * nk
        qs = slice(128 * j, 128 * (j + 1))
        P.dma("sp", qi_t[:], bass.AP(tensor=qiT.tensor, offset=qiT.offset + 128 * j, ap=[[TPC, 64], [64 * TPC, 8], [1, 128]]),
              writes=[qi_b])
        P.dma("sp", qmq[:], qm[:, qs], writes=[qi_b])
        P.dma("sp", wcol[:], wi[qs, :], writes=[qi_b])
        P.dma("sp", qa_t[:], bass.AP(tensor=qaT.tensor, offset=qaT.offset + 128 * j, ap=[[TPC, 128], [128 * TPC, 4], [1, 128]]),
              writes=[qa_b])
        for h in range(8):
            P.op("dve", lambda e: e.tensor_scalar(out=dg[:, h, :], in0=ident_f[:], scalar1=wcol[:, h:h + 1], scalar2=isc,
                                                  op0=ALU.mult, op1=ALU.mult), reads=[qi_b] + idb, writes=[dg_b])
        for r in range(8):
            kb = cnt["k"] % 2
            cnt["k"] += 1
            P.dma("sp", kir[kb][:, 0:nkeys], kiT[64 * r:64 * (r + 1), 0:nkeys], writes=[kir_b[kb]])
            P.dma("sp", kmr[kb][:, :], km[64 * r:64 * (r + 1), 512 * g:512 * (g + 1)], writes=[kir_b[kb]])
            for ch in range(g + 1):
                rb = cnt["r"] % 2
                cnt["r"] += 1
                for h in range(8):
                    di = cnt["d"] % 2
                    cnt["d"] += 1
                    P.op("pe", lambda e: e.matmul(out=dps[di][:], lhsT=qi_t[:, h, :], rhs=kir[kb][:, 512 * ch:512 * (ch + 1)],
                                                  start=True, stop=True), reads=[qi_b, kir_b[kb]], writes=[dps_b[di]])
                    P.op("act", lambda e: e.activation(out=Rh[rb][:, h, :], in_=dps[di][:], func=AF.Relu),
                         reads=[dps_b[di]], writes=[Rh_b[rb]])
                bnd = (ch == g)
                for h in range(8):
                    P.op("pe", lambda e: e.matmul(out=scp[:], lhsT=dg[:, h, :], rhs=Rh[rb][:, h, :], start=(h == 0),
                                                  stop=(h == 7 and not bnd)), reads=[dg_b, Rh_b[rb]], writes=[scp_b])
                if bnd:
                    P.op("pe", lambda e: e.matmul(out=scp[:], lhsT=qmq[:], rhs=kmr[kb][:], start=False, stop=True),
                         reads=[qi_b, kir_b[kb]], writes=[scp_b])
                P.op("dve", lambda e: e.tensor_copy(out=score[:, r, 512 * ch:512 * (ch + 1)], in_=scp[:]), reads=[scp_b],
                     writes=[score_b])
        sv = score[:, :, 0:nkeys]
        jv = junk[:, :, 0:nkeys]
        lo, hd, mid, cn, mm = (bs[:, i:i + 1] for i in range(5))
        P.op("dve", lambda e: e.memset(lo, -DSA_LIM), reads=[bs_b], writes=[bs_b])
        P.op("dve", lambda e: e.memset(hd, 2.0 * DSA_LIM), reads=[bs_b], writes=[bs_b])
        for it in range(DSA_NIT):
            P.op("dve", lambda e: e.tensor_scalar(out=hd, in0=hd, scalar1=0.5, scalar2=None, op0=ALU.mult), reads=[bs_b], writes=[bs_b])
            P.op("dve", lambda e: e.tensor_tensor(out=mid, in0=lo, in1=hd, op=ALU.add), reads=[bs_b], writes=[bs_b])
            P.op("dve", lambda e: e.tensor_scalar(out=jv, in0=sv, scalar1=mid, scalar2=None, op0=ALU.is_ge, op1=ALU.add,
                                                  accum_out=cn), reads=[bs_b, score_b], writes=[bs_b, junk_b])
            P.op("dve", lambda e: e.tensor_scalar(out=mm, in0=cn, scalar1=255.5, scalar2=None, op0=ALU.is_ge), reads=[bs_b], writes=[bs_b])
            P.op("dve", lambda e: e.scalar_tensor_tensor(out=lo, in0=mm, scalar=hd, in1=lo, op0=ALU.mult, op1=ALU.add),
                 reads=[bs_b], writes=[bs_b])
        thr = lo
        P.op("pe", lambda e: e.matmul(out=oacc[:], lhsT=zeros[:], rhs=Rh[0][:, 0, :], start=True, stop=False,
                                      skip_group_check=True), reads=[cst_b, Rh_b[0]], writes=[oacc_b])
        ntile = 8 * nk
        ti = 0
        for r in range(8):
            ab = cnt["k"] % 2
            cnt["k"] += 1
            P.dma("sp", KA[ab][:, :, 0:nkeys], bass.AP(tensor=kaT.tensor, offset=kaT.offset + r * 512 * TPC,
                                                       ap=[[TPC, 128], [128 * TPC, 4], [1, nkeys]]), writes=[KA_b[ab]])
            P.dma("sp", VA[ab][:, 0:nk, :], bass.AP(tensor=va.tensor, offset=va.offset + r * TPC * 512,
                                                    ap=[[512, 128], [128 * 512, nk], [1, 512]]), writes=[VA_b[ab]])
            for ch in range(g + 1):
                mb = cnt["m"] % 2
                cnt["m"] += 1
                P.op("dve", lambda e: e.tensor_scalar(out=m01[mb][:], in0=score[:, r, 512 * ch:512 * (ch + 1)], scalar1=thr,
                                                      scalar2=None, op0=ALU.is_ge), reads=[score_b, bs_b], writes=[m01_b[mb]])
                for t in range(4):
                    P.op("pe", lambda e: e.transpose(out=mtp[:, 128 * t:128 * (t + 1)], in_=m01[mb][:, 128 * t:128 * (t + 1)],
                                                     identity=ident[:]), reads=[m01_b[mb]] + idb, writes=[mtp_b])
                P.op("act", lambda e: e.copy(out=mT4[mb][:], in_=mtp[:].rearrange("p (t q) -> p t q", t=4)), reads=[mtp_b],
                     writes=[mT4_b[mb]])
                for t in range(4):
                    jk = 4 * ch + t
                    si = cnt["s"] % 2
                    cnt["s"] += 1
                    for h in range(4):
                        P.op("pe", lambda e: e.matmul(out=sT[si][:, 128 * h:128 * (h + 1)], lhsT=KA[ab][:, h, 128 * jk:128 * (jk + 1)],
                                                      rhs=qa_t[:, h, :], start=True, stop=True, skip_group_check=True),
                             reads=[KA_b[ab], qa_b], writes=[sT_b[si]])
                    pi = cnt["p"] % 2
                    cnt["p"] += 1
                    P.op("act", lambda e: e.activation(out=PTr[pi][:], in_=sT[si][:], func=AF.Exp, scale=asc), reads=[sT_b[si]],
                         writes=[PTr_b[pi]])
                    pm = cnt["pm"] % 3
                    cnt["pm"] += 1
                    mt = mT4[mb][:, t, :]
                    mbc = bass.AP(tensor=mt.tensor, offset=mt.offset, ap=[list(mt.ap[0]), [0, 4], [1, 128]])
                    eng = "dve" if (cnt["pm"] % 2 == 0) else "pool"
                    P.op(eng, lambda e: e.tensor_tensor(out=PTm[pm][:], in0=PTr[pi][:].rearrange("p (h q) -> p h q", h=4),
                                                        in1=mbc, op=ALU.mult), reads=[PTr_b[pi], mT4_b[mb]], writes=[PTm_b[pm]])
                    last = (ti == ntile - 1)
                    for h in range(4):
                        P.op("pe", lambda e: e.matmul(out=oacc[:, 128 * h:128 * (h + 1)], lhsT=VA[ab][:, jk, 128 * h:128 * (h + 1)],
                                                      rhs=PTm[pm][:, h, :], start=False, stop=last, skip_group_check=True),
                             reads=[VA_b[ab], PTm_b[pm]], writes=[oacc_b])
                    P.op("pe", lambda e: e.matmul(out=zacc[:], lhsT=ones[:], rhs=PTm[pm][:].rearrange("p h q -> p (h q)"),
                                                  start=(ti == 0), stop=last), reads=[cst_b, PTm_b[pm]], writes=[zacc_b])
                    ti += 1
        P.op("dve", lambda e: e.reciprocal(out=rz[:], in_=zacc[:]), reads=[zacc_b], writes=[rz_b])
        ob = cnt["o"] % 2
        cnt["o"] += 1
        P.op("dve", lambda e: e.tensor_tensor(out=oo[ob][:].rearrange("p h q -> p (h q)"), in0=oacc[:], in1=rz[:], op=ALU.mult),
             reads=[oacc_b, rz_b], writes=[oo_b[ob]])
        cat = T["catT"]
        P.dma("sp", bass.AP(tensor=cat.tensor, offset=cat.offset + 128 * j, ap=[[TPC, 128], [128 * TPC, 4], [1, 128]]),
              oo[ob][:], reads=[oo_b[ob]])


def build_DA(slots=range(NSLOT)):
    nc = bass.Bass("TRN2", target_bir_lowering=False)
    T = {}

    def din(name, shape, dt=F32):
        T[name] = nc.dram_tensor(name, list(shape), dt, kind="ExternalInput").ap()

    def dout(name, shape, dt=F32):
        T[name] = nc.dram_tensor(name, list(shape), dt, kind="ExternalOutput").ap()
    din("qiT", [512, TPC], BF16); din("kiT_g", [8 * 64, TPC], BF16); din("wi", [TPC, 8])
    din("qaT", [512, TPC], BF16); din("kaT_g", [8 * 512, TPC], BF16); din("va_g", [8 * TPC, 512], BF16)
    din("qm", [64, TPC], BF16); din("km_g", [8 * 64, TPC], BF16); din("ident", [128, 128])
    dout("catT", [D, TPC], BF16)
    P = Prog(nc)
    P.begin_phase("DA")
    C = make_consts(P, T["ident"])
    emit_phase_DA(P, T, C, slots)
    P.end_phase()
    P.finish()
    return nc


A_OUT = dict(qaT=([512, TPC], BF16), kaT=([512, TPC], BF16), qiT=([512, TPC], BF16), kiT=([64, TPC], BF16),
             qcT=([1024, TPC], BF16), kcT=([1024, TPC], BF16), uT=([512, TPC], BF16), va=([TPC, 512], BF16),
             vc=([TPC, 1024], BF16), wi=([TPC, 8], F32))


def build_PA():
    nc = bass.Bass("TRN2", target_bir_lowering=False)
    perm, FM, TM = win_units()
    T = {}

    def din(name, shape, dt=F32):
        T[name] = nc.dram_tensor(name, list(shape), dt, kind="ExternalInput").ap()

    def dout(name, shape, dt=BF16):
        T[name] = nc.dram_tensor(name, list(shape), dt, kind="ExternalOutput").ap()
    din("x", [TPC, D]); din("g", [1, D]); din("win", [D, IN_WIDTH]); din("ident", [128, 128])
    for nm in ("cosA", "sinA", "cosC", "sinC"):
        din(nm, [128, TPC])
    for k, shp in S5_IN.items():
        din(k, shp)
    for k, (shp, dt) in A_OUT.items():
        dout(k, shp, dt)
    dout("E", [2, 128, 256], F32)
    P = Prog(nc)
    P.begin_phase("A")
    emit_phase_A(P, T, FM, TM)
    P.end_phase()
    P.begin_phase("S1")
    C = make_consts(P, T["ident"])
    emit_s5_pass1(P, T, C)
    P.end_phase()
    P.finish()
    return nc


B_IN_BF = dict(qcT=[1024, TPC], qaT=[512, TPC], qiT=[512, TPC], uT=[512, TPC], kcT_g=[8 * 1024, TPC], vc_g=[8 * TPC, 1024],
               kaT_g=[8 * 512, TPC], va_g=[8 * TPC, 512], kiT_g=[8 * 64, TPC], qm=[64, TPC], km_g=[8 * 64, TPC])
B_IN_F = dict(x=[TPC, D], wi=[TPC, 8], E_g=[8, 2, 128, 256], selr=[128, 8], lamv=[4, 64], lamc=[1, 2], subln=[128, 1],
              ident=[128, 128], wout=[D, D], mem=[256, D], gx=[1, D], gm=[1, D], wq=[D, D], wk=[D, D], wv=[D, D],
              wo=[D, D], gmlp=[1, D], w1=[D, 8192], w2=[8192, D])


def build_PB():
    nc = bass.Bass("TRN2", target_bir_lowering=False)
    T = {}
    for k, shp in B_IN_BF.items():
        T[k] = nc.dram_tensor(k, list(shp), BF16, kind="ExternalInput").ap()
    for k, shp in B_IN_F.items():
        T[k] = nc.dram_tensor(k, list(shp), F32, kind="ExternalInput").ap()
    for k, shp in S5_IN.items():
        T[k] = nc.dram_tensor(k, list(shp), F32, kind="ExternalInput").ap()
    T["catT"] = nc.dram_tensor("catT", [D, TPC], BF16, kind="Internal").ap()
    T["x1"] = nc.dram_tensor("x1", [TPC, D], F32, kind="Internal").ap()
    T["x2"] = nc.dram_tensor("x2", [TPC, D], F32, kind="Internal").ap()
    T["x3"] = nc.dram_tensor("x3", [TPC, D], F32, kind="ExternalOutput").ap()
    P = Prog(nc)
    P.begin_phase("C")
    emit_phase_C(P, T)
    P.end_phase()
    P.begin_phase("DA")
    C = make_consts(P, T["ident"])
    emit_phase_DA(P, T, C)
    P.end_phase()
    P.begin_phase("S2")
    C = make_consts(P, T["ident"])
    emit_s5_pass2(P, T, C)
    P.end_phase()
    for nm, fn in (("O", emit_phase_O), ("X", emit_phase_X), ("M", emit_phase_M)):
        P.begin_phase(nm)
        fn(P, T)
        P.end_phase()
    P.finish()
    return nc, P


def build_PF():
    nc = bass.Bass("TRN2", target_bir_lowering=False)
    T = {}
    T["x3"] = nc.dram_tensor("x3", [TPC, D], F32, kind="ExternalInput").ap()
    T["gf"] = nc.dram_tensor("gf", [1, D], F32, kind="ExternalInput").ap()
    T["y"] = nc.dram_tensor("y", [TPC, D], F32, kind="ExternalOutput").ap()
    P = Prog(nc)
    P.begin_phase("F")
    emit_phase_F(P, T)
    P.end_phase()
    P.finish()
    return nc


def _bf(a):
    import ml_dtypes
    a = np.asarray(a)
    if a.dtype == ml_dtypes.bfloat16:
        return a
    return a.astype(ml_dtypes.bfloat16)


def kernel(x, mem, norm_mix, w_in, s5_a_re, s5_a_im, s5_log_dt, s5_b_re, s5_b_im, s5_c_re, s5_c_im, s5_d, s5_w_glu,
           s5_b_glu, diff_lam_q1, diff_lam_k1, diff_lam_q2, diff_lam_k2, diff_subln, w_out, norm_xattn, norm_mem,
           xattn_q, xattn_k, xattn_v, xattn_o, norm_mlp, w_ff1, w_ff2, norm_final):
    f32 = np.float32
    x = np.asarray(x, f32)
    cores = list(range(NCORES))
    pos = [core_positions(c) for c in cores]
    perm, _, _ = win_units()
    ident = np.eye(128, dtype=f32)
    ropes = [rope_tables_fm(c) for c in cores]
    qms = [_bf(mask_rows_q(c)) for c in cores]
    km_g = _bf(mask_rows_k())
    selrs = []
    for c in cores:
        s = np.zeros((128, 8), f32)
        s[:, c] = 1.0
        selrs.append(s)
    xs = [np.ascontiguousarray(x[0][pos[c]]) for c in cores]
    mem0 = np.ascontiguousarray(np.asarray(mem, f32)[0])
    ncA = build_PA()
    ncB, _ = build_PB()
    ncF = build_PF()
    for l in range(DEPTH):
        s5 = s5_host_layout(*(np.asarray(a[l], f32) for a in (s5_a_re, s5_a_im, s5_log_dt, s5_b_re, s5_b_im, s5_c_re,
                                                                s5_c_im, s5_d, s5_w_glu, s5_b_glu)))
        win = np.ascontiguousarray(np.asarray(w_in[l], f32)[:, perm])
        g = np.asarray(norm_mix[l], f32)[None, :].copy()
        in_maps = []
        for c in cores:
            m = dict(x=xs[c], g=g, win=win, ident=ident)
            m.update(ropes[c])
            m.update(s5)
            in_maps.append(m)
        ra = run_bass_kernel_spmd(ncA, in_maps, core_ids=cores).results
        gath = {}
        for k in ("kcT", "vc", "kaT", "va", "kiT"):
            gath[k + "_g"] = np.ascontiguousarray(np.concatenate([np.asarray(ra[c][k]) for c in cores], axis=0))
        E_g = np.ascontiguousarray(np.stack([np.asarray(ra[c]["E"], f32) for c in cores], axis=0))
        lam_init = 0.8 - 0.6 * math.exp(-0.3 * l)
        lamv = np.stack([np.asarray(a[l], f32) for a in (diff_lam_q1, diff_lam_k1, diff_lam_q2, diff_lam_k2)])
        common = dict(E_g=E_g, km_g=km_g, lamv=lamv, lamc=np.array([[lam_init, 1.0 - lam_init]], f32),
                      subln=np.asarray(diff_subln[l], f32)[:, None].copy(), ident=ident,
                      wout=np.asarray(w_out[l], f32), mem=mem0, gx=np.asarray(norm_xattn[l], f32)[None, :].copy(),
                      gm=np.asarray(norm_mem[l], f32)[None, :].copy(), wq=np.asarray(xattn_q[l], f32),
                      wk=np.asarray(xattn_k[l], f32), wv=np.asarray(xattn_v[l], f32), wo=np.asarray(xattn_o[l], f32),
                      gmlp=np.asarray(norm_mlp[l], f32)[None, :].copy(), w1=np.asarray(w_ff1[l], f32),
                      w2=np.asarray(w_ff2[l], f32))
        common.update(gath)
        common.update(s5)
        in_maps = []
        for c in cores:
            m = dict(common)
            m.update(x=xs[c], selr=selrs[c], qm=qms[c])
            for k in ("qcT", "qaT", "qiT", "uT", "wi"):
                m[k] = np.asarray(ra[c][k])
            in_maps.append(m)
        rb = run_bass_kernel_spmd(ncB, in_maps, core_ids=cores).results
        xs = [np.asarray(rb[c]["x3"], f32) for c in cores]
    gf = np.asarray(norm_final, f32)[None, :].copy()
    rf = run_bass_kernel_spmd(ncF, [dict(x3=xs[c], gf=gf) for c in cores], core_ids=cores).results
    out = np.empty((1, L, D), f32)
    for c in cores:
        out[0, pos[c]] = np.asarray(rf[c]["y"], f32)
    return out
```

```python
import math
import contextlib
import numpy as np
import concourse.bass as bass
import concourse.mybir as mybir
from concourse.bass_utils import run_bass_kernel_spmd

F32 = mybir.dt.float32
BF16 = mybir.dt.bfloat16
AF = mybir.ActivationFunctionType
ALU = mybir.AluOpType
AX = mybir.AxisListType

NCORES = 8
D = 2048
L = 16384
DEPTH = 4
TPC = L // NCORES
NSLOT = TPC // 128
EPS = 1e-6
IN_WIDTH = 5704
OFF = dict(q_a=0, k_a=512, v_a=1024, q_i=1536, k_i=2048, w_i=2112, u_s=2120, q_c=2632, k_c=3656, v_c=4680)


def blk_of(c, j):
    return 16 * (j // 2) + (c if j % 2 == 0 else 15 - c)


class Buf:
    __slots__ = ("name", "w", "r")

    def __init__(self, name):
        self.name = name
        self.w = None
        self.r = {}


class Prog:
    SAME_ENGINE_SYNC = True

    def __init__(self, nc, n_dma_sems=24):
        self.nc = nc
        self.es = contextlib.ExitStack()
        self.eng = {"pe": nc.tensor, "act": nc.scalar, "dve": nc.vector, "pool": nc.gpsimd, "sp": nc.sync}
        self.sems = []
        self.esem = {}
        self.cnt = {}
        for k in self.eng:
            self.esem[k] = self._newsem("e_" + k)
            self.cnt[k] = 0
        self.waited = {k: {} for k in self.eng}
        self.pe_sems = set()
        self.dma_pool = {}
        for q in ("sp", "pool", "act"):
            self.dma_pool[q] = [[self._newsem(f"d_{q}{i}"), 0] for i in range(n_dma_sems if q != "act" else 4)]
        self.dma_rr = {q: 0 for q in self.dma_pool}
        self.n_inst = 0

    def _newsem(self, name):
        s = self.es.enter_context(self.nc.semaphore(name))
        self.sems.append(s)
        return len(self.sems) - 1

    def begin_phase(self, tag):
        self.pes = contextlib.ExitStack()
        self.ptag = tag
        self.pidx = getattr(self, "pidx", 0) + 1

    def end_phase(self):
        self.barrier()
        self.pes.close()
        self.pes = None

    @contextlib.contextmanager
    def sub_scope(self):
        outer = self.pes
        self.pes = contextlib.ExitStack()
        try:
            yield
        finally:
            self.barrier()
            self.pes.close()
            self.pes = outer

    def sb(self, name, shape, dtype):
        self.uid = getattr(self, "uid", 0) + 1
        return self.pes.enter_context(self.nc.sbuf_tensor(f"sb{self.uid}_{name}", list(shape), dtype))

    def ps(self, name, shape, dtype):
        self.uid = getattr(self, "uid", 0) + 1
        return self.pes.enter_context(self.nc.psum_tensor(f"ps{self.uid}_{name}", list(shape), dtype))

    def _wait(self, e, semidx, val):
        if val <= 0:
            return
        if self.waited[e].get(semidx, 0) >= val:
            return
        self.eng[e].wait_ge(self.sems[semidx], val)
        self.waited[e][semidx] = val

    def _deps(self, e, reads, writes):
        deps = {}
        for b in reads:
            if b.w is not None:
                s, v = b.w
                deps[s] = max(deps.get(s, 0), v)
        for b in writes:
            if b.w is not None:
                s, v = b.w
                deps[s] = max(deps.get(s, 0), v)
            for s, v in b.r.items():
                deps[s] = max(deps.get(s, 0), v)
        for s, v in deps.items():
            if (not self.SAME_ENGINE_SYNC) and s == self.esem[e]:
                continue
            if e == "pe" and (s == self.esem["pe"] or s in self.pe_sems):
                continue
            self._wait(e, s, v)

    def _mark(self, ev, reads, writes):
        s, v = ev
        for b in reads:
            if b.r.get(s, 0) < v:
                b.r[s] = v
        for b in writes:
            b.w = ev
            b.r = {}

    SEM_ROTATE = 30000

    def op(self, e, fn, reads=(), writes=()):
        if self.cnt[e] >= self.SEM_ROTATE:
            self.old_esem = getattr(self, "old_esem", [])
            self.old_esem.append((self.esem[e], self.cnt[e]))
            if e == "pe":
                self.pe_sems.add(self.esem[e])
            self.esem[e] = self._newsem(f"e_{e}_{len(self.sems)}")
            self.cnt[e] = 0
        self._deps(e, reads, writes)
        inst = fn(self.eng[e])
        self.cnt[e] += 1
        inst.then_inc(self.sems[self.esem[e]], 1)
        self._mark((self.esem[e], self.cnt[e]), reads, writes)
        self.n_inst += 1

    def dma(self, q, out, in_, reads=(), writes=(), **kw):
        pool = self.dma_pool[q]
        i = self.dma_rr[q]
        self.dma_rr[q] = (i + 1) % len(pool)
        semidx, tot = pool[i]
        self._wait(q, semidx, tot)
        self._deps(q, reads, writes)
        inst = self.eng[q].dma_start(out=out, in_=in_, **kw)
        inst.then_inc(self.sems[semidx], 16)
        pool[i][1] = tot + 16
        self._mark((semidx, tot + 16), reads, writes)
        self.n_inst += 1

    def allgather(self, out_ap, in_ap, reads=(), writes=()):
        if not hasattr(self, "cc_sem"):
            self.cc_sem = self._newsem("cc")
            self.cc_cnt = 0
        self._deps("pool", reads, writes)
        inst = self.eng["pool"].collective_compute("AllGather", ALU.bypass, replica_groups=[list(range(NCORES))],
                                                   ins=[in_ap.opt()], outs=[out_ap.opt()])
        self.cc_cnt += 1
        inst.then_inc(self.sems[self.cc_sem], 1)
        self._mark((self.cc_sem, self.cc_cnt), reads, writes)
        self.n_inst += 1

    def wait_all(self, e):
        if hasattr(self, "cc_sem"):
            self._wait(e, self.cc_sem, self.cc_cnt)
        for k in self.eng:
            if k != e:
                self._wait(e, self.esem[k], self.cnt[k])
        for semidx, tot in getattr(self, "old_esem", []):
            self._wait(e, semidx, tot)
        for q, pool in self.dma_pool.items():
            for semidx, tot in pool:
                self._wait(e, semidx, tot)

    def barrier(self):
        for e in self.eng:
            self.wait_all(e)

    def finish(self):
        self.barrier()
        self.nc.all_engine_barrier()
        self.es.close()


def core_positions(c):
    pos = np.empty(TPC, np.int64)
    for j in range(NSLOT):
        pos[128 * j:128 * (j + 1)] = 128 * blk_of(c, j) + np.arange(128)
    return pos


def rope_tables_fm(c):
    pos = core_positions(c).astype(np.float32)
    out = {}
    for nm, dim in (("A", 128), ("C", 64)):
        inv = (np.float32(10000.0) ** (-(np.arange(0, dim, 2, dtype=np.float32)) / np.float32(dim))).astype(np.float32)
        ang = (pos[:, None] * inv[None, :]).astype(np.float32)
        cos = np.cos(ang).astype(np.float32).T
        sin = np.sin(ang).astype(np.float32).T
        rep = 128 // (dim // 2)
        out["cos" + nm] = np.ascontiguousarray(np.tile(cos, (rep, 1)))
        out["sin" + nm] = np.ascontiguousarray(np.tile(sin, (rep, 1)))
    return out


def win_units():
    perm = []
    fm = []

    def add_rope(name, base, nheads, hd, dst, table):
        half = hd // 2
        hpp = 128 // half
        for p0 in range(0, nheads, hpp):
            heads = list(range(p0, min(nheads, p0 + hpp)))
            A = [base + hd * h + i for h in heads for i in range(half)]
            Bc = [base + hd * h + half + i for h in heads for i in range(half)]
            col0 = len(perm)
            perm.extend(A)
            perm.extend(Bc)
            rowsA = [(hd * h, half, half * k) for k, h in enumerate(heads)]
            rowsB = [(hd * h + half, half, half * k) for k, h in enumerate(heads)]
            fm.append(dict(name=name, col0=col0, M=len(A), rope=table, dst=dst, rowsA=rowsA, rowsB=rowsB))

    add_rope("q_a", OFF["q_a"], 4, 128, "qaT", "A")
    add_rope("k_a", OFF["k_a"], 4, 128, "kaT", "A")
    add_rope("q_i", OFF["q_i"], 8, 64, "qiT", "C")
    add_rope("k_i", OFF["k_i"], 1, 64, "kiT", "C")
    add_rope("q_c", OFF["q_c"], 16, 64, "qcT", "C")
    add_rope("k_c", OFF["k_c"], 16, 64, "kcT", "C")
    for p in range(2):
        col0 = len(perm)
        perm.extend(range(OFF["u_s"] + 256 * p, OFF["u_s"] + 256 * (p + 1)))
        fm.append(dict(name="u_s", col0=col0, M=128, rope=None, dst="uT",
                       rowsA=[(256 * p, 128, 0)], rowsB=[(256 * p + 128, 128, 0)]))
    tm = []
    for name, base, width, dst in (("v_a", OFF["v_a"], 512, "va"), ("v_c0", OFF["v_c"], 512, "vc"),
                                   ("v_c1", OFF["v_c"] + 512, 512, "vc"), ("w_i", OFF["w_i"], 8, "wi")):
        col0 = len(perm)
        perm.extend(range(base, base + width))
        tm.append(dict(name=name, col0=col0, width=width, dst=dst, dcol=(512 if name == "v_c1" else 0)))
    assert len(perm) == IN_WIDTH and len(set(perm)) == IN_WIDTH
    return np.array(perm), fm, tm


class NormT:
    def __init__(self, P, ntok, tag):
        self.P = P
        self.ntok = ntok
        self.tag = tag
        self.hT = P.sb(f"{tag}_hT", [128, 16, ntok], BF16)
        self.hT_b = [Buf(f"hT{j}") for j in range(ntok // 128)]

    def alloc_scratch(self):
        P, tag = self.P, self.tag
        self.xs = [P.sb(f"{tag}_xs{i}", [128, D], F32) for i in range(2)]
        self.xs_b = [Buf(f"{tag}_xs{i}") for i in range(2)]
        self.gb = P.sb(f"{tag}_gb", [128, D], F32)
        self.gb_b = Buf("gb")
        self.hs = [P.sb(f"{tag}_hs{i}", [128, D], BF16) for i in range(2)]
        self.hs_b = [Buf(f"hs{i}") for i in range(2)]
        self.st = [P.sb(f"{tag}_st{i}", [128, 4], F32) for i in range(2)]
        self.st_b = [Buf(f"st{i}") for i in range(2)]
        self.tp = [P.ps(f"{tag}_tp{i}", [128, 512], BF16) for i in range(2)]
        self.tp_b = [Buf(f"tp{i}") for i in range(2)]

    def load_gain(self, g_ap_dram):
        P = self.P
        t = g_ap_dram.tensor
        src = bass.AP(tensor=t, offset=g_ap_dram.offset, ap=[[0, 128], [1, D]])
        P.dma("sp", self.gb[:], src, writes=[self.gb_b])

    def run(self, x_dram, g_dram, C, hT=None, hT_b=None, ntok=None):
        with self.P.sub_scope():
            self.alloc_scratch()
            self.epsb = C["epsb"]
            self.load_gain(g_dram)
            self._run(x_dram, C["ident"], C["ident_b"], hT if hT is not None else self.hT,
                      hT_b if hT_b is not None else self.hT_b, ntok if ntok is not None else self.ntok)

    def _run(self, x_dram, ident, ident_b, hT, hT_b, ntok):
        P = self.P
        nslot = ntok // 128
        k = 0
        for j in range(nslot):
            i = j % 2
            P.dma("sp", self.xs[i][:], x_dram[128 * j:128 * (j + 1), :], writes=[self.xs_b[i]])
            P.op("act", lambda e: e.activation(out=self.hs[i][:], in_=self.xs[i][:], func=AF.Square,
                                               accum_out=self.st[i][:, 0:1]),
                 reads=[self.xs_b[i]], writes=[self.hs_b[i], self.st_b[i]])
            P.op("act", lambda e: e.activation(out=self.st[i][:, 1:2], in_=self.st[i][:, 0:1], func=AF.Sqrt,
                                               scale=1.0 / D, bias=self.epsb[:, 0:1]),
                 reads=[self.st_b[i]], writes=[self.st_b[i]])
            P.op("dve", lambda e: e.reciprocal(out=self.st[i][:, 2:3], in_=self.st[i][:, 1:2]),
                 reads=[self.st_b[i]], writes=[self.st_b[i]])
            P.op("dve", lambda e: e.scalar_tensor_tensor(out=self.hs[i][:], in0=self.xs[i][:],
                                                         scalar=self.st[i][:, 2:3], in1=self.gb[:],
                                                         op0=ALU.mult, op1=ALU.mult),
                 reads=[self.xs_b[i], self.st_b[i], self.gb_b], writes=[self.hs_b[i]])
            for q in range(4):
                tb = k % 2
                k += 1
                for r in range(4):
                    kt = 4 * q + r
                    P.op("pe", lambda e: e.transpose(out=self.tp[tb][:, 128 * r:128 * (r + 1)],
                                                     in_=self.hs[i][:, 128 * kt:128 * (kt + 1)],
                                                     identity=ident[:]),
                         reads=[self.hs_b[i], ident_b], writes=[self.tp_b[tb]])
                dst = hT[:, 4 * q:4 * q + 4, 128 * j:128 * (j + 1)]
                src = self.tp[tb][:].rearrange("p (r t) -> p r t", r=4)
                if q % 2 == 0:
                    P.op("act", lambda e: e.copy(out=dst, in_=src), reads=[self.tp_b[tb]], writes=[hT_b[j]])
                else:
                    P.op("dve", lambda e: e.tensor_copy(out=dst, in_=src), reads=[self.tp_b[tb]],
                         writes=[hT_b[j]])


def make_consts(P, ident_dram):
    c = {}
    c["ident_f"] = P.sb("ident_f", [128, 128], F32)
    c["ident_f_b"] = Buf("ident_f")
    c["ident"] = P.sb("ident", [128, 128], BF16)
    c["ident_b"] = Buf("ident")
    c["epsb"] = P.sb("epsb", [128, 1], F32)
    c["epsb_b"] = Buf("epsb")
    P.dma("sp", c["ident_f"][:], ident_dram, writes=[c["ident_f_b"]])
    P.op("dve", lambda e: e.tensor_copy(out=c["ident"][:], in_=c["ident_f"][:]), reads=[c["ident_f_b"]],
         writes=[c["ident_b"]])
    P.op("dve", lambda e: e.memset(c["epsb"][:], EPS), writes=[c["epsb_b"]])
    return c


def emit_phase_A(P, T, FM, TM):
    nc = P.nc
    C = make_consts(P, T["ident"])
    nt = NormT(P, TPC, "A")
    nt.run(T["x"], T["g"], C)

    tabs = {}
    tab_b = Buf("ropetabs")
    for nm in ("cosA", "sinA", "cosC", "sinC"):
        tabs[nm] = P.sb("tab_" + nm, [128, TPC], F32)
        P.dma("sp", tabs[nm][:], T[nm], writes=[tab_b])
    wbuf = [P.sb(f"A_w{i}", [128, 16, 512], BF16) for i in range(2)]
    wbuf_b = [Buf(f"A_w{i}") for i in range(2)]
    pacc = [P.ps(f"A_acc{i}", [128, 512], F32) for i in range(4)]
    pacc_b = [Buf(f"A_acc{i}") for i in range(4)]
    tmp = [P.sb(f"A_t{i}", [128, 512], F32) for i in range(4)]
    tmp_b = [Buf(f"A_t{i}") for i in range(4)]
    ob = [P.sb(f"A_o{i}", [128, 512], BF16) for i in range(4)]
    ob_b = [Buf(f"A_o{i}") for i in range(4)]
    obf = [P.sb(f"A_of{i}", [128, 8], F32) for i in range(2)]
    obf_b = [Buf(f"A_of{i}") for i in range(2)]
    win = T["win"]
    wt = win.tensor

    wi_ = 0
    pa = 0
    oi = 0
    for u in FM:
        wb = wi_ % 2
        wi_ += 1
        M = u["M"]
        src = bass.AP(tensor=wt, offset=win.offset + u["col0"], ap=[[IN_WIDTH, 128], [128 * IN_WIDTH, 16], [1, 2 * M]])
        P.dma("pool", wbuf[wb][:, :, 0:2 * M], src, writes=[wbuf_b[wb]])
        for tg in range(4):
            ts = slice(512 * tg, 512 * (tg + 1))
            a_i = pa % 4
            b_i = (pa + 1) % 4
            pa += 2
            for half, pi in ((0, a_i), (1, b_i)):
                for kt in range(16):
                    P.op("pe", lambda e: e.matmul(out=pacc[pi][0:M, :], lhsT=wbuf[wb][:, kt, half * M:(half + 1) * M],
                                                  rhs=nt.hT[:, kt, ts], start=(kt == 0), stop=(kt == 15)),
                         reads=[wbuf_b[wb]] + nt.hT_b[4 * tg:4 * tg + 4], writes=[pacc_b[pi]])
            o1 = oi % 4
            o2 = (oi + 1) % 4
            oi += 2
            a = pacc[a_i][0:M, :]
            b = pacc[b_i][0:M, :]
            if u["rope"] is not None:
                cs = tabs["cos" + u["rope"]][0:M, ts]
                sn = tabs["sin" + u["rope"]][0:M, ts]
                t0, t1, t2, t3 = (tmp[i][0:M, :] for i in range(4))
                P.op("dve", lambda e: e.tensor_tensor(out=t0, in0=a, in1=cs, op=ALU.mult),
                     reads=[pacc_b[a_i], tab_b], writes=[tmp_b[0]])
                P.op("dve", lambda e: e.tensor_tensor(out=t1, in0=b, in1=sn, op=ALU.mult),
                     reads=[pacc_b[b_i], tab_b], writes=[tmp_b[1]])
                P.op("dve", lambda e: e.tensor_tensor(out=t2, in0=a, in1=sn, op=ALU.mult),
                     reads=[pacc_b[a_i], tab_b], writes=[tmp_b[2]])
                P.op("dve", lambda e: e.tensor_tensor(out=t3, in0=b, in1=cs, op=ALU.mult),
                     reads=[pacc_b[b_i], tab_b], writes=[tmp_b[3]])
                P.op("pool", lambda e: e.tensor_tensor(out=ob[o1][0:M, :], in0=t0, in1=t1, op=ALU.subtract),
                     reads=[tmp_b[0], tmp_b[1]], writes=[ob_b[o1]])
                P.op("pool", lambda e: e.tensor_tensor(out=ob[o2][0:M, :], in0=t2, in1=t3, op=ALU.add),
                     reads=[tmp_b[2], tmp_b[3]], writes=[ob_b[o2]])
            else:
                P.op("act", lambda e: e.copy(out=ob[o1][0:M, :], in_=a), reads=[pacc_b[a_i]], writes=[ob_b[o1]])
                P.op("dve", lambda e: e.tensor_copy(out=ob[o2][0:M, :], in_=b), reads=[pacc_b[b_i]],
                     writes=[ob_b[o2]])
            dst = T[u["dst"]]
            for rows, oidx in ((u["rowsA"], o1), (u["rowsB"], o2)):
                for (r0, nr, p0) in rows:
                    P.dma("sp", dst[r0:r0 + nr, ts], ob[oidx][p0:p0 + nr, :], reads=[ob_b[oidx]])
    for u in TM:
        wb = wi_ % 2
        wi_ += 1
        W = u["width"]
        src = bass.AP(tensor=wt, offset=win.offset + u["col0"], ap=[[IN_WIDTH, 128], [128 * IN_WIDTH, 16], [1, W]])
        P.dma("pool", wbuf[wb][:, :, 0:W], src, writes=[wbuf_b[wb]])
        dst = T[u["dst"]]
        for j in range(NSLOT):
            pi = pa % 4
            pa += 1
            for kt in range(16):
                P.op("pe", lambda e: e.matmul(out=pacc[pi][:, 0:W], lhsT=nt.hT[:, kt, 128 * j:128 * (j + 1)],
                                              rhs=wbuf[wb][:, kt, 0:W], start=(kt == 0), stop=(kt == 15)),
                     reads=[wbuf_b[wb], nt.hT_b[j]], writes=[pacc_b[pi]])
            if u["name"] == "w_i":
                o1 = j % 2
                P.op("dve", lambda e: e.tensor_copy(out=obf[o1][:, 0:W], in_=pacc[pi][:, 0:W]), reads=[pacc_b[pi]],
                     writes=[obf_b[o1]])
                P.dma("sp", dst[128 * j:128 * (j + 1), :], obf[o1][:, 0:W], reads=[obf_b[o1]])
            else:
                o1 = oi % 4
                oi += 1
                if j % 2 == 0:
                    P.op("act", lambda e: e.copy(out=ob[o1][:, 0:W], in_=pacc[pi][:, 0:W]), reads=[pacc_b[pi]],
                         writes=[ob_b[o1]])
                else:
                    P.op("dve", lambda e: e.tensor_copy(out=ob[o1][:, 0:W], in_=pacc[pi][:, 0:W]),
                         reads=[pacc_b[pi]], writes=[ob_b[o1]])
                P.dma("sp", dst[128 * j:128 * (j + 1), u["dcol"]:u["dcol"] + W], ob[o1][:, 0:W], reads=[ob_b[o1]])


def build_A():
    nc = bass.Bass("TRN2", target_bir_lowering=False)
    perm, FM, TM = win_units()
    T = {}

    def din(name, shape, dt=F32):
        T[name] = nc.dram_tensor(name, list(shape), dt, kind="ExternalInput").ap()

    def dout(name, shape, dt=BF16):
        T[name] = nc.dram_tensor(name, list(shape), dt, kind="ExternalOutput").ap()

    din("x", [TPC, D])
    din("g", [1, D])
    din("win", [D, IN_WIDTH])
    din("ident", [128, 128])
    for nm in ("cosA", "sinA", "cosC", "sinC"):
        din(nm, [128, TPC])
    dout("qaT", [512, TPC]); dout("kaT", [512, TPC]); dout("qiT", [512, TPC]); dout("kiT", [64, TPC])
    dout("qcT", [1024, TPC]); dout("kcT", [1024, TPC]); dout("uT", [512, TPC])
    dout("va", [TPC, 512]); dout("vc", [TPC, 1024]); dout("wi", [TPC, 8], F32)
    P = Prog(nc)
    P.begin_phase("A")
    emit_phase_A(P, T, FM, TM)
    P.end_phase()
    P.finish()
    return nc, perm


def w_src(w_ap, ld, row0, KT, col0, width):
    return bass.AP(tensor=w_ap.tensor, offset=w_ap.offset + row0 * ld + col0, ap=[[ld, 128], [128 * ld, KT], [1, width]])


class WStream:
    def __init__(self, P, tag, nbuf=2):
        self.P = P
        self.buf = [P.sb(f"{tag}_w{i}", [128, 16, 512], BF16) for i in range(nbuf)]
        self.b = [Buf(f"{tag}_w{i}") for i in range(nbuf)]
        self.i = 0
        self.n = nbuf

    def load(self, w_ap, ld, row0, KT, col0, width):
        i = self.i % self.n
        self.i += 1
        self.P.dma("pool", self.buf[i][:, 0:KT, 0:width], w_src(w_ap, ld, row0, KT, col0, width), writes=[self.b[i]])
        return self.buf[i], self.b[i]


def alloc_xr(P, tag):
    return ([P.sb(f"{tag}_xr{i}", [128, 512], F32) for i in range(3)], [Buf(f"{tag}_xr{i}") for i in range(3)])


def emit_resid_proj(P, lhsT, lhsT_b, KT_total, w_ap, x_in, x_out, ws, pacc, pacc_b, xrs, ntok=TPC):
    nslot = ntok // 128
    xr, xr_b = xrs
    xi = 0
    pa = 0
    nchunk = KT_total // 16
    for nb in range(4):
        if nchunk == 1:
            wt, wb = ws.load(w_ap, D, 0, 16, 512 * nb, 512)
            for j in range(nslot):
                pi = pa % len(pacc)
                pa += 1
                lb = lhsT_b[j] if isinstance(lhsT_b, list) else lhsT_b
                for kt in range(16):
                    P.op("pe", lambda e: e.matmul(out=pacc[pi][:], lhsT=lhsT[:, kt, 128 * j:128 * (j + 1)],
                                                  rhs=wt[:, kt, :], start=(kt == 0), stop=(kt == 15)),
                         reads=[wb, lb], writes=[pacc_b[pi]])
                k = xi % 3
                xi += 1
                P.dma("sp", xr[k][:], x_in[128 * j:128 * (j + 1), 512 * nb:512 * (nb + 1)], writes=[xr_b[k]])
                P.op("dve", lambda e: e.tensor_tensor(out=xr[k][:], in0=pacc[pi][:], in1=xr[k][:], op=ALU.add),
                     reads=[pacc_b[pi], xr_b[k]], writes=[xr_b[k]])
                P.dma("sp", x_out[128 * j:128 * (j + 1), 512 * nb:512 * (nb + 1)], xr[k][:], reads=[xr_b[k]])
        else:
            assert nslot <= len(pacc)
            for ch in range(nchunk):
                wt, wb = ws.load(w_ap, D, 2048 * ch, 16, 512 * nb, 512)
                for j in range(nslot):
                    lb = lhsT_b[j] if isinstance(lhsT_b, list) else lhsT_b
                    for kt in range(16):
                        P.op("pe", lambda e: e.matmul(out=pacc[j][:], lhsT=lhsT[:, 16 * ch + kt, 128 * j:128 * (j + 1)],
                                                      rhs=wt[:, kt, :], start=(ch == 0 and kt == 0),
                                                      stop=(ch == nchunk - 1 and kt == 15)),
                             reads=[wb, lb], writes=[pacc_b[j]])
            for j in range(nslot):
                k = xi % 3
                xi += 1
                P.dma("sp", xr[k][:], x_in[128 * j:128 * (j + 1), 512 * nb:512 * (nb + 1)], writes=[xr_b[k]])
                P.op("dve", lambda e: e.tensor_tensor(out=xr[k][:], in0=pacc[j][:], in1=xr[k][:], op=ALU.add),
                     reads=[pacc_b[j], xr_b[k]], writes=[xr_b[k]])
                P.dma("sp", x_out[128 * j:128 * (j + 1), 512 * nb:512 * (nb + 1)], xr[k][:], reads=[xr_b[k]])


def emit_phase_O(P, T):
    cat = P.sb("O_cat", [128, 16, TPC], BF16)
    cat_b = Buf("O_cat")
    for kt in range(16):
        P.dma("sp", cat[:, kt, :], T["catT"][128 * kt:128 * (kt + 1), :], writes=[cat_b])
    ws = WStream(P, "O")
    pacc = [P.ps(f"O_acc{i}", [128, 512], F32) for i in range(4)]
    pacc_b = [Buf(f"O_acc{i}") for i in range(4)]
    emit_resid_proj(P, cat, cat_b, 16, T["wout"], T["x"], T["x1"], ws, pacc, pacc_b, alloc_xr(P, "O"))


def emit_phase_X(P, T):
    C = make_consts(P, T["ident"])
    ones = P.sb("X_ones", [128, 128], BF16)
    ones_b = Buf("X_ones")
    P.op("dve", lambda e: e.memset(ones[:], 1.0), writes=[ones_b])
    nt = NormT(P, TPC, "X")
    memT = P.sb("X_memT", [128, 16, 256], BF16)
    memT_b = [Buf("X_memT0"), Buf("X_memT1")]
    nt.run(T["x1"], T["gx"], C)
    nt.run(T["mem"], T["gm"], C, hT=memT, hT_b=memT_b, ntok=256)
    hT, hT_b = nt.hT, nt.hT_b

    ws = WStream(P, "X")
    pacc = [P.ps(f"X_acc{i}", [128, 512], F32) for i in range(6)]
    pacc_b = [Buf(f"X_acc{i}") for i in range(6)]
    pa = 0
    kmT = P.sb("X_kmT", [128, 16, 256], BF16)
    kmT_b = Buf("X_kmT")
    vm = P.sb("X_vm", [128, 2, D], BF16)
    vm_b = Buf("X_vm")
    for nb4 in range(4):
        wt, wb = ws.load(T["wk"], D, 0, 16, 512 * nb4, 512)
        for r in range(4):
            pi = pa % 6
            pa += 1
            for kt in range(16):
                P.op("pe", lambda e: e.matmul(out=pacc[pi][:, 0:256], lhsT=wt[:, kt, 128 * r:128 * (r + 1)],
                                              rhs=memT[:, kt, :], start=(kt == 0), stop=(kt == 15)),
                     reads=[wb] + memT_b, writes=[pacc_b[pi]])
            P.op("act", lambda e: e.copy(out=kmT[:, 4 * nb4 + r, :], in_=pacc[pi][:, 0:256]), reads=[pacc_b[pi]],
                 writes=[kmT_b])
    for nb4 in range(4):
        wt, wb = ws.load(T["wv"], D, 0, 16, 512 * nb4, 512)
        for mt in range(2):
            pi = pa % 6
            pa += 1
            for kt in range(16):
                P.op("pe", lambda e: e.matmul(out=pacc[pi][:], lhsT=memT[:, kt, 128 * mt:128 * (mt + 1)],
                                              rhs=wt[:, kt, :], start=(kt == 0), stop=(kt == 15)),
                     reads=[wb, memT_b[mt]], writes=[pacc_b[pi]])
            P.op("dve", lambda e: e.tensor_copy(out=vm[:, mt, 512 * nb4:512 * (nb4 + 1)], in_=pacc[pi][:]),
                 reads=[pacc_b[pi]], writes=[vm_b])
    oT = hT
    qT = [P.sb(f"X_qT{i}", [128, 16, 512], BF16) for i in range(1)]
    qT_b = [Buf(f"X_qT{i}") for i in range(1)]
    pT = [P.sb(f"X_pT{i}", [128, 2, 512], BF16) for i in range(2)]
    pT_b = [Buf(f"X_pT{i}") for i in range(2)]
    rs = [P.sb(f"X_rs{i}", [128, 512], F32) for i in range(2)]
    rs_b = [Buf(f"X_rs{i}") for i in range(2)]
    sc = 1.0 / math.sqrt(512.0)
    for tg in range(4):
        ts = slice(512 * tg, 512 * (tg + 1))
        for nb4 in range(4):
            wt, wb = ws.load(T["wq"], D, 0, 16, 512 * nb4, 512)
            for r in range(4):
                pi = pa % 6
                pa += 1
                for kt in range(16):
                    P.op("pe", lambda e: e.matmul(out=pacc[pi][:], lhsT=wt[:, kt, 128 * r:128 * (r + 1)],
                                                  rhs=hT[:, kt, ts], start=(kt == 0), stop=(kt == 15)),
                         reads=[wb] + hT_b[4 * tg:4 * tg + 4], writes=[pacc_b[pi]])
                if r % 2 == 0:
                    P.op("act", lambda e: e.copy(out=qT[0][:, 4 * nb4 + r, :], in_=pacc[pi][:]), reads=[pacc_b[pi]],
                         writes=[qT_b[0]])
                else:
                    P.op("dve", lambda e: e.tensor_copy(out=qT[0][:, 4 * nb4 + r, :], in_=pacc[pi][:]),
                         reads=[pacc_b[pi]], writes=[qT_b[0]])
        for hx in range(4):
            pb = hx % 2
            for mt in range(2):
                pi = pa % 6
                pa += 1
                for dt_ in range(4):
                    P.op("pe", lambda e: e.matmul(out=pacc[pi][:], lhsT=kmT[:, 4 * hx + dt_, 128 * mt:128 * (mt + 1)],
                                                  rhs=qT[0][:, 4 * hx + dt_, :], start=(dt_ == 0), stop=(dt_ == 3)),
                         reads=[kmT_b, qT_b[0]], writes=[pacc_b[pi]])
                P.op("act", lambda e: e.activation(out=pT[pb][:, mt, :], in_=pacc[pi][:], func=AF.Exp, scale=sc),
                     reads=[pacc_b[pi]], writes=[pT_b[pb]])
            pi = pa % 6
            pa += 1
            for mt in range(2):
                P.op("pe", lambda e: e.matmul(out=pacc[pi][:], lhsT=ones[:], rhs=pT[pb][:, mt, :], start=(mt == 0),
                                              stop=(mt == 1)), reads=[ones_b, pT_b[pb]], writes=[pacc_b[pi]])
            P.op("dve", lambda e: e.reciprocal(out=rs[pb][:], in_=pacc[pi][:]), reads=[pacc_b[pi]], writes=[rs_b[pb]])
            for dvt in range(4):
                pi = pa % 6
                pa += 1
                for mt in range(2):
                    c0 = 512 * hx + 128 * dvt
                    P.op("pe", lambda e: e.matmul(out=pacc[pi][:], lhsT=vm[:, mt, c0:c0 + 128], rhs=pT[pb][:, mt, :],
                                                  start=(mt == 0), stop=(mt == 1)),
                         reads=[vm_b, pT_b[pb]], writes=[pacc_b[pi]])
                P.op("dve", lambda e: e.tensor_tensor(out=oT[:, 4 * hx + dvt, ts], in0=pacc[pi][:], in1=rs[pb][:],
                                                      op=ALU.mult), reads=[pacc_b[pi], rs_b[pb]],
                     writes=hT_b[4 * tg:4 * tg + 4])
    emit_resid_proj(P, oT, hT_b, 16, T["wo"], T["x1"], T["x2"], ws, pacc[0:4], pacc_b[0:4], alloc_xr(P, "X"))


def emit_phase_M(P, T):
    C = make_consts(P, T["ident"])
    nt = NormT(P, TPC, "M")
    nt.run(T["x2"], T["gmlp"], C)
    hT, hT_b = nt.hT, nt.hT_b
    ws = WStream(P, "M")
    pacc = [P.ps(f"M_acc{i}", [128, 512], F32) for i in range(6)]
    pacc_b = [Buf(f"M_acc{i}") for i in range(6)]
    hid = P.sb("M_hid", [128, 64, 512], BF16)
    hid_b = Buf("M_hid")
    rl = [P.sb(f"M_rl{i}", [128, 512], F32) for i in range(3)]
    rl_b = [Buf(f"M_rl{i}") for i in range(3)]
    pa = 0
    ri = 0
    xrs = alloc_xr(P, "M")
    for tg in range(4):
        ts = slice(512 * tg, 512 * (tg + 1))
        for fb4 in range(16):
            wt, wb = ws.load(T["w1"], 8192, 0, 16, 512 * fb4, 512)
            for r in range(4):
                pi = 4 + (pa % 2)
                pa += 1
                for kt in range(16):
                    P.op("pe", lambda e: e.matmul(out=pacc[pi][:], lhsT=wt[:, kt, 128 * r:128 * (r + 1)],
                                                  rhs=hT[:, kt, ts], start=(kt == 0), stop=(kt == 15)),
                         reads=[wb] + hT_b[4 * tg:4 * tg + 4], writes=[pacc_b[pi]])
                k = ri % 3
                ri += 1
                P.op("act", lambda e: e.activation(out=rl[k][:], in_=pacc[pi][:], func=AF.Relu), reads=[pacc_b[pi]],
                     writes=[rl_b[k]])
                P.op("pool", lambda e: e.tensor_tensor(out=hid[:, 4 * fb4 + r, :], in0=rl[k][:], in1=rl[k][:],
                                                       op=ALU.mult), reads=[rl_b[k]], writes=[hid_b])
        emit_resid_proj(P, hid, hid_b, 64, T["w2"], T["x2"][512 * tg:512 * (tg + 1), :],
                        T["x3"][512 * tg:512 * (tg + 1), :], ws, pacc[0:4], pacc_b[0:4], xrs, ntok=512)


def emit_phase_F(P, T):
    xs = [P.sb(f"F_xs{i}", [128, D], F32) for i in range(2)]
    xs_b = [Buf(f"F_xs{i}") for i in range(2)]
    ys = [P.sb(f"F_ys{i}", [128, D], F32) for i in range(2)]
    ys_b = [Buf(f"F_ys{i}") for i in range(2)]
    junk = P.sb("F_junk", [128, D], BF16)
    junk_b = Buf("F_junk")
    gb = P.sb("F_gb", [128, D], F32)
    gb_b = Buf("F_gb")
    st = [P.sb(f"F_st{i}", [128, 4], F32) for i in range(2)]
    st_b = [Buf(f"F_st{i}") for i in range(2)]
    epsb = P.sb("F_eps", [128, 1], F32)
    epsb_b = Buf("F_eps")
    P.op("dve", lambda e: e.memset(epsb[:], EPS), writes=[epsb_b])
    g = T["gf"]
    P.dma("sp", gb[:], bass.AP(tensor=g.tensor, offset=g.offset, ap=[[0, 128], [1, D]]), writes=[gb_b])
    for j in range(NSLOT):
        i = j % 2
        P.dma("sp", xs[i][:], T["x3"][128 * j:128 * (j + 1), :], writes=[xs_b[i]])
        P.op("act", lambda e: e.activation(out=junk[:], in_=xs[i][:], func=AF.Square, accum_out=st[i][:, 0:1]),
             reads=[xs_b[i]], writes=[junk_b, st_b[i]])
        P.op("act", lambda e: e.activation(out=st[i][:, 1:2], in_=st[i][:, 0:1], func=AF.Sqrt, scale=1.0 / D,
                                           bias=epsb[:, 0:1]), reads=[st_b[i], epsb_b], writes=[st_b[i]])
        P.op("dve", lambda e: e.reciprocal(out=st[i][:, 2:3], in_=st[i][:, 1:2]), reads=[st_b[i]], writes=[st_b[i]])
        P.op("dve", lambda e: e.scalar_tensor_tensor(out=ys[i][:], in0=xs[i][:], scalar=st[i][:, 2:3], in1=gb[:],
                                                     op0=ALU.mult, op1=ALU.mult),
             reads=[xs_b[i], st_b[i], gb_b], writes=[ys_b[i]])
        P.dma("sp", T["y"][128 * j:128 * (j + 1), :], ys[i][:], reads=[ys_b[i]])


def build_OXM(final=False):
    nc = bass.Bass("TRN2", target_bir_lowering=False)
    T = {}

    def din(name, shape, dt=F32):
        T[name] = nc.dram_tensor(name, list(shape), dt, kind="ExternalInput").ap()

    def dout(name, shape, dt=F32):
        T[name] = nc.dram_tensor(name, list(shape), dt, kind="ExternalOutput").ap()

    def dint(name, shape, dt=F32):
        T[name] = nc.dram_tensor(name, list(shape), dt, kind="Internal").ap()

    din("x", [TPC, D]); din("catT", [D, TPC], BF16); din("wout", [D, D]); din("ident", [128, 128])
    din("mem", [256, D]); din("gx", [1, D]); din("gm", [1, D])
    for w in ("wq", "wk", "wv", "wo"):
        din(w, [D, D])
    din("gmlp", [1, D]); din("w1", [D, 8192]); din("w2", [8192, D])
    dint("x1", [TPC, D]); dint("x2", [TPC, D])
    if final:
        dint("x3", [TPC, D]); din("gf", [1, D]); dout("y", [TPC, D])
    else:
        dout("x3", [TPC, D])
    P = Prog(nc)
    for nm, fn in (("O", emit_phase_O), ("X", emit_phase_X), ("M", emit_phase_M)) + ((("F", emit_phase_F),) if final else ()):
        P.begin_phase(nm)
        fn(P, T)
        P.end_phase()
    P.finish()
    return nc


MASK_NEG = -30000.0


def mask_rows_q(c):
    qm = np.zeros((64, TPC), np.float32)
    for j in range(NSLOT):
        g = j // 4
        for half in range(2):
            qc = 2 * blk_of(c, j) + half - 64 * g
            cols = slice(128 * j + 64 * half, 128 * j + 64 * half + 64)
            qm[qc + 1:, cols] = MASK_NEG
    return qm


def mask_rows_k():
    km = np.zeros((NCORES, 64, TPC), np.float32)
    for r in range(NCORES):
        for j in range(NSLOT):
            g = j // 4
            for half in range(2):
                kc = 2 * blk_of(r, j) + half - 64 * g
                km[r, kc, 128 * j + 64 * half:128 * j + 64 * half + 64] = 1.0
    return km.reshape(NCORES * 64, TPC)


def emit_phase_C(P, T, groups=range(4), heads=range(8)):
    sc = 1.0 / 8.0
    ones = P.sb("C_ones", [128, 128], BF16)
    ones_b = Buf("C_ones")
    P.op("dve", lambda e: e.memset(ones[:], 1.0), writes=[ones_b])
    epsb = P.sb("C_eps", [128, 1], F32)
    epsb_b = Buf("C_eps")
    P.op("dve", lambda e: e.memset(epsb[:], EPS), writes=[epsb_b])
    lv = P.sb("C_lv", [128, 4, 64], F32)
    lv_b = Buf("C_lv")
    lamv = T["lamv"]
    P.dma("sp", lv[:], bass.AP(tensor=lamv.tensor, offset=lamv.offset, ap=[[0, 128], [64, 4], [1, 64]]), writes=[lv_b])
    lc = P.sb("C_lc", [128, 2], F32)
    lc_b = Buf("C_lc")
    lamc = T["lamc"]
    P.dma("sp", lc[:], bass.AP(tensor=lamc.tensor, offset=lamc.offset, ap=[[0, 128], [1, 2]]), writes=[lc_b])
    sg = P.sb("C_sg", [128, 1], F32)
    sg_b = Buf("C_sg")
    P.dma("sp", sg[:], T["subln"], writes=[sg_b])
    lw = P.sb("C_lw", [128, 8], F32)
    lw_b = Buf("C_lw")
    lj = P.sb("C_lj", [128, 64], F32)
    lj_b = Buf("C_lj")
    for i in range(2):
        P.op("dve", lambda e: e.tensor_tensor(out=lj[:], in0=lv[:, 2 * i, :], in1=lv[:, 2 * i + 1, :], op=ALU.mult),
             reads=[lv_b], writes=[lj_b])
        P.op("dve", lambda e: e.tensor_reduce(out=lw[:, i:i + 1], in_=lj[:], axis=AX.X, op=ALU.add),
             reads=[lj_b], writes=[lw_b])
    P.op("act", lambda e: e.activation(out=lw[:, 2:4], in_=lw[:, 0:2], func=AF.Exp), reads=[lw_b], writes=[lw_b])
    P.op("dve", lambda e: e.tensor_tensor(out=lw[:, 4:5], in0=lw[:, 2:3], in1=lw[:, 3:4], op=ALU.subtract),
         reads=[lw_b], writes=[lw_b])
    P.op("dve", lambda e: e.tensor_tensor(out=lw[:, 5:6], in0=lw[:, 4:5], in1=lc[:, 0:1], op=ALU.add),
         reads=[lw_b, lc_b], writes=[lw_b])
    P.op("dve", lambda e: e.tensor_scalar(out=lw[:, 6:7], in0=lw[:, 5:6], scalar1=-1.0, scalar2=None, op0=ALU.mult),
         reads=[lw_b], writes=[lw_b])
    P.op("dve", lambda e: e.tensor_tensor(out=lw[:, 7:8], in0=sg[:], in1=lc[:, 1:2], op=ALU.mult),
         reads=[sg_b, lc_b], writes=[lw_b])
    neglam = lw[:, 6:7]
    gsc = lw[:, 7:8]

    QT = [P.sb(f"C_QT{i}", [128, 512], BF16) for i in range(2)]
    QT_b = [Buf(f"C_QT{i}") for i in range(2)]
    QB = [[P.sb(f"C_QB{c}{i}", [128, 512], BF16) for i in range(2)] for c in range(2)]
    QB_b = [[Buf(f"C_QB{c}{i}") for i in range(2)] for c in range(2)]
    KT = [P.sb(f"C_KT{i}", [128, 8, 1536], BF16) for i in range(2)]
    KT_b = [Buf(f"C_KT{i}") for i in range(2)]
    KB = [[P.sb(f"C_KB{c}{i}", [128, 8, 512], BF16) for i in range(2)] for c in range(2)]
    KB_b = [[Buf(f"C_KB{c}{i}") for i in range(2)] for c in range(2)]
    VT = [P.sb(f"C_VT{i}", [128, 8, 16, 128], BF16) for i in range(2)]
    VT_b = [Buf(f"C_VT{i}") for i in range(2)]
    NPT = 4
    PT = [P.sb(f"C_PT{i}", [128, 512], BF16) for i in range(NPT)]
    PT_b = [Buf(f"C_PT{i}") for i in range(NPT)]
    sps = [P.ps(f"C_S{i}", [128, 512], F32) for i in range(3)]
    sps_b = [Buf(f"C_S{i}") for i in range(3)]
    oacc = [P.ps(f"C_O{i}", [128, 512], F32) for i in range(2)]
    oacc_b = [Buf(f"C_O{i}") for i in range(2)]
    sacc = [P.ps(f"C_Z{i}", [128, 512], F32) for i in range(2)]
    sacc_b = [Buf(f"C_Z{i}") for i in range(2)]
    lnp = P.ps("C_ln", [128, 512], F32)
    lnp_b = Buf("C_ln")
    rs = P.sb("C_rs", [128, 512], F32)
    rs_b = Buf("C_rs")
    on0 = P.sb("C_on0", [128, 512], F32)
    on0_b = Buf("C_on0")
    od = P.sb("C_od", [128, 512], F32)
    od_b = Buf("C_od")
    sq = P.sb("C_sq", [128, 512], BF16)
    sq_b = Buf("C_sq")
    fin = [P.sb(f"C_fin{i}", [128, 512], BF16) for i in range(2)]
    fin_b = [Buf(f"C_fin{i}") for i in range(2)]

    qcT, kcT, vc, qm, km = T["qcT"], T["kcT_g"], T["vc_g"], T["qm"], T["km_g"]
    items = [(g, h) for g in groups for h in heads]

    def load(idx):
        g, h = items[idx]
        b = idx % 2
        qs = slice(512 * g, 512 * (g + 1))
        first_of_g = (idx == 0) or (items[idx - 1][0] != g)
        second_of_g = (idx >= 1 and items[idx - 1][0] == g) and (idx == 1 or items[idx - 2][0] != g)
        P.dma("sp", QT[b][:], qcT[128 * h:128 * (h + 1), qs], writes=[QT_b[b]])
        for c in range(2):
            P.dma("sp", QB[c][b][0:64, :], qcT[128 * h + 64 * c:128 * h + 64 * c + 64, qs], writes=[QB_b[c][b]])
            src = bass.AP(tensor=kcT.tensor, offset=kcT.offset + (128 * h + 64 * c) * TPC + 512 * g,
                          ap=[[TPC, 64], [1024 * TPC, 8], [1, 512]])
            P.dma("sp", KB[c][b][0:64, :, :], src, writes=[KB_b[c][b]])
            if first_of_g or second_of_g:
                P.dma("sp", QB[c][b][64:128, :], qm[:, qs], writes=[QB_b[c][b]])
                srcm = bass.AP(tensor=km.tensor, offset=km.offset + 512 * g, ap=[[TPC, 64], [64 * TPC, 8], [1, 512]])
                P.dma("sp", KB[c][b][64:128, :, :], srcm, writes=[KB_b[c][b]])
        if g > 0:
            for r in range(8):
                P.dma("sp", KT[b][:, r, 0:512 * g], kcT[r * 1024 + 128 * h:r * 1024 + 128 * (h + 1), 0:512 * g],
                      writes=[KT_b[b]])
        nj = 4 * g + 4
        for r in range(8):
            src = bass.AP(tensor=vc.tensor, offset=vc.offset + r * TPC * 1024 + 128 * h,
                          ap=[[1024, 128], [128 * 1024, nj], [1, 128]])
            P.dma("sp", VT[b][:, r, 0:nj, :], src, writes=[VT_b[b]])

    state = dict(si=0, pi=0, fi=0)

    def compute(idx):
        g, h = items[idx]
        b = idx % 2
        tiles = []
        for c in range(2):
            for r in range(8):
                for j in range(4 * g):
                    tiles.append((c, r, j, False))
                for jb in range(4):
                    tiles.append((c, r, 4 * g + jb, True))
        nt_c = len(tiles) // 2

        def s_mm(t):
            c, r, j, bnd = tiles[t]
            s_i = (state["si"] + t) % 3
            if bnd:
                jb = j - 4 * g
                P.op("pe", lambda e: e.matmul(out=sps[s_i][:], lhsT=KB[c][b][:, r, 128 * jb:128 * (jb + 1)],
                                              rhs=QB[c][b][:, :], start=True, stop=True),
                     reads=[KB_b[c][b], QB_b[c][b]], writes=[sps_b[s_i]])
            else:
                P.op("pe", lambda e: e.matmul(out=sps[s_i][:], lhsT=KT[b][64 * c:64 * c + 64, r, 128 * j:128 * (j + 1)],
                                              rhs=QT[b][64 * c:64 * c + 64, :], start=True, stop=True),
                     reads=[KT_b[b], QT_b[b]], writes=[sps_b[s_i]])

        def epilogue(c):
            P.op("dve", lambda e: e.reciprocal(out=rs[:], in_=sacc[c][:]), reads=[sacc_b[c]], writes=[rs_b])
            if c == 0:
                P.op("dve", lambda e: e.tensor_tensor(out=on0[:], in0=oacc[0][:], in1=rs[:], op=ALU.mult),
                     reads=[oacc_b[0], rs_b], writes=[on0_b])
                return
            P.op("dve", lambda e: e.tensor_tensor(out=od[:], in0=oacc[1][:], in1=rs[:], op=ALU.mult),
                 reads=[oacc_b[1], rs_b], writes=[od_b])
            P.op("dve", lambda e: e.scalar_tensor_tensor(out=od[:], in0=od[:], scalar=neglam, in1=on0[:],
                                                         op0=ALU.mult, op1=ALU.add),
                 reads=[od_b, on0_b, lw_b], writes=[od_b])
            P.op("act", lambda e: e.activation(out=sq[:], in_=od[:], func=AF.Square), reads=[od_b], writes=[sq_b])
            P.op("pe", lambda e: e.matmul(out=lnp[:], lhsT=ones[:], rhs=sq[:], start=True, stop=True),
                 reads=[ones_b, sq_b], writes=[lnp_b])
            P.op("act", lambda e: e.activation(out=rs[:], in_=lnp[:], func=AF.Sqrt, scale=1.0 / 128.0,
                                               bias=epsb[:, 0:1]), reads=[lnp_b, epsb_b], writes=[rs_b])
            P.op("dve", lambda e: e.reciprocal(out=rs[:], in_=rs[:]), reads=[rs_b], writes=[rs_b])
            f = state["fi"] % 2
            state["fi"] += 1
            P.op("dve", lambda e: e.scalar_tensor_tensor(out=fin[f][:], in0=od[:], scalar=gsc, in1=rs[:],
                                                         op0=ALU.mult, op1=ALU.mult),
                 reads=[od_b, rs_b, lw_b], writes=[fin_b[f]])
            P.dma("sp", T["catT"][1024 + 128 * h:1024 + 128 * (h + 1), 512 * g:512 * (g + 1)], fin[f][:],
                  reads=[fin_b[f]])

        s_mm(0)
        for t in range(len(tiles)):
            c, r, j, bnd = tiles[t]
            if t + 1 < len(tiles):
                s_mm(t + 1)
            s_i = (state["si"] + t) % 3
            p_i = (state["pi"] + t) % NPT
            P.op("act", lambda e: e.activation(out=PT[p_i][:], in_=sps[s_i][:], func=AF.Exp, scale=sc),
                 reads=[sps_b[s_i]], writes=[PT_b[p_i]])
            tc = t % nt_c
            P.op("pe", lambda e: e.matmul(out=oacc[c][:], lhsT=VT[b][:, r, j, :], rhs=PT[p_i][:], start=(tc == 0),
                                          stop=(tc == nt_c - 1)),
                 reads=[VT_b[b], PT_b[p_i]], writes=[oacc_b[c]])
            P.op("pe", lambda e: e.matmul(out=sacc[c][:], lhsT=ones[:], rhs=PT[p_i][:], start=(tc == 0),
                                          stop=(tc == nt_c - 1)),
                 reads=[ones_b, PT_b[p_i]], writes=[sacc_b[c]])
            if tc == nt_c - 1:
                epilogue(c)
        state["si"] += len(tiles)
        state["pi"] += len(tiles)

    load(0)
    for idx in range(len(items)):
        if idx + 1 < len(items):
            load(idx + 1)
        compute(idx)


def build_C(groups=range(4), heads=range(8)):
    nc = bass.Bass("TRN2", target_bir_lowering=False)
    T = {}

    def din(name, shape, dt=F32):
        T[name] = nc.dram_tensor(name, list(shape), dt, kind="ExternalInput").ap()

    def dout(name, shape, dt=F32):
        T[name] = nc.dram_tensor(name, list(shape), dt, kind="ExternalOutput").ap()

    din("qcT", [1024, TPC], BF16); din("kcT_g", [8 * 1024, TPC], BF16); din("vc_g", [8 * TPC, 1024], BF16)
    din("qm", [64, TPC], BF16); din("km_g", [8 * 64, TPC], BF16)
    din("lamv", [4, 64]); din("lamc", [1, 2]); din("subln", [128, 1])
    dout("catT", [D, TPC], BF16)
    P = Prog(nc)
    P.begin_phase("C")
    emit_phase_C(P, T, groups, heads)
    P.end_phase()
    P.finish()
    return nc


def s5_host_layout(a_re, a_im, log_dt, b_re, b_im, c_re, c_im, dsk, w_glu, b_glu):
    def st(a):
        return np.ascontiguousarray(a.reshape(16, 2, 64).transpose(1, 2, 0).reshape(128, 16)).astype(np.float32)
    out = dict(s5_are=st(a_re), s5_aim=st(a_im), s5_ldt=st(np.repeat(log_dt[:, None], 64, axis=1)))
    def bl(b):
        return np.ascontiguousarray(b.reshape(16, 2, 64, 16).transpose(1, 2, 0, 3).reshape(128, 16, 16)).astype(np.float32)
    out["s5_bre"] = bl(b_re)
    out["s5_bim"] = bl(b_im)
    out["s5_cre"] = bl(c_re.transpose(0, 2, 1))
    out["s5_cim"] = bl(c_im.transpose(0, 2, 1))
    out["s5_d"] = np.ascontiguousarray(dsk.reshape(4, 128).T).astype(np.float32)
    out["s5_wglu"] = np.ascontiguousarray(w_glu).astype(np.float32)
    out["s5_bglu"] = np.ascontiguousarray(b_glu.reshape(4, 128).T).astype(np.float32)
    return out


S5_IN = dict(s5_are=[128, 16], s5_aim=[128, 16], s5_ldt=[128, 16], s5_bre=[128, 16, 16], s5_bim=[128, 16, 16],
             s5_cre=[128, 16, 16], s5_cim=[128, 16, 16], s5_d=[128, 4], s5_wglu=[512, 512], s5_bglu=[128, 4])


def cplx_mul(P, eng, out_r, out_i, a_r, a_i, b_r, b_i, tmp, reads, writes, tmp_b):
    t0, t1 = tmp
    P.op(eng, lambda e: e.tensor_tensor(out=t0, in0=a_r, in1=b_r, op=ALU.mult), reads=reads, writes=[tmp_b])
    P.op(eng, lambda e: e.tensor_tensor(out=t1, in0=a_i, in1=b_i, op=ALU.mult), reads=reads, writes=[tmp_b])
    P.op(eng, lambda e: e.tensor_tensor(out=out_r, in0=t0, in1=t1, op=ALU.subtract), reads=[tmp_b], writes=writes)
    P.op(eng, lambda e: e.tensor_tensor(out=t0, in0=a_r, in1=b_i, op=ALU.mult), reads=reads + writes, writes=[tmp_b])
    P.op(eng, lambda e: e.tensor_tensor(out=t1, in0=a_i, in1=b_r, op=ALU.mult), reads=reads + writes, writes=[tmp_b])
    P.op(eng, lambda e: e.tensor_tensor(out=out_i, in0=t0, in1=t1, op=ALU.add), reads=[tmp_b], writes=writes)


def s5_prep(P, T, C, need_rev, need_fwd):
    R = {}
    prm = Buf("s5prm")
    R["prm"] = prm

    def ld(name, shape):
        t = P.sb("S_" + name, shape, F32)
        P.dma("sp", t[:], T[name], writes=[prm])
        return t
    are, aim, ldt = ld("s5_are", [128, 16]), ld("s5_aim", [128, 16]), ld("s5_ldt", [128, 16])
    bre, bim = ld("s5_bre", [128, 16, 16]), ld("s5_bim", [128, 16, 16])
    w = P.sb("S_w", [128, 24, 16], F32)

    def W(i):
        return w[:, i, :]

    def tt(o, a, b, op):
        P.op("dve", lambda e: e.tensor_tensor(out=o, in0=a, in1=b, op=op), reads=[prm], writes=[prm])

    def ts(o, a, s1, s2, op0, op1=None):
        if op1 is None:
            P.op("dve", lambda e: e.tensor_scalar(out=o, in0=a, scalar1=s1, scalar2=None, op0=op0), reads=[prm], writes=[prm])
        else:
            P.op("dve", lambda e: e.tensor_scalar(out=o, in0=a, scalar1=s1, scalar2=s2, op0=op0, op1=op1), reads=[prm],
                 writes=[prm])

    def act(o, a, f, scale=1.0, bias=None):
        if bias is None:
            P.op("act", lambda e: e.activation(out=o, in_=a, func=f, scale=scale), reads=[prm], writes=[prm])
        else:
            P.op("act", lambda e: e.activation(out=o, in_=a, func=f, scale=scale, bias=bias), reads=[prm], writes=[prm])

    halfpi = P.sb("S_hpi", [128, 1], F32)
    P.op("dve", lambda e: e.memset(halfpi[:], math.pi / 2), writes=[prm])
    dt, ar, ai, mag, cs, sn = W(0), W(1), W(2), W(3), W(4), W(5)
    act(dt, ldt[:], AF.Exp)
    tt(ar, are[:], dt, ALU.mult)
    tt(ai, aim[:], dt, ALU.mult)
    act(mag, ar, AF.Exp)
    act(sn, ai, AF.Sin, scale=1.0 / 16.0)
    act(cs, ai, AF.Sin, scale=1.0 / 16.0, bias=halfpi[:, 0:1])
    for _ in range(4):
        tt(W(6), cs, cs, ALU.mult)
        tt(W(7), sn, sn, ALU.mult)
        tt(W(8), cs, sn, ALU.mult)
        tt(cs, W(6), W(7), ALU.subtract)
        ts(sn, W(8), 2.0, None, ALU.mult)
    lbr, lbi = W(9), W(10)
    tt(lbr, mag, cs, ALU.mult)
    tt(lbi, mag, sn, ALU.mult)
    den, fr, fi, lm1 = W(11), W(12), W(13), W(14)
    tt(W(6), are[:], are[:], ALU.mult)
    tt(W(7), aim[:], aim[:], ALU.mult)
    tt(den, W(6), W(7), ALU.add)
    P.op("dve", lambda e: e.reciprocal(out=den, in_=den), reads=[prm], writes=[prm])
    ts(lm1, lbr, -1.0, None, ALU.add)
    tt(W(6), lm1, are[:], ALU.mult)
    tt(W(7), lbi, aim[:], ALU.mult)
    tt(W(6), W(6), W(7), ALU.add)
    tt(fr, W(6), den, ALU.mult)
    tt(W(6), lbi, are[:], ALU.mult)
    tt(W(7), lm1, aim[:], ALU.mult)
    tt(W(6), W(6), W(7), ALU.subtract)
    tt(fi, W(6), den, ALU.mult)
    bbr = P.sb("S_bbr", [128, 16, 16], F32)
    bbi = P.sb("S_bbi", [128, 16, 16], F32)
    t2 = P.sb("S_t2", [128, 2, 16, 16], F32)

    def bc(a):
        return bass.AP(tensor=a.tensor, offset=a.offset, ap=[list(a.ap[0]), list(a.ap[1]), [0, 16]])
    tt(t2[:, 0], bre[:], bc(fr), ALU.mult)
    tt(t2[:, 1], bim[:], bc(fi), ALU.mult)
    tt(bbr[:], t2[:, 0], t2[:, 1], ALU.subtract)
    tt(t2[:, 0], bim[:], bc(fr), ALU.mult)
    tt(t2[:, 1], bre[:], bc(fi), ALU.mult)
    tt(bbi[:], t2[:, 0], t2[:, 1], ALU.add)

    def zpad(name, src, dtype, neg=False):
        z = P.sb("S_" + name, [128, 16, 128], dtype)
        P.op("dve", lambda e: e.memset(z[:], 0.0), reads=[prm], writes=[prm])
        for two in range(2):
            zt = z[64 * two:64 * two + 64, :, :]
            dst = bass.AP(tensor=zt.tensor, offset=zt.offset + 16 * two, ap=[list(zt.ap[0]), [512, 4], [160, 4], [1, 16]])
            st_ = src[64 * two:64 * two + 64, :, :]
            s4 = bass.AP(tensor=st_.tensor, offset=st_.offset, ap=[list(st_.ap[0]), [64, 4], [16, 4], [1, 16]])
            if neg:
                P.op("dve", lambda e: e.tensor_scalar(out=dst, in0=s4, scalar1=-1.0, scalar2=None, op0=ALU.mult),
                     reads=[prm], writes=[prm])
            else:
                P.op("dve", lambda e: e.tensor_copy(out=dst, in_=s4), reads=[prm], writes=[prm])
        return z
    zbr = zpad("zbr", bbr, F32)
    zbi = zpad("zbi", bbi, F32)
    BTr = P.sb("S_BTr", [128, 16, 128], BF16)
    BTi = P.sb("S_BTi", [128, 16, 128], BF16)
    with P.sub_scope():
        tps = [P.ps(f"S_tp{i}", [128, 512], F32) for i in range(2)]
        tps_b = [Buf(f"S_tp{i}") for i in range(2)]
        k = 0
        for (z, BT) in ((zbr, BTr), (zbi, BTi)):
            for q in range(4):
                tb = k % 2
                k += 1
                for r in range(4):
                    gp = 4 * q + r
                    P.op("pe", lambda e: e.transpose(out=tps[tb][:, 128 * r:128 * (r + 1)], in_=z[:, gp, :],
                                                     identity=C["ident_f"][:]),
                         reads=[prm, C["ident_f_b"]], writes=[tps_b[tb]])
                P.op("act", lambda e: e.copy(out=BT[:, 4 * q:4 * q + 4, :],
                                             in_=tps[tb][:].rearrange("p (r t) -> p r t", r=4)),
                     reads=[tps_b[tb]], writes=[prm])
    R["BTr"], R["BTi"] = BTr, BTi

    ct = P.sb("S_ct", [128, 2, 16, 64], F32)
    lm = P.sb("S_lm", [128, 4, 16], F32)

    def bcj(a, m):
        return bass.AP(tensor=a.tensor, offset=a.offset, ap=[list(a.ap[0]), list(a.ap[1]), [0, m]])

    def table(name, l_r, l_i, reverse):
        tr = P.sb(f"S_{name}r", [128, 16, 128], F32)
        ti = P.sb(f"S_{name}i", [128, 16, 128], F32)
        one = 127 if reverse else 0
        P.op("dve", lambda e: e.memset(tr[:, :, one:one + 1], 1.0), reads=[prm], writes=[prm])
        P.op("dve", lambda e: e.memset(ti[:, :, one:one + 1], 0.0), reads=[prm], writes=[prm])
        P.op("dve", lambda e: e.tensor_copy(out=lm[:, 0, :], in_=l_r), reads=[prm], writes=[prm])
        P.op("dve", lambda e: e.tensor_copy(out=lm[:, 1, :], in_=l_i), reads=[prm], writes=[prm])
        m = 1
        while m < 128:
            if reverse:
                src = slice(128 - m, 128)
                dst = slice(128 - 2 * m, 128 - m)
            else:
                src = slice(0, m)
                dst = slice(m, 2 * m)
            cplx_mul(P, "dve", tr[:, :, dst], ti[:, :, dst], tr[:, :, src], ti[:, :, src], bcj(lm[:, 0, :], m),
                     bcj(lm[:, 1, :], m), (ct[:, 0, :, 0:m], ct[:, 1, :, 0:m]), [prm], [prm], prm)
            tt(lm[:, 2, :], lm[:, 0, :], lm[:, 0, :], ALU.mult)
            tt(lm[:, 3, :], lm[:, 1, :], lm[:, 1, :], ALU.mult)
            tt(lm[:, 1, :], lm[:, 0, :], lm[:, 1, :], ALU.mult)
            ts(lm[:, 1, :], lm[:, 1, :], 2.0, None, ALU.mult)
            tt(lm[:, 0, :], lm[:, 2, :], lm[:, 3, :], ALU.subtract)
            m *= 2
        return tr, ti

    if need_rev:
        R["Qr"], R["Qi"] = table("Q", lbr, lbi, True)
    if need_fwd:
        R["Pr"], R["Pi"] = table("P", lbr, lbi, False)
        l128 = P.sb("S_l128", [128, 2, 16], F32)
        P.op("dve", lambda e: e.tensor_copy(out=l128[:], in_=lm[:, 0:2, :]), reads=[prm], writes=[prm])
        R["l128"] = l128
        lam1 = P.sb("S_lam1", [128, 2, 16], F32)
        P.op("dve", lambda e: e.tensor_copy(out=lam1[:, 0, :], in_=lbr), reads=[prm], writes=[prm])
        P.op("dve", lambda e: e.tensor_copy(out=lam1[:, 1, :], in_=lbi), reads=[prm], writes=[prm])
        R["lam1"] = lam1
        rm2 = W(15)
        act(rm2, ar, AF.Exp, scale=-2.0)
        tt(W(16), lbr, rm2, ALU.mult)
        tt(W(17), lbi, rm2, ALU.mult)
        ts(W(17), W(17), -1.0, None, ALU.mult)
        R["Ir"], R["Ii"] = table("I", W(16), W(17), False)
        cre, cim = ld("s5_cre", [128, 16, 16]), ld("s5_cim", [128, 16, 16])
        R["ZCr"] = zpad("zcr", cre, BF16)
        R["ZCi"] = zpad("zci", cim, BF16, neg=True)
        dl = ld("s5_d", [128, 4])
        Dd = P.sb("S_Dd", [128, 4, 128], BF16)
        for c4 in range(4):
            P.op("dve", lambda e: e.tensor_scalar(out=Dd[:, c4, :], in0=C["ident_f"][:], scalar1=dl[:, c4:c4 + 1],
                                                  scalar2=None, op0=ALU.mult), reads=[prm, C["ident_f_b"]], writes=[prm])
        R["Dd"] = Dd
        R["bglu"] = ld("s5_bglu", [128, 4])
        wg = P.sb("S_wglu", [128, 4, 512], BF16)
        P.dma("pool", wg[:], w_src(T["s5_wglu"], 512, 0, 4, 0, 512), writes=[prm])
        R["wglu"] = wg
    return R


def load_uT(P, T, ub):
    uT = P.sb("S_uT", [128, 4, TPC], BF16)
    uT_b = Buf("S_uT")
    for c4 in range(4):
        P.dma("sp", uT[:, c4, :], T["uT"][128 * c4:128 * (c4 + 1), :], reads=[ub] if ub is not None else [],
              writes=[uT_b])
    return uT, uT_b


def emit_s5_pass1(P, T, C, uT_dram_b=None):
    R = s5_prep(P, T, C, need_rev=True, need_fwd=False)
    prm = R["prm"]
    uT, uT_b = load_uT(P, T, uT_dram_b)
    bu = [P.ps(f"S1_bu{i}", [128, 512], F32) for i in range(4)]
    bu_b = [Buf(f"S1_bu{i}") for i in range(4)]
    junk = P.sb("S1_junk", [128, 128], F32)
    junk_b = Buf("S1_junk")
    Ea = P.sb("S1_Ea", [128, 4, 16, 16], F32)
    Ea_b = Buf("S1_Ea")
    P.op("dve", lambda e: e.memset(Ea[:], 0.0), writes=[Ea_b])
    k = 0
    for tg in range(4):
        ts_ = slice(512 * tg, 512 * (tg + 1))
        for gp in range(16):
            br_i, bi_i = (2 * k) % 4, (2 * k + 1) % 4
            k += 1
            P.op("pe", lambda e: e.matmul(out=bu[br_i][:], lhsT=R["BTr"][:, gp, :], rhs=uT[:, gp // 4, ts_], start=True,
                                          stop=True), reads=[prm, uT_b], writes=[bu_b[br_i]])
            P.op("pe", lambda e: e.matmul(out=bu[bi_i][:], lhsT=R["BTi"][:, gp, :], rhs=uT[:, gp // 4, ts_], start=True,
                                          stop=True), reads=[prm, uT_b], writes=[bu_b[bi_i]])
            for s in range(4):
                j = 4 * tg + s
                cs_ = slice(128 * s, 128 * (s + 1))
                for kind, (src_i, tab) in enumerate(((br_i, R["Qr"]), (bi_i, R["Qi"]), (br_i, R["Qi"]), (bi_i, R["Qr"]))):
                    P.op("dve", lambda e: e.scalar_tensor_tensor(out=junk[:], in0=bu[src_i][:, cs_], scalar=1.0,
                                                                 in1=tab[:, gp, :], op0=ALU.mult, op1=ALU.mult,
                                                                 accum_out=Ea[:, kind, gp, j:j + 1]),
                         reads=[bu_b[src_i], prm], writes=[junk_b, Ea_b])
    Eo = P.sb("S1_Eo", [128, 2, 256], F32)
    Eo_b = Buf("S1_Eo")
    P.op("dve", lambda e: e.tensor_tensor(out=Eo[:, 0, :], in0=Ea[:, 0].rearrange("p a b -> p (a b)"),
                                          in1=Ea[:, 1].rearrange("p a b -> p (a b)"), op=ALU.subtract),
         reads=[Ea_b], writes=[Eo_b])
    P.op("dve", lambda e: e.tensor_tensor(out=Eo[:, 1, :], in0=Ea[:, 2].rearrange("p a b -> p (a b)"),
                                          in1=Ea[:, 3].rearrange("p a b -> p (a b)"), op=ALU.add),
         reads=[Ea_b], writes=[Eo_b])
    for ri in range(2):
        P.dma("sp", T["E"][ri], Eo[:, ri, :], reads=[Eo_b])


def emit_s5_pass2(P, T, C):
    R = s5_prep(P, T, C, need_rev=False, need_fwd=True)
    prm = R["prm"]
    uT, uT_b = load_uT(P, T, None)
    Eall = P.sb("S2_E", [128, 8, 2, 256], F32)
    Eall_b = Buf("S2_E")
    for r in range(8):
        for ri in range(2):
            P.dma("sp", Eall[:, r, ri, :], T["E_g"][r, ri], writes=[Eall_b])
    selr = P.sb("S2_sel", [128, 8], F32)
    P.dma("sp", selr[:], T["selr"], writes=[Eall_b])
    Sall = P.sb("S2_S", [128, 8, 2, 16, 16], F32)
    Sall_b = Buf("S2_S")
    cur = P.sb("S2_cur", [128, 2, 16], F32)
    tq = P.sb("S2_tq", [128, 4, 16], F32)
    P.op("dve", lambda e: e.memset(cur[:], 0.0), writes=[Sall_b])
    order = {}
    for r in range(8):
        for j in range(NSLOT):
            order[blk_of(r, j)] = (r, j)
    Ev = Eall[:].rearrange("p r i (g j) -> p r i g j", j=16)
    L = R["l128"]
    for b in range(128):
        r, j = order[b]
        for ri in range(2):
            P.op("dve", lambda e: e.tensor_copy(out=Sall[:, r, ri, :, j], in_=cur[:, ri, :]), reads=[Sall_b],
                 writes=[Sall_b])
        if b == 127:
            break
        dd = [Sall_b, prm, Eall_b]
        P.op("dve", lambda e: e.tensor_tensor(out=tq[:, 0, :], in0=cur[:, 0, :], in1=L[:, 0, :], op=ALU.mult), reads=dd, writes=[Sall_b])
        P.op("dve", lambda e: e.tensor_tensor(out=tq[:, 1, :], in0=cur[:, 1, :], in1=L[:, 1, :], op=ALU.mult), reads=dd, writes=[Sall_b])
        P.op("dve", lambda e: e.tensor_tensor(out=tq[:, 2, :], in0=cur[:, 0, :], in1=L[:, 1, :], op=ALU.mult), reads=dd, writes=[Sall_b])
        P.op("dve", lambda e: e.tensor_tensor(out=tq[:, 3, :], in0=cur[:, 1, :], in1=L[:, 0, :], op=ALU.mult), reads=dd, writes=[Sall_b])
        P.op("dve", lambda e: e.tensor_tensor(out=tq[:, 0, :], in0=tq[:, 0, :], in1=tq[:, 1, :], op=ALU.subtract), reads=dd, writes=[Sall_b])
        P.op("dve", lambda e: e.tensor_tensor(out=tq[:, 2, :], in0=tq[:, 2, :], in1=tq[:, 3, :], op=ALU.add), reads=dd, writes=[Sall_b])
        P.op("dve", lambda e: e.tensor_tensor(out=cur[:, 0, :], in0=tq[:, 0, :], in1=Ev[:, r, 0, :, j], op=ALU.add), reads=dd, writes=[Sall_b])
        P.op("dve", lambda e: e.tensor_tensor(out=cur[:, 1, :], in0=tq[:, 2, :], in1=Ev[:, r, 1, :, j], op=ALU.add), reads=dd, writes=[Sall_b])
    Sown = P.sb("S2_own", [128, 2, 16, 16], F32)
    So2 = Sown[:].rearrange("p i g j -> p (i g j)")
    for r in range(8):
        src = Sall[:, r].rearrange("p i g j -> p (i g j)")
        if r == 0:
            P.op("dve", lambda e: e.tensor_scalar(out=So2, in0=src, scalar1=selr[:, 0:1], scalar2=None, op0=ALU.mult),
                 reads=[Sall_b, Eall_b], writes=[Sall_b])
        else:
            P.op("dve", lambda e: e.scalar_tensor_tensor(out=So2, in0=src, scalar=selr[:, r:r + 1], in1=So2,
                                                         op0=ALU.mult, op1=ALU.add), reads=[Sall_b, Eall_b],
                 writes=[Sall_b])
    LS = P.sb("S2_LS", [128, 2, 16, 16], F32)
    tl = P.sb("S2_tl", [128, 2, 16, 16], F32)
    lam1 = R["lam1"]

    def bcj(a, m):
        return bass.AP(tensor=a.tensor, offset=a.offset, ap=[list(a.ap[0]), list(a.ap[1]), [0, m]])
    cplx_mul(P, "dve", LS[:, 0], LS[:, 1], Sown[:, 0], Sown[:, 1], bcj(lam1[:, 0, :], 16), bcj(lam1[:, 1, :], 16),
             (tl[:, 0], tl[:, 1]), [Sall_b, prm], [Sall_b], Sall_b)

    bu = [P.ps(f"S2_bu{i}", [128, 512], F32) for i in range(4)]
    bu_b = [Buf(f"S2_bu{i}") for i in range(4)]
    yps = [P.ps(f"S2_y{i}", [128, 512], F32) for i in range(2)]
    yps_b = [Buf(f"S2_y{i}") for i in range(2)]
    gps = P.ps("S2_g", [128, 512], F32)
    gps_b = Buf("S2_g")
    tmp = [P.sb(f"S2_t{i}", [128, 512], F32) for i in range(4)]
    tmp_b = [Buf(f"S2_t{i}") for i in range(4)]
    z = [P.sb(f"S2_z{i}", [128, 512], F32) for i in range(2)]
    z_b = [Buf(f"S2_z{i}") for i in range(2)]
    cc = [P.sb(f"S2_c{i}", [128, 512], F32) for i in range(2)]
    cc_b = [Buf(f"S2_c{i}") for i in range(2)]
    xb = [[P.sb(f"S2_x{i}{k}", [128, 512], BF16) for k in range(2)] for i in range(2)]
    xb_b = [[Buf(f"S2_x{i}{k}") for k in range(2)] for i in range(2)]
    rmask = P.sb("S2_rm", [128, 512], F32)
    rmask_b = Buf("S2_rm")
    P.op("pool", lambda e: e.memset(rmask[:], 1.0), writes=[rmask_b])
    P.op("pool", lambda e: e.memset(rmask[:].rearrange("p (s i) -> p s i", i=128)[:, :, 0:1], 0.0), reads=[rmask_b],
         writes=[rmask_b])
    yf = P.sb("S2_yf", [128, 4, 512], F32)
    yf_b = Buf("S2_yf")
    yg = P.sb("S2_yg", [128, 4, 512], BF16)
    yg_b = Buf("S2_yg")
    gt = [P.sb(f"S2_gt{i}", [128, 512], F32) for i in range(2)]
    gt_b = [Buf(f"S2_gt{i}") for i in range(2)]
    ob = [P.sb(f"S2_ob{i}", [128, 512], BF16) for i in range(2)]
    ob_b = [Buf(f"S2_ob{i}") for i in range(2)]

    def b4(t, gp):
        a = t[:, gp, :]
        return bass.AP(tensor=a.tensor, offset=a.offset, ap=[list(a.ap[0]), [0, 4], [1, 128]])

    def v4(a):
        return a.rearrange("p (s i) -> p s i", i=128)
    k = 0
    oi = 0
    for tg in range(4):
        ts_ = slice(512 * tg, 512 * (tg + 1))
        for c4 in range(4):
            yi = (4 * tg + c4) % 2
            for q in range(4):
                gp = 4 * c4 + q
                br_i, bi_i = (2 * k) % 4, (2 * k + 1) % 4
                xi = k % 2
                k += 1
                P.op("pe", lambda e: e.matmul(out=bu[br_i][:], lhsT=R["BTr"][:, gp, :], rhs=uT[:, c4, ts_], start=True,
                                              stop=True), reads=[prm, uT_b], writes=[bu_b[br_i]])
                P.op("pe", lambda e: e.matmul(out=bu[bi_i][:], lhsT=R["BTi"][:, gp, :], rhs=uT[:, c4, ts_], start=True,
                                              stop=True), reads=[prm, uT_b], writes=[bu_b[bi_i]])
                bur, bui = v4(bu[br_i][:]), v4(bu[bi_i][:])
                Ir, Ii, Pr, Pi = b4(R["Ir"], gp), b4(R["Ii"], gp), b4(R["Pr"], gp), b4(R["Pi"], gp)
                P.op("dve", lambda e: e.tensor_tensor(out=v4(tmp[0][:]), in0=bur, in1=Ir, op=ALU.mult), reads=[bu_b[br_i], prm], writes=[tmp_b[0]])
                P.op("dve", lambda e: e.tensor_tensor(out=v4(tmp[1][:]), in0=bui, in1=Ii, op=ALU.mult), reads=[bu_b[bi_i], prm], writes=[tmp_b[1]])
                P.op("dve", lambda e: e.tensor_tensor(out=v4(tmp[2][:]), in0=bur, in1=Ii, op=ALU.mult), reads=[bu_b[br_i], prm], writes=[tmp_b[2]])
                P.op("dve", lambda e: e.tensor_tensor(out=v4(tmp[3][:]), in0=bui, in1=Ir, op=ALU.mult), reads=[bu_b[bi_i], prm], writes=[tmp_b[3]])
                P.op("pool", lambda e: e.tensor_tensor(out=z[0][:], in0=tmp[0][:], in1=tmp[1][:], op=ALU.subtract), reads=[tmp_b[0], tmp_b[1]], writes=[z_b[0]])
                P.op("pool", lambda e: e.tensor_tensor(out=z[1][:], in0=tmp[2][:], in1=tmp[3][:], op=ALU.add), reads=[tmp_b[2], tmp_b[3]], writes=[z_b[1]])
                for ri in range(2):
                    zc = v4(z[ri][:])[:, :, 0:1]
                    cs_ = LS[:, ri, gp, 4 * tg:4 * tg + 4]
                    cs3 = bass.AP(tensor=cs_.tensor, offset=cs_.offset, ap=[list(cs_.ap[0]), list(cs_.ap[1]), [1, 1]])
                    P.op("pool", lambda e: e.tensor_tensor(out=zc, in0=zc, in1=cs3, op=ALU.add), reads=[z_b[ri], Sall_b], writes=[z_b[ri]])
                    P.op("dve", lambda e: e.tensor_tensor_scan(out=cc[ri][:], data0=rmask[:], data1=z[ri][:], initial=0.0,
                                                               op0=ALU.mult, op1=ALU.add), reads=[z_b[ri], rmask_b], writes=[cc_b[ri]])
                c0, c1 = v4(cc[0][:]), v4(cc[1][:])
                P.op("dve", lambda e: e.tensor_tensor(out=v4(tmp[0][:]), in0=c0, in1=Pr, op=ALU.mult), reads=[cc_b[0], prm], writes=[tmp_b[0]])
                P.op("dve", lambda e: e.tensor_tensor(out=v4(tmp[1][:]), in0=c1, in1=Pi, op=ALU.mult), reads=[cc_b[1], prm], writes=[tmp_b[1]])
                P.op("dve", lambda e: e.tensor_tensor(out=v4(tmp[2][:]), in0=c0, in1=Pi, op=ALU.mult), reads=[cc_b[0], prm], writes=[tmp_b[2]])
                P.op("dve", lambda e: e.tensor_tensor(out=v4(tmp[3][:]), in0=c1, in1=Pr, op=ALU.mult), reads=[cc_b[1], prm], writes=[tmp_b[3]])
                P.op("pool", lambda e: e.tensor_tensor(out=xb[xi][0][:], in0=tmp[0][:], in1=tmp[1][:], op=ALU.subtract), reads=[tmp_b[0], tmp_b[1]], writes=[xb_b[xi][0]])
                P.op("pool", lambda e: e.tensor_tensor(out=xb[xi][1][:], in0=tmp[2][:], in1=tmp[3][:], op=ALU.add), reads=[tmp_b[2], tmp_b[3]], writes=[xb_b[xi][1]])
                P.op("pe", lambda e: e.matmul(out=yps[yi][:], lhsT=R["ZCr"][:, gp, :], rhs=xb[xi][0][:], start=(q == 0), stop=False),
                     reads=[prm, xb_b[xi][0]], writes=[yps_b[yi]])
                P.op("pe", lambda e: e.matmul(out=yps[yi][:], lhsT=R["ZCi"][:, gp, :], rhs=xb[xi][1][:], start=False, stop=False),
                     reads=[prm, xb_b[xi][1]], writes=[yps_b[yi]])
            P.op("pe", lambda e: e.matmul(out=yps[yi][:], lhsT=R["Dd"][:, c4, :], rhs=uT[:, c4, ts_], start=False, stop=True),
                 reads=[prm, uT_b], writes=[yps_b[yi]])
            g0, g1 = gt[0], gt[1]
            P.op("act", lambda e: e.activation(out=g0[:], in_=yps[yi][:], func=AF.Square), reads=[yps_b[yi]], writes=[gt_b[0]])
            P.op("dve", lambda e: e.tensor_scalar(out=g0[:], in0=g0[:], scalar1=0.044715, scalar2=1.0, op0=ALU.mult, op1=ALU.add),
                 reads=[gt_b[0]], writes=[gt_b[0]])
            P.op("dve", lambda e: e.tensor_tensor(out=g0[:], in0=yps[yi][:], in1=g0[:], op=ALU.mult), reads=[yps_b[yi], gt_b[0]], writes=[gt_b[0]])
            P.op("act", lambda e: e.activation(out=g1[:], in_=g0[:], func=AF.Sigmoid, scale=1.5957691216057308), reads=[gt_b[0]], writes=[gt_b[1]])
            P.op("dve", lambda e: e.tensor_tensor(out=yf[:, c4, :], in0=yps[yi][:], in1=g1[:], op=ALU.mult), reads=[yps_b[yi], gt_b[1]], writes=[yf_b])
            P.op("pool", lambda e: e.tensor_copy(out=yg[:, c4, :], in_=yf[:, c4, :]), reads=[yf_b], writes=[yg_b])
        for oc in range(4):
            for c4 in range(4):
                P.op("pe", lambda e: e.matmul(out=gps[:], lhsT=R["wglu"][:, c4, 128 * oc:128 * (oc + 1)], rhs=yg[:, c4, :],
                                              start=(c4 == 0), stop=(c4 == 3)), reads=[prm, yg_b], writes=[gps_b])
            P.op("act", lambda e: e.activation(out=gt[0][:], in_=gps[:], func=AF.Sigmoid, bias=R["bglu"][:, oc:oc + 1]),
                 reads=[gps_b, prm], writes=[gt_b[0]])
            o_i = oi % 2
            oi += 1
            P.op("dve", lambda e: e.tensor_tensor(out=ob[o_i][:], in0=yf[:, oc, :], in1=gt[0][:], op=ALU.mult),
                 reads=[yf_b, gt_b[0]], writes=[ob_b[o_i]])
            P.dma("sp", T["catT"][512 + 128 * oc:512 + 128 * (oc + 1), ts_], ob[o_i][:], reads=[ob_b[o_i]])


def build_S(which):
    nc = bass.Bass("TRN2", target_bir_lowering=False)
    T = {}

    def din(name, shape, dt=F32):
        T[name] = nc.dram_tensor(name, list(shape), dt, kind="ExternalInput").ap()

    def dout(name, shape, dt=F32):
        T[name] = nc.dram_tensor(name, list(shape), dt, kind="ExternalOutput").ap()
    for k, shp in S5_IN.items():
        din(k, shp)
    din("ident", [128, 128]); din("uT", [512, TPC], BF16)
    P = Prog(nc)
    P.begin_phase("S")
    C = make_consts(P, T["ident"])
    if which == 1:
        dout("E", [2, 128, 256])
        emit_s5_pass1(P, T, C)
    else:
        din("E_g", [8, 2, 128, 256]); din("selr", [128, 8]); dout("catT", [D, TPC], BF16)
        emit_s5_pass2(P, T, C)
    P.end_phase()
    P.finish()
    return nc


DSA_NIT = 27
DSA_LIM = 64.0


def emit_phase_DA(P, T, C, slots=range(NSLOT)):
    U8 = mybir.dt.uint8
    isc = (64.0 ** -0.5) * (8.0 ** -0.5)
    asc = 128.0 ** -0.5
    ones = P.sb("D_ones", [128, 128], BF16)
    zeros = P.sb("D_zeros", [128, 128], BF16)
    cst_b = Buf("D_cst")
    P.op("dve", lambda e: e.memset(ones[:], 1.0), writes=[cst_b])
    P.op("dve", lambda e: e.memset(zeros[:], 0.0), writes=[cst_b])
    score = P.sb("D_score", [128, 8, 2048], F32)
    score_b = Buf("D_score")
    junk = P.sb("D_junk", [128, 8, 2048], U8)
    junk_b = Buf("D_junk")
    qi_t = P.sb("D_qi", [64, 8, 128], BF16)
    qi_b = Buf("D_qi")
    qmq = P.sb("D_qmq", [64, 128], BF16)
    wcol = P.sb("D_wcol", [128, 8], F32)
    dg = P.sb("D_dg", [128, 8, 128], BF16)
    dg_b = Buf("D_dg")
    kir = [P.sb(f"D_kir{i}", [64, 2048], BF16) for i in range(2)]
    kmr = [P.sb(f"D_kmr{i}", [64, 512], BF16) for i in range(2)]
    kir_b = [Buf(f"D_kir{i}") for i in range(2)]
    Rh = [P.sb(f"D_Rh{i}", [128, 8, 512], BF16) for i in range(2)]
    Rh_b = [Buf(f"D_Rh{i}") for i in range(2)]
    qa_t = P.sb("D_qa", [128, 4, 128], BF16)
    qa_b = Buf("D_qa")
    KA = [P.sb(f"D_KA{i}", [128, 4, 2048], BF16) for i in range(2)]
    KA_b = [Buf(f"D_KA{i}") for i in range(2)]
    VA = [P.sb(f"D_VA{i}", [128, 16, 512], BF16) for i in range(2)]
    VA_b = [Buf(f"D_VA{i}") for i in range(2)]
    m01 = [P.sb(f"D_m01{i}", [128, 512], BF16) for i in range(2)]
    m01_b = [Buf(f"D_m01{i}") for i in range(2)]
    mT4 = [P.sb(f"D_mT{i}", [128, 4, 128], BF16) for i in range(2)]
    mT4_b = [Buf(f"D_mT{i}") for i in range(2)]
    PTr = [P.sb(f"D_PTr{i}", [128, 512], BF16) for i in range(2)]
    PTr_b = [Buf(f"D_PTr{i}") for i in range(2)]
    PTm = [P.sb(f"D_PTm{i}", [128, 4, 128], BF16) for i in range(3)]
    PTm_b = [Buf(f"D_PTm{i}") for i in range(3)]
    bs = P.sb("D_bs", [128, 8], F32)
    bs_b = Buf("D_bs")
    rz = P.sb("D_rz", [128, 512], F32)
    rz_b = Buf("D_rz")
    oo = [P.sb(f"D_oo{i}", [128, 4, 128], BF16) for i in range(2)]
    oo_b = [Buf(f"D_oo{i}") for i in range(2)]
    dps = [P.ps(f"D_dps{i}", [128, 512], F32) for i in range(2)]
    dps_b = [Buf(f"D_dps{i}") for i in range(2)]
    scp = P.ps("D_scp", [128, 512], F32)
    scp_b = Buf("D_scp")
    sT = [P.ps(f"D_sT{i}", [128, 512], F32) for i in range(2)]
    sT_b = [Buf(f"D_sT{i}") for i in range(2)]
    oacc = P.ps("D_oacc", [128, 512], F32)
    oacc_b = Buf("D_oacc")
    zacc = P.ps("D_zacc", [128, 512], F32)
    zacc_b = Buf("D_zacc")
    mtp = P.ps("D_mtp", [128, 512], BF16)
    mtp_b = Buf("D_mtp")
    qiT, kiT, wi, qaT, kaT, va, qm, km = (T[k] for k in ("qiT", "kiT_g", "wi", "qaT", "kaT_g", "va_g", "qm", "km_g"))
    ident_f, ident, idb = C["ident_f"], C["ident"], [C["ident_f_b"], C["ident_b"]]

    cnt = dict(d=0, r=0, k=0, s=0, p=0, pm=0, m=0, o=0)
    for j in slots:
        g = j // 4
        nk = 4 * g + 4
        nkeys = 128 * nk
        qs = slice(128 * j, 128 * (j + 1))
        P.dma("sp", qi_t[:], bass.AP(tensor=qiT.tensor, offset=qiT.offset + 128 * j, ap=[[TPC, 64], [64 * TPC, 8], [1, 128]]),
              writes=[qi_b])
        P.dma("sp", qmq[:], qm[:, qs], writes=[qi_b])
        P.dma("sp", wcol[:], wi[qs, :], writes=[qi_b])
        P.dma("sp", qa_t[:], bass.AP(tensor=qaT.tensor, offset=qaT.offset + 128 * j, ap=[[TPC, 128], [128 * TPC, 4], [1, 128]]),
              writes=[qa_b])
        for h in range(8):
            P.op("dve", lambda e: e.tensor_scalar(out=dg[:, h, :], in0=ident_f[:], scalar1=wcol[:, h:h + 1], scalar2=isc,
                                                  op0=ALU.mult, op1=ALU.mult), reads=[qi_b] + idb, writes=[dg_b])
        for r in range(8):
            kb = cnt["k"] % 2
            cnt["k"] += 1
            P.dma("sp", kir[kb][:, 0:nkeys], kiT[64 * r:64 * (r + 1), 0:nkeys], writes=[kir_b[kb]])
            P.dma("sp", kmr[kb][:, :], km[64 * r:64 * (r + 1), 512 * g:512 * (g + 1)], writes=[kir_b[kb]])
            for ch in range(g + 1):
                rb = cnt["r"] % 2
                cnt["r"] += 1
                for h in range(8):
                    di = cnt["d"] % 2
                    cnt["d"] += 1
                    P.op("pe", lambda e: e.matmul(out=dps[di][:], lhsT=qi_t[:, h, :], rhs=kir[kb][:, 512 * ch:512 * (ch + 1)],
                                                  start=True, stop=True), reads=[qi_b, kir_b[kb]], writes=[dps_b[di]])
                    P.op("act", lambda e: e.activation(out=Rh[rb][:, h, :], in_=dps[di][:], func=AF.Relu),
                         reads=[dps_b[di]], writes=[Rh_b[rb]])
                bnd = (ch == g)
                for h in range(8):
                    P.op("pe", lambda e: e.matmul(out=scp[:], lhsT=dg[:, h, :], rhs=Rh[rb][:, h, :], start=(h == 0),
                                                  stop=(h == 7 and not bnd)), reads=[dg_b, Rh_b[rb]], writes=[scp_b])
                if bnd:
                    P.op("pe", lambda e: e.matmul(out=scp[:], lhsT=qmq[:], rhs=kmr[kb][:], start=False, stop=True),
                         reads=[qi_b, kir_b[kb]], writes=[scp_b])
                P.op("dve", lambda e: e.tensor_copy(out=score[:, r, 512 * ch:512 * (ch + 1)], in_=scp[:]), reads=[scp_b],
                     writes=[score_b])
        sv = score[:, :, 0:nkeys]
        jv = junk[:, :, 0:nkeys]
        lo, hd, mid, cn, mm = (bs[:, i:i + 1] for i in range(5))
        P.op("dve", lambda e: e.memset(lo, -DSA_LIM), reads=[bs_b], writes=[bs_b])
        P.op("dve", lambda e: e.memset(hd, 2.0 * DSA_LIM), reads=[bs_b], writes=[bs_b])
        for it in range(DSA_NIT):
            P.op("dve", lambda e: e.tensor_scalar(out=hd, in0=hd, scalar1=0.5, scalar2=None, op0=ALU.mult), reads=[bs_b], writes=[bs_b])
            P.op("dve", lambda e: e.tensor_tensor(out=mid, in0=lo, in1=hd, op=ALU.add), reads=[bs_b], writes=[bs_b])
            P.op("dve", lambda e: e.tensor_scalar(out=jv, in0=sv, scalar1=mid, scalar2=None, op0=ALU.is_ge, op1=ALU.add,
                                                  accum_out=cn), reads=[bs_b, score_b], writes=[bs_b, junk_b])
            P.op("dve", lambda e: e.tensor_scalar(out=mm, in0=cn, scalar1=255.5, scalar2=None, op0=ALU.is_ge), reads=[bs_b], writes=[bs_b])
            P.op("dve", lambda e: e.scalar_tensor_tensor(out=lo, in0=mm, scalar=hd, in1=lo, op0=ALU.mult, op1=ALU.add),
                 reads=[bs_b], writes=[bs_b])
        thr = lo
        P.op("pe", lambda e: e.matmul(out=oacc[:], lhsT=zeros[:], rhs=Rh[0][:, 0, :], start=True, stop=False,
                                      skip_group_check=True), reads=[cst_b, Rh_b[0]], writes=[oacc_b])
        ntile = 8 * nk
        ti = 0
        for r in range(8):
            ab = cnt["k"] % 2
            cnt["k"] += 1
            P.dma("sp", KA[ab][:, :, 0:nkeys], bass.AP(tensor=kaT.tensor, offset=kaT.offset + r * 512 * TPC,
                                                       ap=[[TPC, 128], [128 * TPC, 4], [1, nkeys]]), writes=[KA_b[ab]])
            P.dma("sp", VA[ab][:, 0:nk, :], bass.AP(tensor=va.tensor, offset=va.offset + r * TPC * 512,
                                                    ap=[[512, 128], [128 * 512, nk], [1, 512]]), writes=[VA_b[ab]])
            for ch in range(g + 1):
                mb = cnt["m"] % 2
                cnt["m"] += 1
                P.op("dve", lambda e: e.tensor_scalar(out=m01[mb][:], in0=score[:, r, 512 * ch:512 * (ch + 1)], scalar1=thr,
                                                      scalar2=None, op0=ALU.is_ge), reads=[score_b, bs_b], writes=[m01_b[mb]])
                for t in range(4):
                    P.op("pe", lambda e: e.transpose(out=mtp[:, 128 * t:128 * (t + 1)], in_=m01[mb][:, 128 * t:128 * (t + 1)],
                                                     identity=ident[:]), reads=[m01_b[mb]] + idb, writes=[mtp_b])
                P.op("act", lambda e: e.copy(out=mT4[mb][:], in_=mtp[:].rearrange("p (t q) -> p t q", t=4)), reads=[mtp_b],
                     writes=[mT4_b[mb]])
                for t in range(4):
                    jk = 4 * ch + t
                    si = cnt["s"] % 2
                    cnt["s"] += 1
                    for h in range(4):
                        P.op("pe", lambda e: e.matmul(out=sT[si][:, 128 * h:128 * (h + 1)], lhsT=KA[ab][:, h, 128 * jk:128 * (jk + 1)],
                                                      rhs=qa_t[:, h, :], start=True, stop=True, skip_group_check=True),
                             reads=[KA_b[ab], qa_b], writes=[sT_b[si]])
                    pi = cnt["p"] % 2
                    cnt["p"] += 1
                    P.op("act", lambda e: e.activation(out=PTr[pi][:], in_=sT[si][:], func=AF.Exp, scale=asc), reads=[sT_b[si]],
                         writes=[PTr_b[pi]])
                    pm = cnt["pm"] % 3
                    cnt["pm"] += 1
                    mt = mT4[mb][:, t, :]
                    mbc = bass.AP(tensor=mt.tensor, offset=mt.offset, ap=[list(mt.ap[0]), [0, 4], [1, 128]])
                    eng = "dve" if (cnt["pm"] % 2 == 0) else "pool"
                    P.op(eng, lambda e: e.tensor_tensor(out=PTm[pm][:], in0=PTr[pi][:].rearrange("p (h q) -> p h q", h=4),
                                                        in1=mbc, op=ALU.mult), reads=[PTr_b[pi], mT4_b[mb]], writes=[PTm_b[pm]])
                    last = (ti == ntile - 1)
                    for h in range(4):
                        P.op("pe", lambda e: e.matmul(out=oacc[:, 128 * h:128 * (h + 1)], lhsT=VA[ab][:, jk, 128 * h:128 * (h + 1)],
                                                      rhs=PTm[pm][:, h, :], start=False, stop=last, skip_group_check=True),
                             reads=[VA_b[ab], PTm_b[pm]], writes=[oacc_b])
                    P.op("pe", lambda e: e.matmul(out=zacc[:], lhsT=ones[:], rhs=PTm[pm][:].rearrange("p h q -> p (h q)"),
                                                  start=(ti == 0), stop=last), reads=[cst_b, PTm_b[pm]], writes=[zacc_b])
                    ti += 1
        P.op("dve", lambda e: e.reciprocal(out=rz[:], in_=zacc[:]), reads=[zacc_b], writes=[rz_b])
        ob = cnt["o"] % 2
        cnt["o"] += 1
        P.op("dve", lambda e: e.tensor_tensor(out=oo[ob][:].rearrange("p h q -> p (h q)"), in0=oacc[:], in1=rz[:], op=ALU.mult),
             reads=[oacc_b, rz_b], writes=[oo_b[ob]])
        cat = T["catT"]
        P.dma("sp", bass.AP(tensor=cat.tensor, offset=cat.offset + 128 * j, ap=[[TPC, 128], [128 * TPC, 4], [1, 128]]),
              oo[ob][:], reads=[oo_b[ob]])


def build_DA(slots=range(NSLOT)):
    nc = bass.Bass("TRN2", target_bir_lowering=False)
    T = {}

    def din(name, shape, dt=F32):
        T[name] = nc.dram_tensor(name, list(shape), dt, kind="ExternalInput").ap()

    def dout(name, shape, dt=F32):
        T[name] = nc.dram_tensor(name, list(shape), dt, kind="ExternalOutput").ap()
    din("qiT", [512, TPC], BF16); din("kiT_g", [8 * 64, TPC], BF16); din("wi", [TPC, 8])
    din("qaT", [512, TPC], BF16); din("kaT_g", [8 * 512, TPC], BF16); din("va_g", [8 * TPC, 512], BF16)
    din("qm", [64, TPC], BF16); din("km_g", [8 * 64, TPC], BF16); din("ident", [128, 128])
    dout("catT", [D, TPC], BF16)
    T["maskT"] = nc.dram_tensor("maskT", [4, 8, 16, 128, 4, 128], mybir.dt.uint8).ap()
    P = Prog(nc)
    P.begin_phase("DA")
    C = make_consts(P, T["ident"])
    emit_phase_DA(P, T, C, slots)
    P.end_phase()
    P.finish()
    return nc


A_OUT = dict(qaT=([512, TPC], BF16), kaT=([512, TPC], BF16), qiT=([512, TPC], BF16), kiT=([64, TPC], BF16),
             qcT=([1024, TPC], BF16), kcT=([1024, TPC], BF16), uT=([512, TPC], BF16), va=([TPC, 512], BF16),
             vc=([TPC, 1024], BF16), wi=([TPC, 8], F32))


def build_PA():
    nc = bass.Bass("TRN2", target_bir_lowering=False)
    perm, FM, TM = win_units()
    T = {}

    def din(name, shape, dt=F32):
        T[name] = nc.dram_tensor(name, list(shape), dt, kind="ExternalInput").ap()

    def dout(name, shape, dt=BF16):
        T[name] = nc.dram_tensor(name, list(shape), dt, kind="ExternalOutput").ap()
    din("x", [TPC, D]); din("g", [1, D]); din("win", [D, IN_WIDTH]); din("ident", [128, 128])
    for nm in ("cosA", "sinA", "cosC", "sinC"):
        din(nm, [128, TPC])
    for k, shp in S5_IN.items():
        din(k, shp)
    for k, (shp, dt) in A_OUT.items():
        dout(k, shp, dt)
    dout("E", [2, 128, 256], F32)
    P = Prog(nc)
    P.begin_phase("A")
    emit_phase_A(P, T, FM, TM)
    P.end_phase()
    P.begin_phase("S1")
    C = make_consts(P, T["ident"])
    emit_s5_pass1(P, T, C)
    P.end_phase()
    P.finish()
    return nc


B_IN_BF = dict(qcT=[1024, TPC], qaT=[512, TPC], qiT=[512, TPC], uT=[512, TPC], kcT_g=[8 * 1024, TPC], vc_g=[8 * TPC, 1024],
               kaT_g=[8 * 512, TPC], va_g=[8 * TPC, 512], kiT_g=[8 * 64, TPC], qm=[64, TPC], km_g=[8 * 64, TPC])
B_IN_F = dict(x=[TPC, D], wi=[TPC, 8], E_g=[8, 2, 128, 256], selr=[128, 8], lamv=[4, 64], lamc=[1, 2], subln=[128, 1],
              ident=[128, 128], wout=[D, D], mem=[256, D], gx=[1, D], gm=[1, D], wq=[D, D], wk=[D, D], wv=[D, D],
              wo=[D, D], gmlp=[1, D], w1=[D, 8192], w2=[8192, D])


def build_PB():
    nc = bass.Bass("TRN2", target_bir_lowering=False)
    T = {}
    for k, shp in B_IN_BF.items():
        T[k] = nc.dram_tensor(k, list(shp), BF16, kind="ExternalInput").ap()
    for k, shp in B_IN_F.items():
        T[k] = nc.dram_tensor(k, list(shp), F32, kind="ExternalInput").ap()
    for k, shp in S5_IN.items():
        T[k] = nc.dram_tensor(k, list(shp), F32, kind="ExternalInput").ap()
    T["catT"] = nc.dram_tensor("catT", [D, TPC], BF16, kind="Internal").ap()
    T["x1"] = nc.dram_tensor("x1", [TPC, D], F32, kind="Internal").ap()
    T["x2"] = nc.dram_tensor("x2", [TPC, D], F32, kind="Internal").ap()
    T["x3"] = nc.dram_tensor("x3", [TPC, D], F32, kind="ExternalOutput").ap()
    P = Prog(nc)
    P.begin_phase("C")
    emit_phase_C(P, T)
    P.end_phase()
    P.begin_phase("DA")
    C = make_consts(P, T["ident"])
    emit_phase_DA(P, T, C)
    P.end_phase()
    P.begin_phase("S2")
    C = make_consts(P, T["ident"])
    emit_s5_pass2(P, T, C)
    P.end_phase()
    for nm, fn in (("O", emit_phase_O), ("X", emit_phase_X), ("M", emit_phase_M)):
        P.begin_phase(nm)
        fn(P, T)
        P.end_phase()
    P.finish()
    return nc, P


def build_PF():
    nc = bass.Bass("TRN2", target_bir_lowering=False)
    T = {}
    T["x3"] = nc.dram_tensor("x3", [TPC, D], F32, kind="ExternalInput").ap()
    T["gf"] = nc.dram_tensor("gf", [1, D], F32, kind="ExternalInput").ap()
    T["y"] = nc.dram_tensor("y", [TPC, D], F32, kind="ExternalOutput").ap()
    P = Prog(nc)
    P.begin_phase("F")
    emit_phase_F(P, T)
    P.end_phase()
    P.finish()
    return nc


def _bf(a):
    import ml_dtypes
    a = np.asarray(a)
    if a.dtype == ml_dtypes.bfloat16:
        return a
    return a.astype(ml_dtypes.bfloat16)


def kernel_multi(x, mem, norm_mix, w_in, s5_a_re, s5_a_im, s5_log_dt, s5_b_re, s5_b_im, s5_c_re, s5_c_im, s5_d, s5_w_glu,
           s5_b_glu, diff_lam_q1, diff_lam_k1, diff_lam_q2, diff_lam_k2, diff_subln, w_out, norm_xattn, norm_mem,
           xattn_q, xattn_k, xattn_v, xattn_o, norm_mlp, w_ff1, w_ff2, norm_final):
    f32 = np.float32
    x = np.asarray(x, f32)
    cores = list(range(NCORES))
    pos = [core_positions(c) for c in cores]
    perm, _, _ = win_units()
    ident = np.eye(128, dtype=f32)
    ropes = [rope_tables_fm(c) for c in cores]
    qms = [_bf(mask_rows_q(c)) for c in cores]
    km_g = _bf(mask_rows_k())
    selrs = []
    for c in cores:
        s = np.zeros((128, 8), f32)
        s[:, c] = 1.0
        selrs.append(s)
    xs = [np.ascontiguousarray(x[0][pos[c]]) for c in cores]
    mem0 = np.ascontiguousarray(np.asarray(mem, f32)[0])
    ncA = build_PA()
    ncB, _ = build_PB()
    ncF = build_PF()
    for l in range(DEPTH):
        s5 = s5_host_layout(*(np.asarray(a[l], f32) for a in (s5_a_re, s5_a_im, s5_log_dt, s5_b_re, s5_b_im, s5_c_re,
                                                                s5_c_im, s5_d, s5_w_glu, s5_b_glu)))
        win = np.ascontiguousarray(np.asarray(w_in[l], f32)[:, perm])
        g = np.asarray(norm_mix[l], f32)[None, :].copy()
        in_maps = []
        for c in cores:
            m = dict(x=xs[c], g=g, win=win, ident=ident)
            m.update(ropes[c])
            m.update(s5)
            in_maps.append(m)
        ra = run_bass_kernel_spmd(ncA, in_maps, core_ids=cores).results
        gath = {}
        for k in ("kcT", "vc", "kaT", "va", "kiT"):
            gath[k + "_g"] = np.ascontiguousarray(np.concatenate([np.asarray(ra[c][k]) for c in cores], axis=0))
        E_g = np.ascontiguousarray(np.stack([np.asarray(ra[c]["E"], f32) for c in cores], axis=0))
        lam_init = 0.8 - 0.6 * math.exp(-0.3 * l)
        lamv = np.stack([np.asarray(a[l], f32) for a in (diff_lam_q1, diff_lam_k1, diff_lam_q2, diff_lam_k2)])
        common = dict(E_g=E_g, km_g=km_g, lamv=lamv, lamc=np.array([[lam_init, 1.0 - lam_init]], f32),
                      subln=np.asarray(diff_subln[l], f32)[:, None].copy(), ident=ident,
                      wout=np.asarray(w_out[l], f32), mem=mem0, gx=np.asarray(norm_xattn[l], f32)[None, :].copy(),
                      gm=np.asarray(norm_mem[l], f32)[None, :].copy(), wq=np.asarray(xattn_q[l], f32),
                      wk=np.asarray(xattn_k[l], f32), wv=np.asarray(xattn_v[l], f32), wo=np.asarray(xattn_o[l], f32),
                      gmlp=np.asarray(norm_mlp[l], f32)[None, :].copy(), w1=np.asarray(w_ff1[l], f32),
                      w2=np.asarray(w_ff2[l], f32))
        common.update(gath)
        common.update(s5)
        in_maps = []
        for c in cores:
            m = dict(common)
            m.update(x=xs[c], selr=selrs[c], qm=qms[c])
            for k in ("qcT", "qaT", "qiT", "uT", "wi"):
                m[k] = np.asarray(ra[c][k])
            in_maps.append(m)
        rb = run_bass_kernel_spmd(ncB, in_maps, core_ids=cores).results
        xs = [np.asarray(rb[c]["x3"], f32) for c in cores]
    gf = np.asarray(norm_final, f32)[None, :].copy()
    rf = run_bass_kernel_spmd(ncF, [dict(x3=xs[c], gf=gf) for c in cores], core_ids=cores).results
    out = np.empty((1, L, D), f32)
    for c in cores:
        out[0, pos[c]] = np.asarray(rf[c]["y"], f32)
    return out


GATHER = dict(kcT=([1024, TPC], BF16), vc=([TPC, 1024], BF16), kaT=([512, TPC], BF16), va=([TPC, 512], BF16),
              kiT=([64, TPC], BF16), E=([256, 256], F32))
W_IN = dict(win=[DEPTH, D, IN_WIDTH], gmix=[DEPTH, D], wout=[DEPTH, D, D], gxa=[DEPTH, D], gma=[DEPTH, D],
            wq=[DEPTH, D, D], wk=[DEPTH, D, D], wv=[DEPTH, D, D], wo=[DEPTH, D, D], gml=[DEPTH, D],
            w1=[DEPTH, D, 8192], w2=[DEPTH, 8192, D], lamv=[DEPTH, 4, 64], lamc=[DEPTH, 2], subln=[DEPTH, 128, 1])


def build_fused(depth=DEPTH):
    nc = bass.Bass("TRN2", target_bir_lowering=False)
    perm, FM, TM = win_units()
    X = {}

    def din(name, shape, dt=F32):
        X[name] = nc.dram_tensor(name, list(shape), dt, kind="ExternalInput").ap()

    def scr(name, shape, dt):
        X[name] = nc.dram_tensor(name, list(shape), dt).ap()

    din("x", [TPC, D]); din("mem", [256, D]); din("ident", [128, 128]); din("gf", [1, D])
    for nm in ("cosA", "sinA", "cosC", "sinC"):
        din(nm, [128, TPC])
    din("qm", [64, TPC], BF16); din("km_g", [8 * 64, TPC], BF16); din("selr", [128, 8])
    for k, shp in W_IN.items():
        din(k, shp)
    for k, shp in S5_IN.items():
        din(k, [DEPTH] + list(shp))
    X["y"] = nc.dram_tensor("y", [TPC, D], F32, kind="ExternalOutput").ap()
    for p in range(2):
        for k, (shp, dt) in GATHER.items():
            scr(f"{k}_{p}", shp, dt)
            scr(f"{k}_g{p}", [8 * shp[0], shp[1]], dt)
    for k in ("qcT", "qaT", "qiT", "uT", "wi"):
        scr(k, A_OUT[k][0], A_OUT[k][1])
    scr("catT", [D, TPC], BF16)
    scr("maskT", [4, 8, 16, 128, 4, 128], mybir.dt.uint8)
    for k in ("x1", "x2", "x3"):
        scr(k, [TPC, D], F32)

    P = Prog(nc)
    for l in range(depth):
        p = l % 2
        T = dict(ident=X["ident"], mem=X["mem"], qm=X["qm"], km_g=X["km_g"], selr=X["selr"], catT=X["catT"],
                 x1=X["x1"], x2=X["x2"], x3=X["x3"], maskT=X["maskT"])
        for nm in ("cosA", "sinA", "cosC", "sinC", "qcT", "qaT", "qiT", "uT", "wi"):
            T[nm] = X[nm]
        T["x"] = X["x"] if l == 0 else X["x3"]
        T["g"] = X["gmix"][l:l + 1, :]
        T["win"] = X["win"][l]
        for k in S5_IN:
            T[k] = X[k][l]
        for k in ("kcT", "vc", "kaT", "va", "kiT"):
            T[k] = X[f"{k}_{p}"]
            T[k + "_g"] = X[f"{k}_g{p}"]
        T["E"] = X[f"E_{p}"].rearrange("(i q) c -> i q c", i=2)
        T["E_g"] = X[f"E_g{p}"].rearrange("(r i q) c -> r i q c", r=8, i=2)
        T["lamv"] = X["lamv"][l]
        T["lamc"] = X["lamc"][l:l + 1, :]
        T["subln"] = X["subln"][l]
        T["wout"] = X["wout"][l]
        T["gx"] = X["gxa"][l:l + 1, :]
        T["gm"] = X["gma"][l:l + 1, :]
        for k in ("wq", "wk", "wv", "wo", "w1", "w2"):
            T[k] = X[k][l]
        T["gmlp"] = X["gml"][l:l + 1, :]

        P.begin_phase("A")
        emit_phase_A(P, T, FM, TM)
        P.end_phase()
        P.begin_phase("S1")
        C = make_consts(P, T["ident"])
        emit_s5_pass1(P, T, C)
        P.end_phase()
        for k in GATHER:
            P.allgather(X[f"{k}_g{p}"], X[f"{k}_{p}"])
        P.barrier()
        P.begin_phase("C")
        emit_phase_C(P, T)
        P.end_phase()
        P.begin_phase("DA")
        C = make_consts(P, T["ident"])
        emit_phase_DA3(P, T, C)
        P.end_phase()
        P.begin_phase("S2")
        C = make_consts(P, T["ident"])
        emit_s5_pass2(P, T, C)
        P.end_phase()
        for nm, fn in (("O", emit_phase_O), ("X", emit_phase_X), ("M", emit_phase_M)):
            P.begin_phase(nm)
            fn(P, T)
            P.end_phase()
    P.begin_phase("F")
    emit_phase_F(P, dict(x3=X["x3"], gf=X["gf"], y=X["y"]))
    P.end_phase()
    P.finish()
    return nc, P


def kernel(x, mem, norm_mix, w_in, s5_a_re, s5_a_im, s5_log_dt, s5_b_re, s5_b_im, s5_c_re, s5_c_im, s5_d, s5_w_glu,
           s5_b_glu, diff_lam_q1, diff_lam_k1, diff_lam_q2, diff_lam_k2, diff_subln, w_out, norm_xattn, norm_mem,
           xattn_q, xattn_k, xattn_v, xattn_o, norm_mlp, w_ff1, w_ff2, norm_final):
    f32 = np.float32
    A = lambda a: np.ascontiguousarray(np.asarray(a, f32))
    x = A(x)
    cores = list(range(NCORES))
    pos = [core_positions(c) for c in cores]
    perm, _, _ = win_units()
    s5l = [s5_host_layout(*(np.asarray(a[l], f32) for a in (s5_a_re, s5_a_im, s5_log_dt, s5_b_re, s5_b_im, s5_c_re,
                                                             s5_c_im, s5_d, s5_w_glu, s5_b_glu))) for l in range(DEPTH)]
    s5 = {k: np.ascontiguousarray(np.stack([s5l[l][k] for l in range(DEPTH)])) for k in S5_IN}
    lam_init = [0.8 - 0.6 * math.exp(-0.3 * l) for l in range(DEPTH)]
    common = dict(
        mem=A(mem)[0], ident=np.eye(128, dtype=f32), gf=A(norm_final)[None, :], km_g=_bf(mask_rows_k()),
        win=np.ascontiguousarray(A(w_in)[:, :, perm]), gmix=A(norm_mix), wout=A(w_out), gxa=A(norm_xattn),
        gma=A(norm_mem), wq=A(xattn_q), wk=A(xattn_k), wv=A(xattn_v), wo=A(xattn_o), gml=A(norm_mlp), w1=A(w_ff1),
        w2=A(w_ff2),
        lamv=np.ascontiguousarray(np.stack([A(diff_lam_q1), A(diff_lam_k1), A(diff_lam_q2), A(diff_lam_k2)], axis=1)),
        lamc=np.array([[li, 1.0 - li] for li in lam_init], f32), subln=A(diff_subln)[:, :, None].copy())
    common.update(s5)
    in_maps = []
    for c in cores:
        m = dict(common)
        m["x"] = np.ascontiguousarray(x[0][pos[c]])
        m.update(rope_tables_fm(c))
        m["qm"] = _bf(mask_rows_q(c))
        sel = np.zeros((128, 8), f32)
        sel[:, c] = 1.0
        m["selr"] = sel
        in_maps.append(m)
    nc, _ = build_fused()
    res = run_bass_kernel_spmd(nc, in_maps, core_ids=cores).results
    out = np.empty((1, L, D), f32)
    for c in cores:
        out[0, pos[c]] = np.asarray(res[c]["y"], f32)
    return out


DSA_ACT_RANKS = 3


def emit_phase_DA2(P, T, C, slots=range(NSLOT)):
    U8 = mybir.dt.uint8
    slots = list(slots)
    isc = (64.0 ** -0.5) * (8.0 ** -0.5)
    asc = 128.0 ** -0.5
    RS = 8 - DSA_ACT_RANKS
    ones = P.sb("D_ones", [128, 128], BF16)
    zeros = P.sb("D_zeros", [128, 128], BF16)
    cst_b = Buf("D_cst")
    P.op("pool", lambda e: e.memset(ones[:], 1.0), writes=[cst_b])
    P.op("pool", lambda e: e.memset(zeros[:], 0.0), writes=[cst_b])
    score = P.sb("D_score", [128, 8, 2048], F32)
    score_b = Buf("D_score")
    maskb = P.sb("D_maskb", [128, 8, 2048], BF16)
    maskb_b = Buf("D_maskb")
    junkd = P.sb("D_junkd", [128, 16], U8)
    junkd_b = Buf("D_junkd")
    junka = P.sb("D_junka", [128, 16], BF16)
    junka_b = Buf("D_junka")
    qi_t = P.sb("D_qi", [64, 8, 128], BF16)
    qi_b = Buf("D_qi")
    qmq = P.sb("D_qmq", [64, 128], BF16)
    wcol = P.sb("D_wcol", [128, 8], F32)
    dg = P.sb("D_dg", [128, 8, 128], BF16)
    dg_b = Buf("D_dg")
    kir = [P.sb(f"D_kir{i}", [64, 2048], BF16) for i in range(2)]
    kmr = [P.sb(f"D_kmr{i}", [64, 512], BF16) for i in range(2)]
    kir_b = [Buf(f"D_kir{i}") for i in range(2)]
    Rh = [P.sb(f"D_Rh{i}", [128, 8, 512], BF16) for i in range(2)]
    Rh_b = [Buf(f"D_Rh{i}") for i in range(2)]
    qa_t = P.sb("D_qa", [128, 4, 128], BF16)
    qa_b = Buf("D_qa")
    KA = [P.sb(f"D_KA{i}", [128, 4, 2048], BF16) for i in range(2)]
    KA_b = [Buf(f"D_KA{i}") for i in range(2)]
    VA = [P.sb(f"D_VA{i}", [128, 16, 512], BF16) for i in range(2)]
    VA_b = [Buf(f"D_VA{i}") for i in range(2)]
    mT4 = [P.sb(f"D_mT{i}", [128, 4, 128], BF16) for i in range(2)]
    mT4_b = [Buf(f"D_mT{i}") for i in range(2)]
    PTr = [P.sb(f"D_PTr{i}", [128, 512], BF16) for i in range(3)]
    PTr_b = [Buf(f"D_PTr{i}") for i in range(3)]
    PTm = [P.sb(f"D_PTm{i}", [128, 4, 128], BF16) for i in range(3)]
    PTm_b = [Buf(f"D_PTm{i}") for i in range(3)]
    bs = P.sb("D_bs", [128, 8], F32)
    bs_b = Buf("D_bs")
    thr_t = P.sb("D_thr", [128, 2], F32)
    thr_b = Buf("D_thr")
    rz = P.sb("D_rz", [128, 512], F32)
    rz_b = Buf("D_rz")
    oo = [P.sb(f"D_oo{i}", [128, 4, 128], BF16) for i in range(2)]
    oo_b = [Buf(f"D_oo{i}") for i in range(2)]
    dps = [P.ps(f"D_dps{i}", [128, 512], F32) for i in range(2)]
    dps_b = [Buf(f"D_dps{i}") for i in range(2)]
    scp = P.ps("D_scp", [128, 512], F32)
    scp_b = Buf("D_scp")
    sT = [P.ps(f"D_sT{i}", [128, 512], F32) for i in range(2)]
    sT_b = [Buf(f"D_sT{i}") for i in range(2)]
    oacc = P.ps("D_oacc", [128, 512], F32)
    oacc_b = Buf("D_oacc")
    zacc = P.ps("D_zacc", [128, 512], F32)
    zacc_b = Buf("D_zacc")
    mtp = P.ps("D_mtp", [128, 512], BF16)
    mtp_b = Buf("D_mtp")
    qiT, kiT, wi, qaT, kaT, va, qm, km = (T[k] for k in ("qiT", "kiT_g", "wi", "qaT", "kaT_g", "va_g", "qm", "km_g"))
    ident_f, ident, idb = C["ident_f"], C["ident"], [C["ident_f_b"], C["ident_b"]]
    cnt = dict(d=0, r=0, k=0, ka=0, s=0, p=0, pm=0, m=0, o=0)

    def gen_I(j):
        g = j // 4
        nkeys = 128 * (4 * g + 4)
        qs = slice(128 * j, 128 * (j + 1))
        P.dma("sp", qi_t[:], bass.AP(tensor=qiT.tensor, offset=qiT.offset + 128 * j, ap=[[TPC, 64], [64 * TPC, 8], [1, 128]]),
              writes=[qi_b])
        P.dma("sp", qmq[:], qm[:, qs], writes=[qi_b])
        P.dma("sp", wcol[:], wi[qs, :], writes=[qi_b])
        for h in range(8):
            P.op("pool", lambda e: e.tensor_scalar(out=dg[:, h, :], in0=ident_f[:], scalar1=wcol[:, h:h + 1], scalar2=isc,
                                                   op0=ALU.mult, op1=ALU.mult), reads=[qi_b] + idb, writes=[dg_b])
        for r in range(8):
            kb = cnt["k"] % 2
            cnt["k"] += 1
            P.dma("sp", kir[kb][:, 0:nkeys], kiT[64 * r:64 * (r + 1), 0:nkeys], writes=[kir_b[kb]])
            P.dma("sp", kmr[kb][:, :], km[64 * r:64 * (r + 1), 512 * g:512 * (g + 1)], writes=[kir_b[kb]])
            for ch in range(g + 1):
                rb = cnt["r"] % 2
                cnt["r"] += 1
                for h in range(8):
                    di = cnt["d"] % 2
                    cnt["d"] += 1
                    P.op("pe", lambda e: e.matmul(out=dps[di][:], lhsT=qi_t[:, h, :], rhs=kir[kb][:, 512 * ch:512 * (ch + 1)],
                                                  start=True, stop=True), reads=[qi_b, kir_b[kb]], writes=[dps_b[di]])
                    P.op("act", lambda e: e.activation(out=Rh[rb][:, h, :], in_=dps[di][:], func=AF.Relu),
                         reads=[dps_b[di]], writes=[Rh_b[rb]])
                bnd = (ch == g)
                for h in range(8):
                    P.op("pe", lambda e: e.matmul(out=scp[:], lhsT=dg[:, h, :], rhs=Rh[rb][:, h, :], start=(h == 0),
                                                  stop=(h == 7 and not bnd)), reads=[dg_b, Rh_b[rb]], writes=[scp_b])
                if bnd:
                    P.op("pe", lambda e: e.matmul(out=scp[:], lhsT=qmq[:], rhs=kmr[kb][:], start=False, stop=True),
                         reads=[qi_b, kir_b[kb]], writes=[scp_b])
                P.op("dve", lambda e: e.tensor_copy(out=score[:, r, 512 * ch:512 * (ch + 1)], in_=scp[:]), reads=[scp_b],
                     writes=[score_b])
                yield

    def gen_B(j):
        g = j // 4
        nkeys = 128 * (4 * g + 4)
        svd = score[:, 0:RS, 0:nkeys]
        sva = score[:, RS:8, 0:nkeys]
        jd = bass.AP(tensor=junkd[:].tensor, offset=junkd[:].offset, ap=[list(junkd[:].ap[0]), [0, RS], [0, nkeys]])
        ja = bass.AP(tensor=junka[:].tensor, offset=junka[:].offset, ap=[list(junka[:].ap[0]), [0, 8 - RS], [0, nkeys]])
        nA = float((8 - RS) * nkeys)
        mid, cd, ca, t0, t1 = (bs[:, i:i + 1] for i in range(5))
        lo0 = -DSA_LIM + 0.0123
        hds = [DSA_LIM * 2.0 / (2.0 ** (i + 1)) for i in range(DSA_NIT + 1)]
        P.op("dve", lambda e: e.memset(mid, lo0 + hds[0]), reads=[bs_b], writes=[bs_b])
        for it in range(DSA_NIT):
            P.op("act", lambda e: e.activation(out=ja, in_=sva, func=AF.Sign, scale=-1.0, bias=mid, accum_out=ca),
                 reads=[bs_b, score_b], writes=[junka_b, thr_b])
            P.op("dve", lambda e: e.tensor_scalar(out=jd, in0=svd, scalar1=mid, scalar2=None, op0=ALU.is_ge, op1=ALU.add,
                                                  accum_out=cd), reads=[bs_b, score_b], writes=[bs_b, junkd_b])
            P.op("dve", lambda e: e.scalar_tensor_tensor(out=t0, in0=cd, scalar=2.0, in1=ca, op0=ALU.mult, op1=ALU.subtract),
                 reads=[bs_b, thr_b], writes=[bs_b])
            P.op("dve", lambda e: e.tensor_scalar(out=t1, in0=t0, scalar1=511.0 - nA, scalar2=hds[it], op0=ALU.is_ge,
                                                  op1=ALU.mult), reads=[bs_b], writes=[bs_b])
            nxt = (hds[it + 1] - hds[it]) if it + 1 < DSA_NIT else -hds[it]
            P.op("dve", lambda e: e.scalar_tensor_tensor(out=mid, in0=t1, scalar=nxt, in1=mid, op0=ALU.add, op1=ALU.add),
                 reads=[bs_b], writes=[bs_b])
            yield

    def emit_M(j):
        g = j // 4
        nkeys = 128 * (4 * g + 4)
        P.op("dve", lambda e: e.tensor_scalar(out=maskb[:, :, 0:nkeys], in0=score[:, :, 0:nkeys], scalar1=bs[:, 0:1],
                                              scalar2=None, op0=ALU.is_ge), reads=[score_b, bs_b], writes=[maskb_b])

    def gen_Att(j):
        g = j // 4
        nk = 4 * g + 4
        nkeys = 128 * nk
        P.dma("sp", qa_t[:], bass.AP(tensor=qaT.tensor, offset=qaT.offset + 128 * j, ap=[[TPC, 128], [128 * TPC, 4], [1, 128]]),
              writes=[qa_b])
        P.op("pe", lambda e: e.matmul(out=oacc[:], lhsT=zeros[:], rhs=PTr[0][:],
                                      start=True, stop=False, skip_group_check=True), reads=[cst_b, PTr_b[0]], writes=[oacc_b])
        ntile = 8 * nk
        ti = 0
        for r in range(8):
            ab = cnt["ka"] % 2
            cnt["ka"] += 1
            P.dma("sp", KA[ab][:, :, 0:nkeys], bass.AP(tensor=kaT.tensor, offset=kaT.offset + r * 512 * TPC,
                                                       ap=[[TPC, 128], [128 * TPC, 4], [1, nkeys]]), writes=[KA_b[ab]])
            P.dma("sp", VA[ab][:, 0:nk, :], bass.AP(tensor=va.tensor, offset=va.offset + r * TPC * 512,
                                                    ap=[[512, 128], [128 * 512, nk], [1, 512]]), writes=[VA_b[ab]])
            for ch in range(g + 1):
                mb = cnt["m"] % 2
                cnt["m"] += 1
                for t in range(4):
                    c0 = 512 * ch + 128 * t
                    P.op("pe", lambda e: e.transpose(out=mtp[:, 128 * t:128 * (t + 1)], in_=maskb[:, r, c0:c0 + 128],
                                                     identity=ident[:]), reads=[maskb_b] + idb, writes=[mtp_b])
                P.op("act", lambda e: e.copy(out=mT4[mb][:], in_=mtp[:].rearrange("p (t q) -> p t q", t=4)), reads=[mtp_b],
                     writes=[mT4_b[mb]])
                for t in range(4):
                    jk = 4 * ch + t
                    si = cnt["s"] % 2
                    cnt["s"] += 1
                    for h in range(4):
                        P.op("pe", lambda e: e.matmul(out=sT[si][:, 128 * h:128 * (h + 1)], lhsT=KA[ab][:, h, 128 * jk:128 * (jk + 1)],
                                                      rhs=qa_t[:, h, :], start=True, stop=True, skip_group_check=True),
                             reads=[KA_b[ab], qa_b], writes=[sT_b[si]])
                    pi = cnt["p"] % 3
                    cnt["p"] += 1
                    P.op("act", lambda e: e.activation(out=PTr[pi][:], in_=sT[si][:], func=AF.Exp, scale=asc), reads=[sT_b[si]],
                         writes=[PTr_b[pi]])
                    pm = cnt["pm"] % 3
                    cnt["pm"] += 1
                    mt = mT4[mb][:, t, :]
                    mbc = bass.AP(tensor=mt.tensor, offset=mt.offset, ap=[list(mt.ap[0]), [0, 4], [1, 128]])
                    P.op("pool", lambda e: e.tensor_tensor(out=PTm[pm][:], in0=PTr[pi][:].rearrange("p (h q) -> p h q", h=4),
                                                           in1=mbc, op=ALU.mult), reads=[PTr_b[pi], mT4_b[mb]], writes=[PTm_b[pm]])
                    last = (ti == ntile - 1)
                    for h in range(4):
                        P.op("pe", lambda e: e.matmul(out=oacc[:, 128 * h:128 * (h + 1)], lhsT=VA[ab][:, jk, 128 * h:128 * (h + 1)],
                                                      rhs=PTm[pm][:, h, :], start=False, stop=last, skip_group_check=True),
                             reads=[VA_b[ab], PTm_b[pm]], writes=[oacc_b])
                    P.op("pe", lambda e: e.matmul(out=zacc[:], lhsT=ones[:], rhs=PTm[pm][:].rearrange("p h q -> p (h q)"),
                                                  start=(ti == 0), stop=last), reads=[cst_b, PTm_b[pm]], writes=[zacc_b])
                    ti += 1
                yield
        P.op("dve", lambda e: e.reciprocal(out=rz[:], in_=zacc[:]), reads=[zacc_b], writes=[rz_b])
        ob = cnt["o"] % 2
        cnt["o"] += 1
        P.op("dve", lambda e: e.tensor_tensor(out=oo[ob][:].rearrange("p h q -> p (h q)"), in0=oacc[:], in1=rz[:], op=ALU.mult),
             reads=[oacc_b, rz_b], writes=[oo_b[ob]])
        cat = T["catT"]
        P.dma("sp", bass.AP(tensor=cat.tensor, offset=cat.offset + 128 * j, ap=[[TPC, 128], [128 * TPC, 4], [1, 128]]),
              oo[ob][:], reads=[oo_b[ob]])

    def drain(gen):
        for _ in gen:
            pass

    def interleave(ga, na, gb, nb):
        ia = ib = 0
        da = db = False
        while not (da and db):
            if not da and (db or ia * nb <= ib * na):
                try:
                    next(ga)
                    ia += 1
                except StopIteration:
                    da = True
            else:
                try:
                    next(gb)
                    ib += 1
                except StopIteration:
                    db = True

    P.op("pool", lambda e: e.memset(PTr[0][:], 0.0), writes=[PTr_b[0]])
    drain(gen_I(slots[0]))
    drain(gen_B(slots[0]))
    emit_M(slots[0])
    for idx, j in enumerate(slots):
        nxt = slots[idx + 1] if idx + 1 < len(slots) else None
        if nxt is not None:
            drain(gen_I(nxt))
            interleave(gen_Att(j), 8 * (j // 4 + 1), gen_B(nxt), DSA_NIT)
            emit_M(nxt)
        else:
            drain(gen_Att(j))


def emit_phase_DA3(P, T, C, slots=range(NSLOT)):
    U8 = mybir.dt.uint8
    slots = list(slots)
    isc = (64.0 ** -0.5) * (8.0 ** -0.5)
    asc = 128.0 ** -0.5
    RS = 8 - DSA_ACT_RANKS
    qiT, kiT, wi, qaT, kaT, va, qm, km = (T[k] for k in ("qiT", "kiT_g", "wi", "qaT", "kaT_g", "va_g", "qm", "km_g"))
    mTd = T["maskT"]
    ident_f, ident, idb = C["ident_f"], C["ident"], [C["ident_f_b"], C["ident_b"]]
    mTd_b = Buf("maskT_dram")

    def mT_ap(g, r, jk0, njk, jq, nq):
        off = mTd.offset + (((g * 8 + r) * 16 + jk0) * 128) * 512 + jq * 128
        return bass.AP(tensor=mTd.tensor, offset=off, ap=[[512, 128], [128 * 512, njk], [1, nq * 128]])

    with P.sub_scope():
        score = [P.sb(f"D_score{i}", [128, 8, 2048], F32) for i in range(2)]
        score_b = [Buf(f"D_score{i}") for i in range(2)]
        maskb = P.sb("D_maskb", [128, 8, 2048], BF16)
        maskb_b = Buf("D_maskb")
        junkd = P.sb("D_junkd", [128, 16], U8)
        junkd_b = Buf("D_junkd")
        junka = P.sb("D_junka", [128, 16], BF16)
        junka_b = Buf("D_junka")
        qi_t = P.sb("D_qi", [64, 8, 128], BF16)
        qi_b = Buf("D_qi")
        qmq = P.sb("D_qmq", [64, 128], BF16)
        wcol = P.sb("D_wcol", [128, 8], F32)
        dg = P.sb("D_dg", [128, 8, 128], BF16)
        dg_b = Buf("D_dg")
        kir = [P.sb(f"D_kir{i}", [64, 2048], BF16) for i in range(2)]
        kmr = [P.sb(f"D_kmr{i}", [64, 512], BF16) for i in range(2)]
        kir_b = [Buf(f"D_kir{i}") for i in range(2)]
        Rh = [P.sb(f"D_Rh{i}", [128, 8, 512], BF16) for i in range(2)]
        Rh_b = [Buf(f"D_Rh{i}") for i in range(2)]
        mTu = [P.sb(f"D_mTu{i}", [128, 4, 128], U8) for i in range(3)]
        mTu_b = [Buf(f"D_mTu{i}") for i in range(3)]
        bs = P.sb("D_bs", [128, 8], F32)
        bs_b = Buf("D_bs")
        cab = P.sb("D_cab", [128, 2], F32)
        cab_b = Buf("D_cab")
        dps = [P.ps(f"D_dps{i}", [128, 512], F32) for i in range(3)]
        dps_b = [Buf(f"D_dps{i}") for i in range(3)]
        scp = [P.ps(f"D_scp{i}", [128, 512], F32) for i in range(2)]
        scp_b = [Buf(f"D_scp{i}") for i in range(2)]
        mtp = [P.ps(f"D_mtp{i}", [128, 512], BF16) for i in range(2)]
        mtp_b = [Buf(f"D_mtp{i}") for i in range(2)]
        cnt = dict(d=0, r=0, k=0, sc=0, m=0)

        def gen_I(j, sb_i):
            g = j // 4
            nkeys = 128 * (4 * g + 4)
            qs = slice(128 * j, 128 * (j + 1))
            P.dma("sp", qi_t[:], bass.AP(tensor=qiT.tensor, offset=qiT.offset + 128 * j, ap=[[TPC, 64], [64 * TPC, 8], [1, 128]]),
                  writes=[qi_b])
            P.dma("sp", qmq[:], qm[:, qs], writes=[qi_b])
            P.dma("sp", wcol[:], wi[qs, :], writes=[qi_b])
            for h in range(8):
                P.op("pool", lambda e: e.tensor_scalar(out=dg[:, h, :], in0=ident_f[:], scalar1=wcol[:, h:h + 1], scalar2=isc,
                                                       op0=ALU.mult, op1=ALU.mult), reads=[qi_b] + idb, writes=[dg_b])
            for r in range(8):
                kb = cnt["k"] % 2
                cnt["k"] += 1
                P.dma("sp", kir[kb][:, 0:nkeys], kiT[64 * r:64 * (r + 1), 0:nkeys], writes=[kir_b[kb]])
                P.dma("sp", kmr[kb][:, :], km[64 * r:64 * (r + 1), 512 * g:512 * (g + 1)], writes=[kir_b[kb]])
                for ch in range(g + 1):
                    rb = cnt["r"] % 2
                    cnt["r"] += 1
                    for h in range(8):
                        di = cnt["d"] % 3
                        cnt["d"] += 1
                        P.op("pe", lambda e: e.matmul(out=dps[di][:], lhsT=qi_t[:, h, :], rhs=kir[kb][:, 512 * ch:512 * (ch + 1)],
                                                      start=True, stop=True), reads=[qi_b, kir_b[kb]], writes=[dps_b[di]])
                        P.op("act", lambda e: e.activation(out=Rh[rb][:, h, :], in_=dps[di][:], func=AF.Relu),
                             reads=[dps_b[di]], writes=[Rh_b[rb]])
                    bnd = (ch == g)
                    si = cnt["sc"] % 2
                    cnt["sc"] += 1
                    for h in range(8):
                        P.op("pe", lambda e: e.matmul(out=scp[si][:], lhsT=dg[:, h, :], rhs=Rh[rb][:, h, :], start=(h == 0),
                                                      stop=(h == 7 and not bnd)), reads=[dg_b, Rh_b[rb]], writes=[scp_b[si]])
                    if bnd:
                        P.op("pe", lambda e: e.matmul(out=scp[si][:], lhsT=qmq[:], rhs=kmr[kb][:], start=False, stop=True),
                             reads=[qi_b, kir_b[kb]], writes=[scp_b[si]])
                    P.op("pool" if False else "dve", lambda e: e.tensor_copy(out=score[sb_i][:, r, 512 * ch:512 * (ch + 1)], in_=scp[si][:]),
                         reads=[scp_b[si]], writes=[score_b[sb_i]])
                    yield

        def gen_B(j, sb_i):
            g = j // 4
            nkeys = 128 * (4 * g + 4)
            svd = score[sb_i][:, 0:RS, 0:nkeys]
            sva = score[sb_i][:, RS:8, 0:nkeys]
            jd = bass.AP(tensor=junkd[:].tensor, offset=junkd[:].offset, ap=[list(junkd[:].ap[0]), [0, RS], [0, nkeys]])
            ja = bass.AP(tensor=junka[:].tensor, offset=junka[:].offset, ap=[list(junka[:].ap[0]), [0, 8 - RS], [0, nkeys]])
            nA = float((8 - RS) * nkeys)
            mid, cd, t0, t1 = (bs[:, i:i + 1] for i in range(4))
            ca = cab[:, 0:1]
            lo0 = -DSA_LIM + 0.0123
            hds = [DSA_LIM * 2.0 / (2.0 ** (i + 1)) for i in range(DSA_NIT + 1)]
            P.op("dve", lambda e: e.memset(mid, lo0 + hds[0]), reads=[bs_b], writes=[bs_b])
            for it in range(DSA_NIT):
                P.op("act", lambda e: e.activation(out=ja, in_=sva, func=AF.Sign, scale=-1.0, bias=mid, accum_out=ca),
                     reads=[bs_b, score_b[sb_i]], writes=[junka_b, cab_b])
                P.op("dve", lambda e: e.tensor_scalar(out=jd, in0=svd, scalar1=mid, scalar2=None, op0=ALU.is_ge, op1=ALU.add,
                                                      accum_out=cd), reads=[bs_b, score_b[sb_i]], writes=[bs_b, junkd_b])
                P.op("dve", lambda e: e.scalar_tensor_tensor(out=t0, in0=cd, scalar=2.0, in1=ca, op0=ALU.mult, op1=ALU.subtract),
                     reads=[bs_b, cab_b], writes=[bs_b])
                P.op("dve", lambda e: e.tensor_scalar(out=t1, in0=t0, scalar1=511.0 - nA, scalar2=hds[it], op0=ALU.is_ge,
                                                      op1=ALU.mult), reads=[bs_b], writes=[bs_b])
                nxt = (hds[it + 1] - hds[it]) if it + 1 < DSA_NIT else -hds[it]
                P.op("dve", lambda e: e.scalar_tensor_tensor(out=mid, in0=t1, scalar=nxt, in1=mid, op0=ALU.add, op1=ALU.add),
                     reads=[bs_b], writes=[bs_b])
                yield

        def emit_MT(j, sb_i):
            g = j // 4
            jq = j % 4
            nkeys = 128 * (4 * g + 4)
            P.op("dve", lambda e: e.tensor_scalar(out=maskb[:, :, 0:nkeys], in0=score[sb_i][:, :, 0:nkeys], scalar1=bs[:, 0:1],
                                                  scalar2=None, op0=ALU.is_ge), reads=[score_b[sb_i], bs_b], writes=[maskb_b])
            for r in range(8):
                for ch in range(g + 1):
                    mi = cnt["m"] % 2
                    ui = cnt["m"] % 3
                    cnt["m"] += 1
                    for t in range(4):
                        c0 = 512 * ch + 128 * t
                        P.op("pe", lambda e: e.transpose(out=mtp[mi][:, 128 * t:128 * (t + 1)], in_=maskb[:, r, c0:c0 + 128],
                                                         identity=ident[:]), reads=[maskb_b] + idb, writes=[mtp_b[mi]])
                    P.op("act", lambda e: e.copy(out=mTu[ui][:], in_=mtp[mi][:].rearrange("p (t q) -> p t q", t=4)),
                         reads=[mtp_b[mi]], writes=[mTu_b[ui]])
                    P.dma("sp", mT_ap(g, r, 4 * ch, 4, jq, 1), mTu[ui][:], reads=[mTu_b[ui]], writes=[mTd_b])

        def drain(gen):
            for _ in gen:
                pass

        def interleave(ga, na, gb, nb):
            ia = ib = 0
            da = db = False
            while not (da and db):
                if not da and (db or ia * nb <= ib * na):
                    try:
                        next(ga)
                        ia += 1
                    except StopIteration:
                        da = True
                else:
                    try:
                        next(gb)
                        ib += 1
                    except StopIteration:
                        db = True

        drain(gen_I(slots[0], 0))
        for idx, j in enumerate(slots):
            sb_i = idx % 2
            nxt = slots[idx + 1] if idx + 1 < len(slots) else None
            if nxt is not None:
                interleave(gen_I(nxt, 1 - sb_i), 8 * (nxt // 4 + 1), gen_B(j, sb_i), DSA_NIT)
            else:
                drain(gen_B(j, sb_i))
            emit_MT(j, sb_i)

    groups = sorted(set(j // 4 for j in slots))
    ones = P.sb("D2_ones", [128, 128], BF16)
    ones_b = Buf("D2_ones")
    P.op("pool", lambda e: e.memset(ones[:], 1.0), writes=[ones_b])
    QA = [P.sb(f"D2_QA{i}", [128, 2, 512], BF16) for i in range(2)]
    QA_b = [Buf(f"D2_QA{i}") for i in range(2)]
    KAr = [P.sb(f"D2_KA{i}", [128, 2, 2048], BF16) for i in range(2)]
    VAr = [P.sb(f"D2_VA{i}", [128, 16, 256], BF16) for i in range(2)]
    MTr = [P.sb(f"D2_MT{i}", [128, 16, 512], U8) for i in range(2)]
    KV_b = [Buf(f"D2_KV{i}") for i in range(2)]
    NPT = 3
    PTr = [P.sb(f"D2_PTr{i}", [128, 512], BF16) for i in range(NPT)]
    PTr_b = [Buf(f"D2_PTr{i}") for i in range(NPT)]
    PTm = [P.sb(f"D2_PTm{i}", [128, 512], BF16) for i in range(NPT)]
    PTm_b = [Buf(f"D2_PTm{i}") for i in range(NPT)]
    sps = [P.ps(f"D2_S{i}", [128, 512], F32) for i in range(3)]
    sps_b = [Buf(f"D2_S{i}") for i in range(3)]
    oacc = [P.ps(f"D2_O{i}", [128, 512], F32) for i in range(2)]
    oacc_b = [Buf(f"D2_O{i}") for i in range(2)]
    zacc = [P.ps(f"D2_Z{i}", [128, 512], F32) for i in range(2)]
    zacc_b = [Buf(f"D2_Z{i}") for i in range(2)]
    rz = P.sb("D2_rz", [128, 512], F32)
    rz_b = Buf("D2_rz")
    oo = [P.sb(f"D2_oo{i}", [128, 512], BF16) for i in range(2)]
    oo_b = [Buf(f"D2_oo{i}") for i in range(2)]
    st = dict(kv=0, s=0, p=0, o=0, q=0)
    items = [(g, hp) for g in groups for hp in range(2)]

    def load_q(idx):
        g, hp = items[idx]
        b = idx % 2
        for h2 in range(2):
            h = 2 * hp + h2
            P.dma("sp", QA[b][:, h2, :], qaT[128 * h:128 * (h + 1), 512 * g:512 * (g + 1)], writes=[QA_b[b]])

    def load_kv(g, hp, r):
        kb = st["kv"] % 2
        st["kv"] += 1
        nk = 4 * g + 4
        nkeys = 128 * nk
        for h2 in range(2):
            h = 2 * hp + h2
            P.dma("sp", KAr[kb][:, h2, 0:nkeys], kaT[r * 512 + 128 * h:r * 512 + 128 * (h + 1), 0:nkeys], writes=[KV_b[kb]])
        P.dma("sp", VAr[kb][:, 0:nk, :], bass.AP(tensor=va.tensor, offset=va.offset + r * TPC * 512 + 256 * hp,
                                                 ap=[[512, 128], [128 * 512, nk], [1, 256]]), writes=[KV_b[kb]])
        P.dma("sp", MTr[kb][:, 0:nk, :], mT_ap(g, r, 0, nk, 0, 4), reads=[mTd_b], writes=[KV_b[kb]])
        return kb

    for idx, (g, hp) in enumerate(items):
        b = idx % 2
        if idx == 0:
            load_q(0)
        if idx + 1 < len(items):
            load_q(idx + 1)
        nk = 4 * g + 4
        tiles = []
        kbs = {}
        kbs[0] = load_kv(g, hp, 0)
        for r in range(8):
            for jk in range(nk):
                for h2 in range(2):
                    tiles.append((r, jk, h2))
        ntile_h = 8 * nk

        def s_mm(t):
            r, jk, h2 = tiles[t]
            s_i = (st["s"] + t) % 3
            kb = kbs[r]
            P.op("pe", lambda e: e.matmul(out=sps[s_i][:], lhsT=KAr[kb][:, h2, 128 * jk:128 * (jk + 1)], rhs=QA[b][:, h2, :],
                                          start=True, stop=True), reads=[KV_b[kb], QA_b[b]], writes=[sps_b[s_i]])

        s_mm(0)
        for t in range(len(tiles)):
            r, jk, h2 = tiles[t]
            if jk == 0 and h2 == 0 and r + 1 < 8:
                kbs[r + 1] = load_kv(g, hp, r + 1)
            if t + 1 < len(tiles):
                s_mm(t + 1)
            s_i = (st["s"] + t) % 3
            p_i = (st["p"] + t) % NPT
            kb = kbs[r]
            P.op("act", lambda e: e.activation(out=PTr[p_i][:], in_=sps[s_i][:], func=AF.Exp, scale=asc), reads=[sps_b[s_i]],
                 writes=[PTr_b[p_i]])
            eng = "dve" if (t % 3 != 2) else "pool"
            P.op(eng, lambda e: e.tensor_tensor(out=PTm[p_i][:], in0=PTr[p_i][:], in1=MTr[kb][:, jk, :], op=ALU.mult),
                 reads=[PTr_b[p_i], KV_b[kb]], writes=[PTm_b[p_i]])
            th = r * nk + jk
            P.op("pe", lambda e: e.matmul(out=oacc[h2][:], lhsT=VAr[kb][:, jk, 128 * h2:128 * (h2 + 1)], rhs=PTm[p_i][:],
                                          start=(th == 0), stop=(th == ntile_h - 1)), reads=[KV_b[kb], PTm_b[p_i]],
                 writes=[oacc_b[h2]])
            P.op("pe", lambda e: e.matmul(out=zacc[h2][:], lhsT=ones[:], rhs=PTm[p_i][:], start=(th == 0),
                                          stop=(th == ntile_h - 1)), reads=[ones_b, PTm_b[p_i]], writes=[zacc_b[h2]])
        st["s"] += len(tiles)
        st["p"] += len(tiles)
        for h2 in range(2):
            h = 2 * hp + h2
            P.op("dve", lambda e: e.reciprocal(out=rz[:], in_=zacc[h2][:]), reads=[zacc_b[h2]], writes=[rz_b])
            ob = st["o"] % 2
            st["o"] += 1
            P.op("dve", lambda e: e.tensor_tensor(out=oo[ob][:], in0=oacc[h2][:], in1=rz[:], op=ALU.mult),
                 reads=[oacc_b[h2], rz_b], writes=[oo_b[ob]])
            P.dma("sp", T["catT"][128 * h:128 * (h + 1), 512 * g:512 * (g + 1)], oo[ob][:], reads=[oo_b[ob]])
```
